# Optimizing a Trainium2 kernel written in Bass

```python
import math
import jax, jax.numpy as jnp
from jax import lax
import numpy as np

D_MODEL = 1024
BATCH = 4
SEQ = 8192
DEPTH = 2

CHUNK = 64
D_MIX = D_MODEL
HEAD_DIM = 64
SGU_WIDTH = D_MIX // 4
SGU_HEADS = SGU_WIDTH // HEAD_DIM
SGU_BLOCK = 128
S5_WIDTH = D_MIX // 4
S5_GROUP = 16
S5_N_GROUPS = S5_WIDTH // S5_GROUP
S5_STATE = 64
S5_DT_MIN = 0.001
S5_DT_MAX = 0.1
ATTN_WIDTH = D_MIX // 2
ATTN_HEADS = ATTN_WIDTH // HEAD_DIM
BAND_CHUNKS = 9
BAND = BAND_CHUNKS * CHUNK
MAX_REL = 256
IN_COLS = 2 * SGU_WIDTH + S5_WIDTH + 3 * ATTN_WIDTH
OUT_NORM_GROUP = 64
N_EXPERT_GROUPS = 4
EXPERTS_PER_GROUP = 4
N_EXPERTS = N_EXPERT_GROUPS * EXPERTS_PER_GROUP
TOP_K = 2
D_EXPERT = D_MODEL // 4
EPS = 1e-6
NEG_INF = -1e30

kernel_name = "hybrid_chunk_causal_parallel_heads_hmoe"


def rms_norm(x, g):
    xf = x.astype(jnp.float32)
    y = xf * lax.rsqrt(jnp.mean(xf * xf, axis=-1, keepdims=True) + EPS)
    return (y * g.astype(jnp.float32)).astype(x.dtype)


def sgu_mixer(z, norm_g, w_s, b_s):
    b_, s_, _ = z.shape
    u, v = jnp.split(z, 2, axis=-1)
    v = rms_norm(v, norm_g)
    v = v.reshape(b_, s_ // SGU_BLOCK, SGU_BLOCK, SGU_HEADS, HEAD_DIM)
    chunk_of = jnp.arange(SGU_BLOCK) // CHUNK
    mask = chunk_of[None, :] <= chunk_of[:, None]
    w = jnp.where(mask[None], w_s, 0).astype(v.dtype)
    v = jnp.einsum('hij,bnjhc->bnihc', w, v) + b_s.T[None, None, :, :, None].astype(v.dtype)
    return u * v.reshape(b_, s_, SGU_WIDTH)


def s5_mixer(u, lam_re, lam_im, log_dt, b_re, b_im, c_re, c_im, d, glu_w, glu_b):
    f32 = jnp.float32
    b_, s_, _ = u.shape
    lam = lax.complex(lam_re.astype(f32), lam_im.astype(f32))
    dt = jnp.exp(log_dt.astype(f32))[:, None]
    a_bar = jnp.exp(lam * dt)
    b_mat = lax.complex(b_re.astype(f32), b_im.astype(f32))
    b_bar = ((a_bar - 1) / lam)[..., None] * b_mat
    c_mat = lax.complex(c_re.astype(f32), c_im.astype(f32))
    uf = u.astype(f32)
    ug = uf.reshape(b_, s_, S5_N_GROUPS, S5_GROUP)
    bu = jnp.einsum('gph,blgh->blgp', b_bar, ug.astype(jnp.complex64))
    a = jnp.broadcast_to(a_bar, bu.shape)

    def combine(e1, e2):
        a1, x1 = e1
        a2, x2 = e2
        return a1 * a2, a2 * x1 + x2

    _, states = lax.associative_scan(combine, (a, bu), axis=1)
    y = jnp.einsum('ghp,blgp->blgh', c_mat, states).real.reshape(b_, s_, S5_WIDTH)
    y = jax.nn.gelu(y + d.astype(f32) * uf)
    y = y * jax.nn.sigmoid(y @ glu_w.astype(f32) + glu_b.astype(f32))
    return y.astype(u.dtype)


def band_attention(q, k, v, q_g, k_g, rel_bias):
    f32 = jnp.float32
    b_, s_, n_h, hd = q.shape
    n_c = s_ // CHUNK
    q = rms_norm(q, q_g)
    k = rms_norm(k, k_g)
    q_pos = jnp.arange(CHUNK) + (BAND_CHUNKS - 1) * CHUNK
    k_pos = jnp.arange(BAND)
    rel = jnp.clip(q_pos[:, None] - k_pos[None, :], -MAX_REL, MAX_REL) + MAX_REL
    bias = rel_bias.astype(f32)[:, rel]
    band_idx = jnp.arange(n_c)[:, None] + jnp.arange(BAND_CHUNKS)[None, :]
    valid = jnp.repeat(band_idx >= BAND_CHUNKS - 1, CHUNK, axis=1)
    scale = HEAD_DIM ** -0.5

    def one_seq(args):
        qs, ks, vs = args
        qc = qs.reshape(n_c, CHUNK, n_h, hd)
        pad = jnp.zeros(((BAND_CHUNKS - 1) * CHUNK, n_h, hd), ks.dtype)
        kc = jnp.concatenate([pad, ks], axis=0).reshape(n_c + BAND_CHUNKS - 1, CHUNK, n_h, hd)[band_idx]
        vc = jnp.concatenate([pad, vs], axis=0).reshape(n_c + BAND_CHUNKS - 1, CHUNK, n_h, hd)[band_idx]
        kc = kc.reshape(n_c, BAND, n_h, hd)
        vc = vc.reshape(n_c, BAND, n_h, hd)
        sc = jnp.einsum('cqhd,ckhd->chqk', qc, kc).astype(f32) * scale + bias[None]
        sc = jnp.where(valid[:, None, None, :], sc, NEG_INF)
        p = jax.nn.softmax(sc, axis=-1).astype(vs.dtype)
        o = jnp.einsum('chqk,ckhd->cqhd', p, vc)
        return o.reshape(s_, n_h * hd)

    return lax.map(one_seq, (q, k, v))


def hier_moe(x, wg, bg, we, be, w_gate, w_up, w_down):
    f32 = jnp.float32
    xf = x.astype(f32)
    g_prob = jax.nn.softmax(xf @ wg.astype(f32) + bg.astype(f32), axis=-1)
    g_top = jnp.argmax(g_prob, axis=-1)
    p_g = jnp.max(g_prob, axis=-1)
    e_logits = jnp.einsum('sd,gde->sge', xf, we.astype(f32)) + be.astype(f32)
    e_logits = jnp.take_along_axis(e_logits, g_top[:, None, None], axis=1)[:, 0]
    e_prob = jax.nn.softmax(e_logits, axis=-1)
    w2, i2 = lax.top_k(e_prob, TOP_K)
    w2 = w2 / jnp.sum(w2, axis=-1, keepdims=True)
    expert_id = g_top[:, None] * EXPERTS_PER_GROUP + i2
    gate = jnp.sum(jax.nn.one_hot(expert_id, N_EXPERTS, dtype=f32) * (p_g[:, None] * w2)[..., None], axis=1)
    h = jax.nn.silu(jnp.einsum('sd,edf->sef', x, w_gate)) * jnp.einsum('sd,edf->sef', x, w_up)
    h = h * gate[:, :, None].astype(h.dtype)
    return jnp.einsum('sef,efd->sd', h, w_down)


def setup_inputs(seed: int = 0) -> dict:
    key = jax.random.key(seed)
    ks = jax.random.split(key, 32)
    f32 = jnp.float32
    L = DEPTH
    nrm = lambda k, shape, s: jax.random.normal(k, shape, f32) * s
    lam_im0 = jnp.pi * jnp.arange(S5_STATE, dtype=f32)
    return {
        "x": nrm(ks[0], (BATCH, SEQ, D_MODEL), 1.0),
        "norm_mix": 1.0 + nrm(ks[1], (L, D_MODEL), 0.02),
        "w_in": nrm(ks[2], (L, D_MODEL, IN_COLS), D_MODEL ** -0.5),
        "sgu_norm": 1.0 + nrm(ks[3], (L, SGU_WIDTH), 0.02),
        "sgu_w": nrm(ks[4], (L, SGU_HEADS, SGU_BLOCK, SGU_BLOCK), 0.5 * SGU_BLOCK ** -0.5),
        "sgu_b": 1.0 + nrm(ks[5], (L, SGU_HEADS, SGU_BLOCK), 0.02),
        "s5_lambda_re": -0.5 + nrm(ks[6], (L, S5_N_GROUPS, S5_STATE), 0.01),
        "s5_lambda_im": lam_im0 + nrm(ks[7], (L, S5_N_GROUPS, S5_STATE), 0.01),
        "s5_log_dt": jax.random.uniform(ks[8], (L, S5_N_GROUPS), f32, math.log(S5_DT_MIN), math.log(S5_DT_MAX)),
        "s5_b_re": nrm(ks[9], (L, S5_N_GROUPS, S5_STATE, S5_GROUP), (2.0 * S5_GROUP) ** -0.5),
        "s5_b_im": nrm(ks[10], (L, S5_N_GROUPS, S5_STATE, S5_GROUP), (2.0 * S5_GROUP) ** -0.5),
        "s5_c_re": nrm(ks[11], (L, S5_N_GROUPS, S5_GROUP, S5_STATE), (2.0 * S5_STATE) ** -0.5),
        "s5_c_im": nrm(ks[12], (L, S5_N_GROUPS, S5_GROUP, S5_STATE), (2.0 * S5_STATE) ** -0.5),
        "s5_d": nrm(ks[13], (L, S5_WIDTH), 1.0),
        "s5_glu_w": nrm(ks[14], (L, S5_WIDTH, S5_WIDTH), S5_WIDTH ** -0.5),
        "s5_glu_b": nrm(ks[15], (L, S5_WIDTH), 0.02),
        "q_norm": 1.0 + nrm(ks[16], (L, HEAD_DIM), 0.02),
        "k_norm": 1.0 + nrm(ks[17], (L, HEAD_DIM), 0.02),
        "rel_bias": nrm(ks[18], (L, ATTN_HEADS, 2 * MAX_REL + 1), 0.1),
        "out_norm": 1.0 + nrm(ks[19], (L, D_MIX), 0.02),
        "w_out": nrm(ks[20], (L, D_MIX, D_MODEL), D_MIX ** -0.5),
        "norm_ffn": 1.0 + nrm(ks[21], (L, D_MODEL), 0.02),
        "router_group_w": nrm(ks[22], (L, D_MODEL, N_EXPERT_GROUPS), D_MODEL ** -0.5),
        "router_group_b": nrm(ks[23], (L, N_EXPERT_GROUPS), 0.01),
        "router_expert_w": nrm(ks[24], (L, N_EXPERT_GROUPS, D_MODEL, EXPERTS_PER_GROUP), D_MODEL ** -0.5),
        "router_expert_b": nrm(ks[25], (L, N_EXPERT_GROUPS, EXPERTS_PER_GROUP), 0.01),
        "w_gate": nrm(ks[26], (L, N_EXPERTS, D_MODEL, D_EXPERT), D_MODEL ** -0.5),
        "w_up": nrm(ks[27], (L, N_EXPERTS, D_MODEL, D_EXPERT), D_MODEL ** -0.5),
        "w_down": nrm(ks[28], (L, N_EXPERTS, D_EXPERT, D_MODEL), D_EXPERT ** -0.5),
    }


def reference(x, norm_mix, w_in, sgu_norm, sgu_w, sgu_b, s5_lambda_re, s5_lambda_im, s5_log_dt,
              s5_b_re, s5_b_im, s5_c_re, s5_c_im, s5_d, s5_glu_w, s5_glu_b, q_norm, k_norm, rel_bias,
              out_norm, w_out, norm_ffn, router_group_w, router_group_b, router_expert_w,
              router_expert_b, w_gate, w_up, w_down):
    b_, s_, _ = x.shape
    for l in range(DEPTH):
        h = rms_norm(x, norm_mix[l])
        proj = h @ w_in[l]
        z_sgu, z_s5, z_qkv = jnp.split(proj, [2 * SGU_WIDTH, 2 * SGU_WIDTH + S5_WIDTH], axis=-1)
        o_a = sgu_mixer(jax.nn.gelu(z_sgu), sgu_norm[l], sgu_w[l], sgu_b[l])
        o_b = s5_mixer(z_s5, s5_lambda_re[l], s5_lambda_im[l], s5_log_dt[l], s5_b_re[l], s5_b_im[l],
                       s5_c_re[l], s5_c_im[l], s5_d[l], s5_glu_w[l], s5_glu_b[l])
        q, k, v = jnp.split(z_qkv.reshape(b_, s_, 3, ATTN_HEADS, HEAD_DIM), 3, axis=2)
        o_c = band_attention(q[:, :, 0], k[:, :, 0], v[:, :, 0], q_norm[l], k_norm[l], rel_bias[l])
        o = jnp.concatenate([o_a, o_b, o_c], axis=-1)
        o = rms_norm(o.reshape(b_, s_, D_MIX // OUT_NORM_GROUP, OUT_NORM_GROUP),
                     out_norm[l].reshape(D_MIX // OUT_NORM_GROUP, OUT_NORM_GROUP)).reshape(b_, s_, D_MIX)
        x = x + o @ w_out[l]
        h = rms_norm(x, norm_ffn[l])
        moe = lambda hs, l=l: hier_moe(hs, router_group_w[l], router_group_b[l], router_expert_w[l],
                                       router_expert_b[l], w_gate[l], w_up[l], w_down[l])
        x = x + lax.map(moe, h)
    return x
```

```python
import math
from contextlib import ExitStack
import numpy as np
import concourse.bass as bass
import concourse.mybir as mybir
from concourse.bass_utils import run_bass_kernel_spmd

F32 = mybir.dt.float32
BF16 = mybir.dt.bfloat16
I32 = mybir.dt.int32
AF = mybir.ActivationFunctionType
ALU = mybir.AluOpType
AX = mybir.AxisListType

D = 1024
IN_COLS = 2304
EPS = 1e-6
NEG = -30000.0
TWO_PI = 2.0 * math.pi


class Sem:
    def __init__(self, h):
        self.h = h
        self.count = 0


class Buf:
    __slots__ = ("name", "w", "r", "sem")

    def __init__(self, name=""):
        self.name = name
        self.w = {}
        self.r = {}
        self.sem = None


ENGS = ["tensor", "vector", "scalar", "gpsimd", "sync"]


class Prog:
    def __init__(self, nc, stack, npool=94):
        self.nc = nc
        self.esem = {e: Sem(stack.enter_context(nc.semaphore("e_" + e))) for e in ENGS}
        nsw = 40
        self.pools = {"sw": [Sem(stack.enter_context(nc.semaphore("w%d" % i))) for i in range(nsw)],
                      "hw": [Sem(stack.enter_context(nc.semaphore("d%d" % i))) for i in range(npool - nsw)]}
        self.pool_i = {"sw": 0, "hw": 0}
        self.ops = {e: [] for e in ENGS}
        self.seen = {e: {} for e in ENGS}
        self.nops = 0

    def dsem(self, buf, eng):
        if buf.sem is None:
            k = "sw" if eng == "gpsimd" else "hw"
            assert self.pool_i[k] < len(self.pools[k]), "out of %s semaphores" % k
            buf.sem = self.pools[k][self.pool_i[k]]
            self.pool_i[k] += 1
        return buf.sem

    def op(self, eng, fn, reads=(), writes=(), dma=False, store=False):
        need = {}
        es = self.esem[eng]

        def add(d, skip_own):
            for sm, v in d.items():
                if skip_own and sm is es:
                    continue
                if need.get(sm, 0) < v:
                    need[sm] = v

        for b in reads:
            add(b.w, False)
        so = (eng == "tensor") and not dma
        for b in writes:
            if not store:
                add(b.w, so)
            add(b.r, so)
        waits = []
        seen = self.seen[eng]
        for sm, v in need.items():
            if seen.get(sm, 0) >= v:
                continue
            seen[sm] = v
            waits.append((sm, v))
        if fn is None:
            self.ops[eng].append((waits, None, None, 0))
            return
        if dma:
            sm = self.dsem(reads[0] if store else writes[0], eng)
            sm.count += 16
            tok = (sm, sm.count)
            inc = 16
        else:
            es.count += 1
            tok = (es, es.count)
            inc = 1
        for b in reads:
            if b.r.get(tok[0], 0) < tok[1]:
                b.r[tok[0]] = tok[1]
        for b in writes:
            if store:
                b.w[tok[0]] = tok[1]
            else:
                b.w = {tok[0]: tok[1]}
                b.r = {}
        self.ops[eng].append((waits, fn, tok[0], inc))
        self.nops += 1

    def barrier(self):
        for e in ENGS:
            waits = []
            seen = self.seen[e]
            for sm in list(self.esem.values()) + self.pools["sw"][: self.pool_i["sw"]] + self.pools["hw"][: self.pool_i["hw"]]:
                if sm.count == 0:
                    continue
                if seen.get(sm, 0) >= sm.count:
                    continue
                seen[sm] = sm.count
                waits.append((sm, sm.count))
            self.ops[e].append((waits, None, None, 0))

    def emit(self, block):
        for e in ENGS:
            ops = self.ops[e]

            def body(eng, ops=ops):
                for waits, fn, sm, inc in ops:
                    for (w, v) in waits:
                        eng.wait_ge(w.h, v)
                    if fn is not None:
                        fn(eng).then_inc(sm.h, inc)

            getattr(block, e)(body)


class Tile:
    def __init__(self, h, name):
        self.h = h
        self.b = Buf(name)
        self.sub = {}

    def __getitem__(self, k):
        return self.h[k]

    def sb(self, key):
        if key not in self.sub:
            self.sub[key] = Buf("%s/%s" % (self.b.name, key))
        return self.sub[key]


class SBAlloc:
    def __init__(self, nc):
        self.nc = nc
        self.top = 16640
        self.lim = 229376 - 64
        self.top2 = self.lim
        self.n = 0
        self.peak = 0
        self.P = None

    def tile(self, name, shape, dt, top=False):
        esz = {F32: 4, BF16: 2, I32: 4}[dt]
        nb = esz
        for s in shape[1:]:
            nb *= s
        if top:
            off = (self.top2 - nb) // 64 * 64
            self.top2 = off
        else:
            off = (self.top + 63) // 64 * 64
            self.top = off + nb
        self.peak = max(self.peak, self.top + (self.lim - self.top2))
        assert self.top <= self.top2, "SBUF overflow at %s: %d/%d" % (name, self.top, self.top2)
        self.n += 1
        h = self.nc.alloc_sbuf_tensor_at("%s_%d" % (name, self.n), list(shape), dt, offset=off)
        return Tile(h, name)

    def mark(self):
        return (self.top, dict(self.P.pool_i) if self.P else None, self.top2)

    def release(self, m):
        self.top = m[0]
        self.top2 = m[2]
        if self.P:
            self.P.pool_i = dict(m[1])


def _rep(a, n=128):
    return np.ascontiguousarray(np.broadcast_to(a[None], (n,) + a.shape)).astype(np.float32)


def host_layouts(inp, L):
    f = np.float32
    o = {}
    o["w_in"] = np.ascontiguousarray(inp["w_in"], dtype=f)
    ws5 = np.zeros((L, D, 4, 4, 32), f)
    ws5[:, :, :, :, :16] = inp["w_in"][:, :, 512:768].reshape(L, D, 4, 4, 16)
    o["w_s5"] = ws5.reshape(L, D, 512)
    gT = lambda g: np.ascontiguousarray(np.broadcast_to(g.reshape(L, 8, 128).transpose(0, 2, 1)[:, :, :, None], (L, 128, 8, 128))).astype(f)
    o["gT_mix"] = gT(inp["norm_mix"])
    o["gT_ffn"] = gT(inp["norm_ffn"])
    o["g_sgu"] = np.stack([_rep(inp["sgu_norm"][l]) for l in range(L)])
    o["wsT"] = np.ascontiguousarray(inp["sgu_w"].transpose(0, 3, 1, 2)).astype(f)
    o["bsT"] = np.ascontiguousarray(inp["sgu_b"].transpose(0, 2, 1)).astype(f)
    o["g_q"] = np.stack([_rep(np.tile(inp["q_norm"][l], 8)) for l in range(L)])
    o["g_k"] = np.stack([_rep(np.tile(inp["k_norm"][l], 8)) for l in range(L)])
    ki = np.arange(128)[:, None, None]
    kt = np.arange(5)[None, :, None]
    qi = np.arange(128)[None, None, :]
    idx = np.clip(128 * (4 - kt) + qi - ki, -256, 256) + 256
    o["biasT"] = np.ascontiguousarray(inp["rel_bias"][:, :, idx].transpose(0, 2, 1, 3, 4)).astype(f)
    o["g_oa"] = np.stack([_rep(inp["out_norm"][l, 0:256]) for l in range(L)])
    o["g_oc"] = np.stack([_rep(inp["out_norm"][l, 512:1024]) for l in range(L)])
    o["g_ob"] = np.ascontiguousarray(inp["out_norm"][:, 256:512].reshape(L, 2, 128).transpose(0, 2, 1)).astype(f)
    o["w_out"] = np.ascontiguousarray(inp["w_out"], dtype=f)
    wr = np.concatenate([inp["router_group_w"], inp["router_expert_w"].transpose(0, 2, 1, 3).reshape(L, D, 16)], axis=2)
    o["w_r"] = np.ascontiguousarray(wr).astype(f)
    br = np.concatenate([inp["router_group_b"], inp["router_expert_b"].reshape(L, 16)], axis=1)
    o["b_r"] = np.stack([_rep(br[l]) for l in range(L)])
    o["w_gate"] = np.ascontiguousarray(inp["w_gate"], dtype=f)
    o["w_up"] = np.ascontiguousarray(inp["w_up"], dtype=f)
    o["w_down"] = np.ascontiguousarray(inp["w_down"], dtype=f)
    lre, lim, ldt = inp["s5_lambda_re"], inp["s5_lambda_im"], inp["s5_log_dt"]
    bre, bim, cre, cim = inp["s5_b_re"], inp["s5_b_im"], inp["s5_c_re"], inp["s5_c_im"]
    def layA(v):
        return np.ascontiguousarray(v.reshape(L, 8, 2, 64).transpose(0, 2, 3, 1).reshape(L, 128, 8)).astype(f)
    o["lamA"] = np.stack([layA(lre), layA(lim), layA(np.broadcast_to(ldt[:, :, None], (L, 16, 64)))], axis=2)
    cA = np.zeros((L, 2, 2, 64, 8, 128), f)
    bZ = np.zeros((L, 2, 2, 64, 8, 64), f)
    for g in range(16):
        P_, gl2 = g // 2, g % 2
        c0 = 16 * (g % 8)
        cA[:, 0, gl2, :, P_, c0:c0 + 16] = cre[:, g].transpose(0, 2, 1)
        cA[:, 1, gl2, :, P_, c0:c0 + 16] = cim[:, g].transpose(0, 2, 1)
        bZ[:, 0, gl2, :, P_, 32 * gl2:32 * gl2 + 16] = bre[:, g]
        bZ[:, 1, gl2, :, P_, 32 * gl2:32 * gl2 + 16] = bim[:, g]
    o["cA"] = np.ascontiguousarray(cA.reshape(L, 2, 128, 8, 128).transpose(0, 2, 1, 3, 4))
    o["bZ"] = np.ascontiguousarray(bZ.reshape(L, 2, 128, 8, 64).transpose(0, 2, 1, 3, 4))
    lamB = np.zeros((L, 3, 4, 32, 4, 2, 64), f)
    bB = np.zeros((L, 2, 4, 32, 4, 2, 64), f)
    for g in range(16):
        q, gl = g // 4, g % 4
        lamB[:, 0, gl, :, q, :, :] = lre[:, g][:, None, None, :]
        lamB[:, 1, gl, :, q, :, :] = lim[:, g][:, None, None, :]
        lamB[:, 2, gl, :, q, :, :] = ldt[:, g][:, None, None, None]
        bB[:, 0, gl, :16, q, gl % 2, :] = bre[:, g].transpose(0, 2, 1)
        bB[:, 1, gl, :16, q, gl % 2, :] = bim[:, g].transpose(0, 2, 1)
    o["lamB"] = np.ascontiguousarray(lamB.reshape(L, 3, 128, 512).transpose(0, 2, 1, 3))
    o["bB"] = np.ascontiguousarray(bB.reshape(L, 2, 128, 512).transpose(0, 2, 1, 3))
    o["d_bc"] = np.stack([_rep(inp["s5_d"][l]) for l in range(L)])
    o["glu_w"] = np.ascontiguousarray(inp["s5_glu_w"], dtype=f)
    o["glu_b"] = np.ascontiguousarray(inp["s5_glu_b"].reshape(L, 2, 128).transpose(0, 2, 1)).astype(f)
    o["c_ident"] = np.eye(128, dtype=f)
    ch = np.arange(128) // 64
    o["c_maskT"] = (ch[:, None] <= ch[None, :]).astype(f)
    am = np.zeros((128, 2, 128), f)
    am[:64, 0, 64:] = NEG
    am[64:, 1, :64] = NEG
    o["c_amask"] = am
    E = np.zeros((128, 4, 128), f)
    for g in range(16):
        q, gl = g // 4, g % 4
        for h in range(16):
            E[32 * gl + h, q, 16 * (g % 8) + h] = 1.0
    o["c_E"] = E
    kv = np.zeros((128, 9, 1), f)
    kv[:, :, 0] = np.arange(9)[None, :]
    o["c_kvec"] = kv
    blk = np.arange(128) // 64
    o["c_blk64"] = (blk[:, None] == blk[None, :]).astype(f)
    return o


def build(cfg, shapes):
    S = cfg["S"]
    L = cfg["L"]
    SEG = cfg.get("SEG") or min(S, 4096)
    NSEG = S // SEG
    NT = SEG // 128
    NB = min(SEG, 1024)
    NBT = NB // 128
    NCH = SEG // 8
    dbg = cfg.get("debug")

    nc = bass.Bass("TRN2", target_bir_lowering=False)
    dr = {k: nc.dram_tensor(k, list(v), F32, kind="ExternalInput").ap() for k, v in shapes.items()}
    out_d = nc.dram_tensor("out", [S, D], F32, kind="ExternalOutput").ap()
    x1_d = nc.dram_tensor("x1s", [S, D], F32, kind="Internal").ap() if L > 1 else None
    oac_d = nc.dram_tensor("oac", [SEG, 768], BF16, kind=("ExternalOutput" if dbg else "Internal")).ap()
    dbg_d = {}
    if dbg:
        dbg_d["ob"] = nc.dram_tensor("dbg_ob", [128, 2, SEG], F32, kind="ExternalOutput").ap()
        dbg_d["xm"] = nc.dram_tensor("dbg_xm", [S, D], F32, kind="ExternalOutput").ap()

    NCH_ = SEG // 8
    dr["_s5cache"] = {
        "WSk": nc.dram_tensor("s5c_WSk", [128, 8 * 2 * 512], BF16, kind="Internal").ap(),
        "BD": nc.dram_tensor("s5c_BD", [128, 4 * 8 * 128], BF16, kind="Internal").ap(),
        "CA": nc.dram_tensor("s5c_CA", [128, 8 * 2 * 8 * 128], BF16, kind="Internal").ap(),
        "Tc": nc.dram_tensor("s5c_Tc", [128, 8 * NCH_], F32, kind="Internal").ap(),
        "Ts": nc.dram_tensor("s5c_Ts", [128, 8 * NCH_], F32, kind="Internal").ap(),
        "rho8": nc.dram_tensor("s5c_rho8", [128, 8], F32, kind="Internal").ap(),
        "buf": Buf("s5cache"),
    }
    stack = ExitStack()
    P = Prog(nc, stack)
    sba = SBAlloc(nc)
    sba.P = P
    banks = []
    for i in range(8):
        banks.append(Tile(nc.alloc_psum_tensor("bank%d" % i, [128, 512], F32), "bank%d" % i))
    bank_i = [0]

    def psum():
        t = banks[bank_i[0] % 6]
        bank_i[0] += 1
        return t

    def Vv(fn, r, w):
        P.op("vector", fn, r, w)

    def Aa(fn, r, w):
        P.op("scalar", fn, r, w)

    def Gg(fn, r, w):
        P.op("gpsimd", fn, r, w)

    def Tt(fn, r, w):
        P.op("tensor", fn, r, w)

    def DMA(eng, out_ap, in_ap, r, w, store=False):
        P.op(eng, lambda e: e.dma_start(out=out_ap, in_=in_ap), r, w, dma=True, store=store)

    xin_bufs = []
    for l_ in range(L + 1):
        grp = [Buf("x%d_%d" % (l_, t)) for t in range((S // 128 + 7) // 8)]
        xin_bufs.append([grp[t // 8] for t in range(S // 128)])
    ogrp = [Buf("oac%d" % t) for t in range((NT + 3) // 4)]
    oac_bufs = [ogrp[t // 4] for t in range(NT)]
    x_aps = [dr["x"]] + [x1_d] * (L - 1) + [out_d]
    cb = Buf("consts")

    ident_f = sba.tile("ident_f", [128, 128], F32)
    ident_b = sba.tile("ident_b", [128, 128], BF16)
    eps_t = sba.tile("eps", [128, 1], F32)
    DMA("sync", ident_f[:], dr["c_ident"], [], [ident_f.b])
    DMA("gpsimd", ident_b[:], dr["c_ident"], [], [ident_b.b])
    Vv(lambda e: e.memset(eps_t[:], EPS), [], [eps_t.b])
    s5_state = sba.tile("s5state", [128, 8, 2], F32)
    if dbg:
        dbg_d["_b"] = Buf("dbg")

    for l in range(L):
        x_in = x_aps[l]
        x_out = x_aps[l + 1]
        def do_seg(l, seg, x_in, x_out):
            t0 = seg * NT
            mk_seg = sba.mark()
            obT = sba.tile("obT", [128, 2, SEG], BF16)
            mk_UT = sba.mark()
            UT = sba.tile("UT", [128, 4, SEG], BF16)
            UTb = [Buf("UT%d" % t) for t in range(NT)]
            mk_A = sba.mark()
            w_in_sb = sba.tile("w_in", [128, 8, IN_COLS], BF16)
            w_s5_sb = sba.tile("w_s5", [128, 8, 512], BF16)
            w_in_v = dr["w_in"][l].rearrange("(dc p) c -> p dc c", p=128)
            w_s5_v = dr["w_s5"][l].rearrange("(dc p) c -> p dc c", p=128)
            for dc in range(0, 8, 2):
                DMA("gpsimd", w_in_sb[:, dc:dc + 2, :], w_in_v[:, dc:dc + 2, :], [], [w_in_sb.sb(dc)])
            DMA("gpsimd", w_s5_sb[:], w_s5_v, [], [w_s5_sb.b])
            w_in_bufs = [w_in_sb.sb(dc) for dc in range(0, 8, 2)]
            gT = sba.tile("gT", [128, 1024], F32)
            DMA("sync", gT[:], dr["gT_mix"][l].rearrange("p a b -> p (a b)"), [], [gT.b])
            g_sgu = sba.tile("g_sgu", [128, 256], F32)
            DMA("sync", g_sgu[:], dr["g_sgu"][l], [], [g_sgu.b])
            wsT_f = sba.tile("wsT_f", [128, 4, 128], F32)
            DMA("sync", wsT_f[:], dr["wsT"][l], [], [wsT_f.b])
            maskT = sba.tile("maskT", [128, 128], F32)
            DMA("sync", maskT[:], dr["c_maskT"], [], [maskT.b])
            wsT_b = sba.tile("wsT_b", [128, 4, 128], BF16)
            Vv(lambda e: e.tensor_tensor(out=wsT_b[:], in0=wsT_f[:], in1=maskT[:].unsqueeze(1).to_broadcast([128, 4, 128]), op=ALU.mult),
               [wsT_f.b, maskT.b], [wsT_b.b])
            bsT = sba.tile("bsT", [128, 4], F32)
            DMA("sync", bsT[:], dr["bsT"][l], [], [bsT.b])
            g_q = sba.tile("g_q", [128, 512], F32)
            g_k = sba.tile("g_k", [128, 512], F32)
            g_oa = sba.tile("g_oa", [128, 256], F32)
            g_oc = sba.tile("g_oc", [128, 512], F32)
            DMA("sync", g_q[:], dr["g_q"][l], [], [g_q.b])
            DMA("sync", g_k[:], dr["g_k"][l], [], [g_k.b])
            DMA("sync", g_oa[:], dr["g_oa"][l], [], [g_oa.b])
            DMA("sync", g_oc[:], dr["g_oc"][l], [], [g_oc.b])
            biasT = sba.tile("biasT", [128, 8, 5, 128], BF16)
            mk_tmp = sba.mark()
            biasT_f = sba.tile("biasT_f", [128, 8, 5, 128], F32)
            amask = sba.tile("amask", [128, 2, 128], F32)
            DMA("sync", biasT_f[:], dr["biasT"][l], [], [biasT_f.b])
            DMA("sync", amask[:], dr["c_amask"], [], [amask.b])
            Vv(lambda e: e.tensor_tensor(out=biasT_f[:, :, 0, :], in0=biasT_f[:, :, 0, :], in1=amask[:, 0:1, :].to_broadcast([128, 8, 128]), op=ALU.add),
               [biasT_f.b, amask.b], [biasT_f.b])
            Vv(lambda e: e.tensor_tensor(out=biasT_f[:, :, 4, :], in0=biasT_f[:, :, 4, :], in1=amask[:, 1:2, :].to_broadcast([128, 8, 128]), op=ALU.add),
               [biasT_f.b, amask.b], [biasT_f.b])
            Aa(lambda e: e.activation(out=biasT[:], in_=biasT_f[:], func=AF.Exp), [biasT_f.b], [biasT.b])
            P.barrier()
            sba.release(mk_tmp)
            KT = sba.tile("KT", [128, 4, 8 * 128], BF16)
            KTb = [Buf("KT%d" % s) for s in range(8)]
            Vx = sba.tile("Vx", [128, 8, 8, 65], BF16)
            Vxb = [Buf("Vx%d" % s) for s in range(8)]
            Vv(lambda e: e.memset(Vx[:], 1.0), [], [Vx.b] + Vxb)
            xts = [sba.tile("xt%d" % i, [128, 1024], F32) for i in range(2)]
            junk = sba.tile("junk", [128, 1024], BF16)
            stat = [sba.tile("stat%d" % i, [128, 96], F32) for i in range(2)]
            for st_ in stat:
                Vv(lambda e, st_=st_: e.memset(st_[:], 1.0), [], [st_.b] + [st_.sb(k_) for k_ in ("ssq", "ln", "rstd", "s2", "ln2", "r2", "s3", "ln3", "r3", "rs0", "rs1", "s4", "ln4", "r4")])
            hb = sba.tile("hb", [128, 1024], BF16)
            hTr = sba.tile("hTr", [128, 8, 512], BF16)
            hTb = [Buf("hT%d" % i) for i in range(4)]
            gz = sba.tile("gz", [128, 512], F32)
            vn = sba.tile("vn", [128, 256], BF16)
            oa = sba.tile("oa", [128, 256], F32)
            oa2 = sba.tile("oa2", [128, 256], F32)
            oan = sba.tile("oan", [128, 256], BF16)
            qraw = [sba.tile("qraw%d" % i, [128, 512], F32) for i in range(2)]
            kraw = [sba.tile("kraw%d" % i, [128, 512], F32) for i in range(2)]
            sq = sba.tile("sq", [128, 512], F32)
            sq2 = sba.tile("sq2", [128, 512], F32)
            qn = sba.tile("qn", [128, 512], BF16)
            kn = sba.tile("kn", [128, 512], BF16)
            qTs = [sba.tile("qT%d" % i, [128, 4, 128], BF16) for i in range(3)]
            PTs = [sba.tile("PT%d" % i, [128, 5, 128], BF16) for i in range(4)]
            oc = sba.tile("oc", [128, 8, 64], F32)
            oc2 = sba.tile("oc2", [128, 8, 64], F32)
            ocn = sba.tile("ocn", [128, 512], BF16)
            pt_i = [0]

            def ut_batch(tl0):
                for q in range(4):
                    pu = psum()

                    def mmu(e, q=q, pu=pu):
                        for dc in range(8):
                            ins = e.matmul(pu[:, 0:512], lhsT=w_s5_sb[:, dc, q * 128:(q + 1) * 128], rhs=hTr[:, dc, :], start=(dc == 0), stop=(dc == 7))
                        return ins
                    Tt(mmu, hTb + [w_s5_sb.b], [pu.b])
                    P.op("vector" if q % 2 == 0 else "scalar",
                         (lambda e, q=q, pu=pu: e.tensor_copy(out=UT[:, q, :].rearrange("p (j n) -> p j n", j=8)[:, :, tl0 * 16:tl0 * 16 + 64], in_=pu[:, 0:512].rearrange("p (n j) -> p j n", j=8))) if q % 2 == 0 else
                         (lambda e, q=q, pu=pu: e.activation(out=UT[:, q, :].rearrange("p (j n) -> p j n", j=8)[:, :, tl0 * 16:tl0 * 16 + 64], in_=pu[:, 0:512].rearrange("p (n j) -> p j n", j=8), func=AF.Copy)),
                         [pu.b], [UTb[tl0 + q]])

            def stage1(T, full):
                tl = T - t0
                pend_mix = []
                xt = xts[T % 2]
                st = stat[T % 2]
                hs = T % 4
                slot = T % 8
                DMA("sync", xt[:], x_in[T * 128:(T + 1) * 128, :], [xin_bufs[l][T]], [xt.b])
                Aa(lambda e: e.activation(out=junk[:], in_=xt[:], func=AF.Square, scale=1.0 / 32.0, accum_out=st[:, 0:1]),
                   [xt.b], [junk.b, st.sb("ssq")])
                Aa(lambda e: e.activation(out=st[:, 1:2], in_=st[:, 0:1], func=AF.Ln, bias=eps_t[:], scale=1.0),
                   [st.sb("ssq"), eps_t.b], [st.sb("ln")])
                Aa(lambda e: e.activation(out=st[:, 2:3], in_=st[:, 1:2], func=AF.Exp, scale=-0.5),
                   [st.sb("ln")], [st.sb("rstd")])
                Vv(lambda e: e.tensor_scalar(out=hb[:], in0=xt[:], scalar1=st[:, 2:3], scalar2=None, op0=ALU.mult),
                   [xt.b, st.sb("rstd")], [hb.b])
                yield
                pT = psum()
                pTv = pT[:].bitcast(BF16)

                def tr(e):
                    for dc in range(8):
                        ins = e.transpose(out=pTv[:, dc * 128:(dc + 1) * 128], in_=hb[:, dc * 128:(dc + 1) * 128], identity=ident_b[:])
                    return ins
                Tt(tr, [hb.b, ident_b.b], [pT.b])
                yield
                Vv(lambda e: e.tensor_tensor(out=hTr[:, :, hs * 128:(hs + 1) * 128], in0=pTv[:, 0:1024].rearrange("p (a b) -> p a b", a=8),
                                             in1=gT[:].rearrange("p (a b) -> p a b", a=8), op=ALU.mult),
                   [pT.b, gT.b], [hTb[hs]])
                yield

                def proj(c0, n=512):
                    ps = psum()

                    def mm(e):
                        for dc in range(8):
                            ins = e.matmul(ps[:, 0:n], lhsT=hTr[:, dc, hs * 128:(hs + 1) * 128], rhs=w_in_sb[:, dc, c0:c0 + n], start=(dc == 0), stop=(dc == 7))
                        return ins
                    Tt(mm, [hTb[hs]] + w_in_bufs, [ps.b])
                    return ps

                if full:
                    pz = proj(0)
                    yield
                    Aa(lambda e: e.activation(out=gz[:], in_=pz[:, 0:512], func=AF.Gelu_apprx_tanh), [pz.b], [gz.b])
                    yield
                    pq = proj(768)
                    yield
                    qr = qraw[T % 2]
                    Aa(lambda e: e.activation(out=qr[:], in_=pq[:, 0:512], func=AF.Copy), [pq.b], [qr.b])
                    yield
                pk = proj(1280)
                yield
                kr = kraw[T % 2]
                Aa(lambda e: e.activation(out=kr[:], in_=pk[:, 0:512], func=AF.Copy), [pk.b], [kr.b])
                yield
                pv = proj(1792)
                yield
                Aa(lambda e: e.activation(out=Vx[:, slot, :, 0:64], in_=pv[:, 0:512].rearrange("p (h d) -> p h d", h=8), func=AF.Copy),
                   [pv.b], [Vxb[slot]])
                yield
                rd = []
                if full:
                    Aa(lambda e: e.activation(out=junk[:, 0:256], in_=gz[:, 256:512], func=AF.Square, scale=1.0 / 16.0, accum_out=st[:, 8:9]),
                       [gz.b], [junk.b, st.sb("s2")])
                    Gg(lambda e: e.tensor_tensor(out=sq[:], in0=qr[:], in1=qr[:], op=ALU.mult), [qr.b], [sq.b])
                    Vv(lambda e: e.tensor_reduce(out=st[:, 16:24], in_=sq[:].rearrange("p (h d) -> p h d", h=8), axis=AX.X, op=ALU.add),
                       [sq.b], [st.sb("s2")])
                Gg(lambda e: e.tensor_tensor(out=sq2[:], in0=kr[:], in1=kr[:], op=ALU.mult), [kr.b], [sq2.b])
                Vv(lambda e: e.tensor_reduce(out=st[:, 24:32], in_=sq2[:].rearrange("p (h d) -> p h d", h=8), axis=AX.X, op=ALU.add),
                   [sq2.b], [st.sb("s2")])
                if not full:
                    Vv(lambda e: e.memset(st[:, 8:24], 1.0), [], [st.sb("s2")])
                if full:
                    Vv(lambda e: e.tensor_scalar(out=st[:, 8:9], in0=st[:, 8:9], scalar1=64.0, scalar2=None, op0=ALU.mult),
                       [st.sb("s2")], [st.sb("s2")])
                Aa(lambda e: e.activation(out=st[:, 32:56], in_=st[:, 8:32], func=AF.Ln, bias=eps_t[:], scale=1.0 / 64.0),
                   [st.sb("s2"), eps_t.b], [st.sb("ln2")])
                Aa(lambda e: e.activation(out=st[:, 32:56], in_=st[:, 32:56], func=AF.Exp, scale=-0.5),
                   [st.sb("ln2")], [st.sb("r2")])
                if full:
                    Vv(lambda e: e.scalar_tensor_tensor(out=vn[:], in0=gz[:, 256:512], scalar=st[:, 32:33], in1=g_sgu[:], op0=ALU.mult, op1=ALU.mult),
                       [gz.b, st.sb("r2"), g_sgu.b], [vn.b])
                    def sgu_mix():
                        pm = psum()

                        def mmx(e):
                            for hh in range(4):
                                ins = e.matmul(pm[:, hh * 64:(hh + 1) * 64], lhsT=wsT_b[:, hh, :], rhs=vn[:, hh * 64:(hh + 1) * 64], start=True, stop=True)
                            return ins
                        Tt(mmx, [wsT_b.b, vn.b], [pm.b])
                        yield

                        def sgu_out(e):
                            for hh in range(4):
                                ins = e.scalar_tensor_tensor(out=oa[:, hh * 64:(hh + 1) * 64], in0=pm[:, hh * 64:(hh + 1) * 64], scalar=bsT[:, hh:hh + 1],
                                                             in1=gz[:, hh * 64:(hh + 1) * 64], op0=ALU.add, op1=ALU.mult)
                            return ins
                        Vv(sgu_out, [pm.b, bsT.b, gz.b], [oa.b])
                        Gg(lambda e: e.tensor_tensor(out=oa2[:], in0=oa[:], in1=oa[:], op=ALU.mult), [oa.b], [oa2.b])
                        Vv(lambda e: e.tensor_reduce(out=st[:, 56:60], in_=oa2[:].rearrange("p (h d) -> p h d", h=4), axis=AX.X, op=ALU.add),
                           [oa2.b], [st.sb("s3")])
                        Aa(lambda e: e.activation(out=st[:, 60:64], in_=st[:, 56:60], func=AF.Ln, bias=eps_t[:], scale=1.0 / 64.0),
                           [st.sb("s3"), eps_t.b], [st.sb("ln3")])
                        Aa(lambda e: e.activation(out=st[:, 60:64], in_=st[:, 60:64], func=AF.Exp, scale=-0.5), [st.sb("ln3")], [st.sb("r3")])
                        Vv(lambda e: e.tensor_tensor(out=oa2[:].rearrange("p (h d) -> p h d", h=4), in0=oa[:].rearrange("p (h d) -> p h d", h=4),
                                                     in1=st[:, 60:64].unsqueeze(2).to_broadcast([128, 4, 64]), op=ALU.mult),
                           [oa.b, st.sb("r3")], [oa2.b])
                        Gg(lambda e: e.tensor_tensor(out=oan[:], in0=oa2[:], in1=g_oa[:], op=ALU.mult), [oa2.b, g_oa.b], [oan.b])
                        DMA("gpsimd", oac_d[tl * 128:(tl + 1) * 128, 0:256], oan[:], [oan.b], [oac_bufs[tl]], store=True)
                    pend_mix.append(sgu_mix)
                    Vv(lambda e: e.tensor_scalar(out=st[:, 40:48], in0=st[:, 40:48], scalar1=0.125, scalar2=None, op0=ALU.mult),
                       [st.sb("r2")], [st.sb("r2")])
                    Vv(lambda e: e.tensor_tensor(out=sq[:].rearrange("p (h d) -> p h d", h=8), in0=qr[:].rearrange("p (h d) -> p h d", h=8),
                                                 in1=st[:, 40:48].unsqueeze(2).to_broadcast([128, 8, 64]), op=ALU.mult),
                       [qr.b, st.sb("r2")], [sq.b])
                    Gg(lambda e: e.tensor_tensor(out=qn[:], in0=sq[:], in1=g_q[:], op=ALU.mult), [sq.b, g_q.b], [qn.b])
                Vv(lambda e: e.tensor_tensor(out=sq2[:].rearrange("p (h d) -> p h d", h=8), in0=kr[:].rearrange("p (h d) -> p h d", h=8),
                                             in1=st[:, 48:56].unsqueeze(2).to_broadcast([128, 8, 64]), op=ALU.mult),
                   [kr.b, st.sb("r2")], [sq2.b])
                Gg(lambda e: e.tensor_tensor(out=kn[:], in0=sq2[:], in1=g_k[:], op=ALU.mult), [sq2.b, g_k.b], [kn.b])
                yield
                def trans():
                    pkT = psum()
                    pkTv = pkT[:].bitcast(BF16)

                    def trk(e):
                        for hp in range(4):
                            ins = e.transpose(out=pkTv[:, hp * 128:(hp + 1) * 128], in_=kn[:, hp * 128:(hp + 1) * 128], identity=ident_b[:])
                        return ins
                    Tt(trk, [kn.b, ident_b.b], [pkT.b])
                    yield
                    Aa(lambda e: e.activation(out=KT[:, :, slot * 128:(slot + 1) * 128], in_=pkTv[:, 0:512].rearrange("p (a t) -> p a t", a=4), func=AF.Copy),
                       [pkT.b], [KTb[slot]])
                    if full:
                        qT = qTs[T % 3]
                        pqT = psum()
                        pqTv = pqT[:].bitcast(BF16)

                        def trq(e):
                            for hp in range(4):
                                ins = e.transpose(out=pqTv[:, hp * 128:(hp + 1) * 128], in_=qn[:, hp * 128:(hp + 1) * 128], identity=ident_b[:])
                            return ins
                        Tt(trq, [qn.b, ident_b.b], [pqT.b])
                        yield
                        Vv(lambda e: e.tensor_copy(out=qT[:].rearrange("p a t -> p (a t)"), in_=pqTv[:, 0:512]), [pqT.b], [qT.b])
                for f_ in pend_mix:
                    yield from f_()
                yield
                yield from trans()

            def stage2(T, gen):
                tl = T - t0
                qT = qTs[T % 3]
                st = stat[T % 2]
                kts = [kt for kt in range(5) if T - 4 + kt >= 0]
                po = [banks[6], banks[7]]
                pend = []
                for hh in range(8):
                    hp, hl = hh // 2, hh % 2
                    pr = slice(64 * hl, 64 * hl + 64)
                    psA = psum()
                    psD = psum()

                    def mms(e, hh=hh, hp=hp, pr=pr, psA=psA, psD=psD):
                        for kt in kts:
                            slot = (T - 4 + kt) % 8
                            dst = psD[:, 0:128] if kt == 4 else psA[:, kt * 128:(kt + 1) * 128]
                            ins = e.matmul(dst, lhsT=KT[pr, hp, slot * 128:(slot + 1) * 128], rhs=qT[pr, hp, :], start=True, stop=True)
                        return ins
                    Tt(mms, [qT.b] + [KTb[(T - 4 + kt) % 8] for kt in kts], [psA.b, psD.b])
                    gen()
                    PT = PTs[pt_i[0] % 4]
                    pt_i[0] += 1
                    ka = [kt for kt in kts if kt < 4]

                    def ex(e, psA=psA, psD=psD, PT=PT, ka=ka):
                        if ka:
                            e.activation(out=PT[:, ka[0]:4, :], in_=psA[:, ka[0] * 128:512].rearrange("p (k t) -> p k t", t=128), func=AF.Exp)
                        return e.activation(out=PT[:, 4, :], in_=psD[:, 0:128], func=AF.Exp)
                    Aa(ex, [psA.b, psD.b], [PT.b])
                    k0 = kts[0]
                    P.op("vector",
                         lambda e, PT=PT, hh=hh, k0=k0: e.tensor_tensor(out=PT[:, k0:5, :], in0=PT[:, k0:5, :], in1=biasT[:, hh, k0:5, :], op=ALU.mult),
                         [PT.b, biasT.b], [PT.b])
                    pob = po[hh // 4]

                    def mmo(e, hh=hh, PT=PT, pob=pob):
                        for i, kt in enumerate(kts):
                            slot = (T - 4 + kt) % 8
                            ins = e.matmul(pob[:, (hh % 4) * 65:(hh % 4) * 65 + 65], lhsT=PT[:, kt, :], rhs=Vx[:, slot, hh, :],
                                           start=(i == 0), stop=(i == len(kts) - 1))
                        return ins
                    pend.append((mmo, [PT.b] + [Vxb[(T - 4 + kt) % 8] for kt in kts], [pob.b]))
                    if len(pend) > 2:
                        Tt(*pend.pop(0))
                    gen()
                while pend:
                    Tt(*pend.pop(0))
                for half in range(2):
                    pob = po[half]
                    pv4 = pob[:, 0:260].rearrange("p (h d) -> p h d", d=65)
                    Vv(lambda e, pv4=pv4, half=half: e.reciprocal(out=st[:, 64 + half * 4: 68 + half * 4].unsqueeze(2), in_=pv4[:, :, 64:65]),
                       [pob.b], [st.sb("rs%d" % half)])
                    Vv(lambda e, pv4=pv4, half=half: e.tensor_tensor(out=oc[:, half * 4:(half + 1) * 4, :], in0=pv4[:, :, 0:64],
                                                                     in1=st[:, 64 + half * 4: 68 + half * 4].unsqueeze(2).to_broadcast([128, 4, 64]), op=ALU.mult),
                       [pob.b, st.sb("rs%d" % half)], [oc.b])
                Gg(lambda e: e.tensor_tensor(out=oc2[:], in0=oc[:], in1=oc[:], op=ALU.mult), [oc.b], [oc2.b])
                Vv(lambda e: e.tensor_reduce(out=st[:, 72:80], in_=oc2[:], axis=AX.X, op=ALU.add), [oc2.b], [st.sb("s4")])
                Aa(lambda e: e.activation(out=st[:, 80:88], in_=st[:, 72:80], func=AF.Ln, bias=eps_t[:], scale=1.0 / 64.0),
                   [st.sb("s4"), eps_t.b], [st.sb("ln4")])
                Aa(lambda e: e.activation(out=st[:, 80:88], in_=st[:, 80:88], func=AF.Exp, scale=-0.5), [st.sb("ln4")], [st.sb("r4")])
                Vv(lambda e: e.tensor_tensor(out=oc2[:], in0=oc[:], in1=st[:, 80:88].unsqueeze(2).to_broadcast([128, 8, 64]), op=ALU.mult),
                   [oc.b, st.sb("r4")], [oc2.b])
                Gg(lambda e: e.tensor_tensor(out=ocn[:], in0=oc2[:].rearrange("p h d -> p (h d)"), in1=g_oc[:], op=ALU.mult), [oc2.b, g_oc.b], [ocn.b])
                DMA("gpsimd", oac_d[tl * 128:(tl + 1) * 128, 256:768], ocn[:], [ocn.b], [oac_bufs[tl]], store=True)

            halo = [T for T in range(t0 - 4, t0) if T >= 0]
            for T in halo:
                for _ in stage1(T, False):
                    pass
            for T in (t0, t0 + 1):
                if T < t0 + NT:
                    for _ in stage1(T, True):
                        pass
            genq = []

            def adv():
                while genq:
                    try:
                        next(genq[0][1])
                        return
                    except StopIteration:
                        genq.pop(0)

            def drain_upto(Tmax):
                while genq and genq[0][0] <= Tmax:
                    for _ in genq[0][1]:
                        pass
                    genq.pop(0)

            for tl in range(NT):
                if tl % 4 == 2:
                    drain_upto(t0 + tl + 1)
                    ut_batch(tl - 2)
                if tl + 2 < NT:
                    genq.append((t0 + tl + 2, stage1(t0 + tl + 2, True)))
                stage2(t0 + tl, adv)
                drain_upto(t0 + tl + 1)
            drain_upto(t0 + NT)
            P.barrier()
            sba.release(mk_A)

            phase_s5(nc, P, sba, dr, l, seg, cfg, UT, UTb, obT, s5_state, banks, eps_t, ident_f, dbg_d, SEG, NCH)
            P.barrier()
            sba.release(mk_UT)

            phase_c(nc, P, sba, dr, l, seg, cfg, obT, oac_d, oac_bufs, x_in, x_out, xin_bufs, banks, eps_t, ident_f, ident_b,
                    dbg_d, SEG, NT, NB, NBT, t0)
            P.barrier()
            sba.release(mk_seg)

        for seg in range(NSEG):
            do_seg(l, seg, x_in, x_out)

    P.op("sync", None, reads=xin_bufs[L], writes=())
    if dbg:
        P.op("sync", None, reads=[dbg_d["_b"]], writes=())
    block = stack.enter_context(nc.Block())
    P.emit(block)
    stack.close()
    return nc, sba.peak, P.nops


def cmul(P, eng, o_re, o_im, a_re, a_im, b_re, b_im, t1, t2, rb, wb):
    def f(e):
        e.tensor_tensor(out=t1, in0=a_re, in1=b_re, op=ALU.mult)
        e.tensor_tensor(out=t2, in0=a_im, in1=b_im, op=ALU.mult)
        e.tensor_tensor(out=o_re, in0=t1, in1=t2, op=ALU.subtract)
        e.tensor_tensor(out=t1, in0=a_re, in1=b_im, op=ALU.mult)
        e.tensor_tensor(out=t2, in0=a_im, in1=b_re, op=ALU.mult)
        return e.tensor_tensor(out=o_im, in0=t1, in1=t2, op=ALU.add)
    P.op(eng, f, rb, wb)


def _mk(P):
    def Vv(fn, r, w):
        P.op("vector", fn, r, w)

    def Aa(fn, r, w):
        P.op("scalar", fn, r, w)

    def Gg(fn, r, w):
        P.op("gpsimd", fn, r, w)

    def Tt(fn, r, w):
        P.op("tensor", fn, r, w)

    def DMA(eng, out_ap, in_ap, r, w, store=False):
        P.op(eng, lambda e: e.dma_start(out=out_ap, in_=in_ap), r, w, dma=True, store=store)
    return Vv, Aa, Gg, Tt, DMA


def powers(P, sba, name, lre, lim, ldt, rb, n, nk, kvec, keep_unit=False):
    Vv, Aa, Gg, Tt, DMA = _mk(P)
    t = lambda nm, shp, dt=F32: sba.tile(name + nm, shp, dt)
    dt_ = t("dt", [128, n]); lm = t("lm", [128, n]); th = t("th", [128, n])
    Aa(lambda e: e.activation(out=dt_[:], in_=ldt, func=AF.Exp), rb, [dt_.b])
    Vv(lambda e: e.tensor_tensor(out=lm[:], in0=lre, in1=dt_[:], op=ALU.mult), rb + [dt_.b], [lm.b])
    Vv(lambda e: e.tensor_tensor(out=th[:], in0=lim, in1=dt_[:], op=ALU.mult), rb + [dt_.b], [th.b])
    Vv(lambda e: e.tensor_scalar(out=th[:], in0=th[:], scalar1=1.0 / TWO_PI, scalar2=None, op0=ALU.mult), [th.b], [th.b])
    kb = kvec[:, 0:nk, :].to_broadcast([128, nk, n])
    mag = t("mag", [128, nk, n]); y = t("y", [128, nk, n]); yi = t("yi", [128, nk, n], I32); yf = t("yf", [128, nk, n])
    sn = t("sn", [128, nk, n]); cs = t("cs", [128, nk, n])
    Vv(lambda e: e.tensor_tensor(out=mag[:], in0=kb, in1=lm[:].unsqueeze(1).to_broadcast([128, nk, n]), op=ALU.mult), [lm.b, kvec.b], [mag.b])
    Aa(lambda e: e.activation(out=mag[:], in_=mag[:], func=AF.Exp), [mag.b], [mag.b])
    Vv(lambda e: e.tensor_tensor(out=y[:], in0=kb, in1=th[:].unsqueeze(1).to_broadcast([128, nk, n]), op=ALU.mult), [th.b, kvec.b], [y.b])
    for (dst, shift) in ((sn, 0.0), (cs, 0.25)):
        if shift:
            Vv(lambda e: e.tensor_scalar(out=y[:], in0=y[:], scalar1=shift, scalar2=None, op0=ALU.add), [y.b], [y.b])
        Vv(lambda e: e.tensor_copy(out=yi[:], in_=y[:]), [y.b], [yi.b])
        Vv(lambda e: e.tensor_copy(out=yf[:], in_=yi[:]), [yi.b], [yf.b])
        Vv(lambda e: e.tensor_tensor(out=yf[:], in0=y[:], in1=yf[:], op=ALU.subtract), [y.b, yf.b], [yf.b])
        Aa(lambda e, dst=dst: e.activation(out=dst[:], in_=yf[:], func=AF.Sin, scale=TWO_PI), [yf.b], [dst.b])
    if keep_unit:
        ark = t("ark", [128, nk, n]); aik = t("aik", [128, nk, n])
    else:
        ark, aik = cs, sn
    Vv(lambda e: e.tensor_tensor(out=ark[:], in0=mag[:], in1=cs[:], op=ALU.mult), [mag.b, cs.b], [ark.b])
    Vv(lambda e: e.tensor_tensor(out=aik[:], in0=mag[:], in1=sn[:], op=ALU.mult), [mag.b, sn.b], [aik.b])
    nr = t("nr", [128, n]); den = t("den", [128, n]); t1 = t("t1", [128, n]); t2 = t("t2", [128, n])
    cr = t("cr", [128, n]); ci = t("ci", [128, n])
    Vv(lambda e: e.tensor_scalar(out=nr[:], in0=ark[:, 1, :], scalar1=-1.0, scalar2=None, op0=ALU.add), [ark.b], [nr.b])
    Vv(lambda e: e.tensor_tensor(out=t1[:], in0=lre, in1=lre, op=ALU.mult), rb, [t1.b])
    Vv(lambda e: e.tensor_tensor(out=t2[:], in0=lim, in1=lim, op=ALU.mult), rb, [t2.b])
    Vv(lambda e: e.tensor_tensor(out=den[:], in0=t1[:], in1=t2[:], op=ALU.add), [t1.b, t2.b], [den.b])
    Vv(lambda e: e.reciprocal(out=den[:], in_=den[:]), [den.b], [den.b])
    Vv(lambda e: e.tensor_tensor(out=t1[:], in0=nr[:], in1=lre, op=ALU.mult), rb + [nr.b, den.b], [t1.b])
    Vv(lambda e: e.tensor_tensor(out=t2[:], in0=aik[:, 1, :], in1=lim, op=ALU.mult), rb + [aik.b, den.b], [t2.b])
    Vv(lambda e: e.tensor_tensor(out=cr[:], in0=t1[:], in1=t2[:], op=ALU.add), [t1.b, t2.b], [cr.b])
    Vv(lambda e: e.tensor_tensor(out=cr[:], in0=cr[:], in1=den[:], op=ALU.mult), [cr.b, den.b], [cr.b])
    Vv(lambda e: e.tensor_tensor(out=t1[:], in0=aik[:, 1, :], in1=lre, op=ALU.mult), rb + [aik.b, cr.b], [t1.b])
    Vv(lambda e: e.tensor_tensor(out=t2[:], in0=nr[:], in1=lim, op=ALU.mult), rb + [nr.b, cr.b], [t2.b])
    Vv(lambda e: e.tensor_tensor(out=ci[:], in0=t1[:], in1=t2[:], op=ALU.subtract), [t1.b, t2.b], [ci.b])
    Vv(lambda e: e.tensor_tensor(out=ci[:], in0=ci[:], in1=den[:], op=ALU.mult), [ci.b, den.b], [ci.b])
    return ark, aik, mag, cs, sn, cr, ci


def cmul_ops(P, eng, o_re, o_im, a_re, a_im, b_re, b_im, t1, t2, rb, ob_re, ob_im, tb1, tb2, neg_im=False):
    def op(fn, r, w):
        P.op(eng, fn, r, w)
    op(lambda e: e.tensor_tensor(out=t1, in0=a_re, in1=b_re, op=ALU.mult), rb, [tb1])
    op(lambda e: e.tensor_tensor(out=t2, in0=a_im, in1=b_im, op=ALU.mult), rb, [tb2])
    op(lambda e: e.tensor_tensor(out=o_re, in0=t1, in1=t2, op=ALU.subtract), [tb1, tb2], [ob_re])
    op(lambda e: e.tensor_tensor(out=t1, in0=a_re, in1=b_im, op=ALU.mult), rb + [ob_re], [tb1])
    op(lambda e: e.tensor_tensor(out=t2, in0=a_im, in1=b_re, op=ALU.mult), rb + [ob_re], [tb2])
    if neg_im:
        op(lambda e: e.scalar_tensor_tensor(out=o_im, in0=t1, scalar=-1.0, in1=t2, op0=ALU.mult, op1=ALU.subtract), [tb1, tb2], [ob_im])
    else:
        op(lambda e: e.tensor_tensor(out=o_im, in0=t1, in1=t2, op=ALU.add), [tb1, tb2], [ob_im])


def phase_s5(nc, P, sba, dr, l, seg, cfg, UT, UTb, obT, s5_state, banks, eps_t, ident_f, dbg_d, SEG, NCH):
    Vv, Aa, Gg, Tt, DMA = _mk(P)
    bi = [0]

    def psum():
        t = banks[bi[0] % 8]
        bi[0] += 1
        return t
    ld = lambda name, shape, src, dt=F32, eng="sync": (lambda t: (DMA(eng, t[:], src, [], [t.b]), t)[1])(sba.tile(name, shape, dt))
    d_bc = ld("d_bc", [128, 256], dr["d_bc"][l])
    Em = ld("Em", [128, 4, 128], dr["c_E"])
    kvec = ld("kvec", [128, 9, 1], dr["c_kvec"])
    glu_b = ld("glu_b", [128, 2], dr["glu_b"][l])
    g_ob = ld("g_ob", [128, 2], dr["g_ob"][l])
    glu_w = ld("glu_w", [128, 2, 256], dr["glu_w"][l].rearrange("(ct p) c -> p ct c", p=128), BF16, "gpsimd")
    blk64 = ld("blk64", [128, 128], dr["c_blk64"], BF16, "gpsimd")
    if seg == 0:
        Vv(lambda e: e.memset(s5_state[:], 0.0), [], [s5_state.b])
    Xp = sba.tile("Xp", [128, 8, 2, NCH + 1], BF16)
    Xpb = [Buf("Xp%d" % i) for i in range(8)]
    BD = sba.tile("BD", [128, 4, 8, 128], BF16)
    CA = sba.tile("CA", [128, 8, 2, 8, 128], BF16)
    mk_L1 = sba.mark()
    if seg == 0:
        WSk = sba.tile("WSk", [128, 8, 2, 512], BF16, top=True)
        mk = sba.mark()
        lamB = ld("lamB", [128, 3, 512], dr["lamB"][l])
        bB = ld("bB", [128, 2, 512], dr["bB"][l])
        lre_c = sba.tile("lre_c", [128, 3, 256], F32)
        Vv(lambda e: e.tensor_copy(out=lre_c[:].rearrange("p w (q c) -> p w q c", q=4),
                                   in_=lamB[:].rearrange("p w (q g c) -> p w q g c", q=4, g=2)[:, :, :, 0, :]), [lamB.b], [lre_c.b])
        arB, aiB, magB, csB, snB, crB, ciB = powers(P, sba, "pB", lre_c[:, 0, :], lre_c[:, 1, :], lre_c[:, 2, :], [lre_c.b], 256, 8, kvec)
        cbr = sba.tile("cbr", [128, 4, 2, 64], F32)
        cbi = sba.tile("cbi", [128, 4, 2, 64], F32)
        wt1 = sba.tile("wt1", [128, 4, 2, 64], F32)
        wt2 = sba.tile("wt2", [128, 4, 2, 64], F32)
        b4 = lambda ap: ap.rearrange("p (q g c) -> p q g c", q=4, g=2)
        c4 = lambda ap: ap.rearrange("p (q c) -> p q c", q=4).unsqueeze(2).to_broadcast([128, 4, 2, 64])
        cmul_ops(P, "vector", cbr[:], cbi[:], b4(bB[:, 0, :]), b4(bB[:, 1, :]), c4(crB[:]), c4(ciB[:]), wt1[:], wt2[:],
                 [bB.b, crB.b, ciB.b], cbr.b, cbi.b, wt1.b, wt2.b)
        wt3 = sba.tile("wt3", [128, 4, 2, 64], F32)
        wt4 = sba.tile("wt4", [128, 4, 2, 64], F32)
        for k in range(8):
            ev = (k % 2 == 0)
            cmul_ops(P, "vector" if ev else "gpsimd", b4(WSk[:, k, 0, :]), b4(WSk[:, k, 1, :]), cbr[:], cbi[:], c4(arB[:, k, :]), c4(aiB[:, k, :]),
                     (wt1 if ev else wt3)[:], (wt2 if ev else wt4)[:], [cbr.b, cbi.b, arB.b, aiB.b], WSk.sb(("r", k)), WSk.sb(("i", k)),
                     (wt1 if ev else wt3).b, (wt2 if ev else wt4).b)
        WSb = [WSk.sb((r, k)) for r in ("r", "i") for k in range(8)]
        P.barrier()
        sba.release(mk)
        Tc = sba.tile("Tc", [128, 8, NCH], F32, top=True)
        Ts = sba.tile("Ts", [128, 8, NCH], F32, top=True)
        rho8 = sba.tile("rho8", [128, 8], F32, top=True)
        mk = sba.mark()
        lamA = ld("lamA", [128, 3, 8], dr["lamA"][l])
        cA = ld("cA", [128, 2, 8, 128], dr["cA"][l])
        bZ = ld("bZ", [128, 2, 8, 64], dr["bZ"][l])
        arA, aiA, magA, csA, snA, crA, ciA = powers(P, sba, "pA", lamA[:, 0, :], lamA[:, 1, :], lamA[:, 2, :], [lamA.b], 8, 9, kvec, keep_unit=True)
        Vv(lambda e: e.tensor_copy(out=rho8[:], in_=magA[:, 8, :]), [magA.b], [rho8.b])
        Vv(lambda e: e.tensor_copy(out=Tc[:, :, 0:1], in_=csA[:, 8, :].unsqueeze(2)), [csA.b], [Tc.b])
        Vv(lambda e: e.tensor_copy(out=Ts[:, :, 0:1], in_=snA[:, 8, :].unsqueeze(2)), [snA.b], [Ts.b])
        mk2 = sba.mark()
        tt1 = sba.tile("tt1", [128, 8, NCH // 2], F32)
        tt2 = sba.tile("tt2", [128, 8, NCH // 2], F32)
        n = 1
        while n < NCH:
            mr = Tc[:, :, n - 1:n].to_broadcast([128, 8, n])
            mi = Ts[:, :, n - 1:n].to_broadcast([128, 8, n])
            cmul_ops(P, "vector", Tc[:, :, n:2 * n], Ts[:, :, n:2 * n], Tc[:, :, 0:n], Ts[:, :, 0:n], mr, mi,
                     tt1[:, :, 0:n], tt2[:, :, 0:n], [Tc.b, Ts.b], Tc.b, Ts.b, tt1.b, tt2.b)
            n *= 2
        P.barrier()
        sba.release(mk2)
        ncim = sba.tile("ncim", [128, 8, 128], F32)
        Vv(lambda e: e.tensor_scalar(out=ncim[:], in0=cA[:, 1, :, :], scalar1=-1.0, scalar2=None, op0=ALU.mult), [cA.b], [ncim.b])
        ct1 = sba.tile("ct1", [128, 8, 128], F32)
        ct2 = sba.tile("ct2", [128, 8, 128], F32)
        for i in range(8):
            ar = arA[:, i + 1, :].unsqueeze(2).to_broadcast([128, 8, 128])
            ai = aiA[:, i + 1, :].unsqueeze(2).to_broadcast([128, 8, 128])
            cmul_ops(P, "vector", CA[:, i, 0, :, :], CA[:, i, 1, :, :], cA[:, 0, :, :], cA[:, 1, :, :], ar, ai,
                     ct1[:], ct2[:], [cA.b, arA.b, aiA.b], CA.sb(("r", i)), CA.sb(("i", i)), ct1.b, ct2.b, neg_im=True)
        CAb = [CA.sb((r, i)) for r in ("r", "i") for i in range(8)]
        cbZr = sba.tile("cbZr", [128, 8, 64], F32)
        cbZi = sba.tile("cbZi", [128, 8, 64], F32)
        zt1a = ct1[:, :, 0:64]
        zt2a = ct2[:, :, 0:64]
        cmul_ops(P, "vector", cbZr[:], cbZi[:], bZ[:, 0, :, :], bZ[:, 1, :, :], crA[:].unsqueeze(2).to_broadcast([128, 8, 64]),
                 ciA[:].unsqueeze(2).to_broadcast([128, 8, 64]), zt1a, zt2a, [bZ.b, crA.b, ciA.b], cbZr.b, cbZi.b, ct1.b, ct2.b)
        Zr = sba.tile("Zr", [128, 4, 8, 64], F32)
        Zi = sba.tile("Zi", [128, 4, 8, 64], F32)
        Ed = sba.tile("Ed", [128, 4, 128], F32)
        for q in range(4):
            ctq = q // 2
            Vv(lambda e, q=q, ctq=ctq: e.tensor_tensor(out=Ed[:, q, :], in0=Em[:, q, :], in1=d_bc[:, ctq * 128:(ctq + 1) * 128], op=ALU.mult),
               [Em.b, d_bc.b], [Ed.sb(q)])
        for half in range(2):
            for ts_ in range(4):
                tau = half * 4 + ts_
                ar = arA[:, tau, :].unsqueeze(2).to_broadcast([128, 8, 64])
                ai = aiA[:, tau, :].unsqueeze(2).to_broadcast([128, 8, 64])
                cmul_ops(P, "vector", Zr[:, ts_, :, :], Zi[:, ts_, :, :], cbZr[:], cbZi[:], ar, ai, zt1a, zt2a,
                         [cbZr.b, cbZi.b, arA.b, aiA.b], Zr.sb(ts_), Zi.sb(ts_), ct1.b, ct2.b)
            Zb = [Zr.sb(t) for t in range(4)] + [Zi.sb(t) for t in range(4)]
            for q in range(4):
                ps = psum()

                def mmk(e, q=q, ps=ps):
                    for pl in range(2):
                        Pp = 2 * q + pl
                        for ts_ in range(4):
                            dst = ps[64 * pl:64 * pl + 64, ts_ * 128:(ts_ + 1) * 128]
                            e.matmul(dst, lhsT=Zr[:, ts_, Pp, :], rhs=cA[:, 0, Pp, :], start=True, stop=False)
                            ins = e.matmul(dst, lhsT=Zi[:, ts_, Pp, :], rhs=ncim[:, Pp, :], start=False, stop=True)
                    return ins
                Tt(mmk, Zb + [cA.b, ncim.b], [ps.b])
                if half == 0:
                    Vv(lambda e, q=q, ps=ps: e.tensor_tensor(out=BD[:, q, 0, :], in0=ps[:, 0:128], in1=Ed[:, q, :], op=ALU.add), [ps.b, Ed.sb(q)], [BD.sb((q, 0))])
                    Vv(lambda e, q=q, ps=ps: e.tensor_copy(out=BD[:, q, 1:4, :], in_=ps[:, 128:512].rearrange("p (t c) -> p t c", t=3)), [ps.b], [BD.sb((q, 1))])
                else:
                    Vv(lambda e, q=q, ps=ps: e.tensor_copy(out=BD[:, q, 4:8, :], in_=ps[:, 0:512].rearrange("p (t c) -> p t c", t=4)), [ps.b], [BD.sb((q, 2))])
        BDb = [BD.sb((q, k)) for q in range(4) for k in range(3)]
        P.barrier()
        sba.release(mk)

        cd = dr["_s5cache"]
        DMA("sync", cd["WSk"], WSk[:].rearrange("p a b c -> p (a b c)"), WSb, [cd["buf"]], store=True)
        DMA("sync", cd["BD"], BD[:].rearrange("p a b c -> p (a b c)"), BDb, [cd["buf"]], store=True)
        DMA("sync", cd["CA"], CA[:].rearrange("p a b c d -> p (a b c d)"), CAb, [cd["buf"]], store=True)
        DMA("sync", cd["Tc"], Tc[:].rearrange("p a b -> p (a b)"), [Tc.b], [cd["buf"]], store=True)
        DMA("sync", cd["Ts"], Ts[:].rearrange("p a b -> p (a b)"), [Ts.b], [cd["buf"]], store=True)
        DMA("sync", cd["rho8"], rho8[:], [rho8.b], [cd["buf"]], store=True)
    else:
        cd = dr["_s5cache"]
        WSk = sba.tile("WSk", [128, 8, 2, 512], BF16, top=True)
        Tc = sba.tile("Tc", [128, 8, NCH], F32, top=True)
        Ts = sba.tile("Ts", [128, 8, NCH], F32, top=True)
        rho8 = sba.tile("rho8", [128, 8], F32, top=True)
        DMA("sync", WSk[:].rearrange("p a b c -> p (a b c)"), cd["WSk"], [cd["buf"]], [WSk.b])
        DMA("sync", BD[:].rearrange("p a b c -> p (a b c)"), cd["BD"], [cd["buf"]], [BD.b])
        DMA("sync", CA[:].rearrange("p a b c d -> p (a b c d)"), cd["CA"], [cd["buf"]], [CA.b])
        DMA("sync", Tc[:].rearrange("p a b -> p (a b)"), cd["Tc"], [cd["buf"]], [Tc.b])
        DMA("sync", Ts[:].rearrange("p a b -> p (a b)"), cd["Ts"], [cd["buf"]], [Ts.b])
        DMA("sync", rho8[:], cd["rho8"], [cd["buf"]], [rho8.b])
        WSb, BDb, CAb = [WSk.b], [BD.b], [CA.b]

    tmp2 = [[sba.tile("s5t%d_%d" % (k, i), [128, NCH], F32) for i in range(8)] for k in range(2)]
    rhob2 = [sba.tile("rhob%d" % k, [128, NCH], F32) for k in range(2)]
    def do_pair(Pp):
        q, pl = Pp // 2, Pp % 2
        pr = slice(64 * pl, 64 * pl + 64)
        pS = [psum(), psum()]
        for ri in range(2):
            def mms(e, ri=ri, ps=pS[ri]):
                for j in range(8):
                    ins = e.matmul(ps[:, 0:NCH], lhsT=WSk[pr, 7 - j, ri, q * 128:(q + 1) * 128], rhs=UT[pr, q, j * NCH:(j + 1) * NCH], start=(j == 0), stop=(j == 7))
                return ins
            Tt(mms, WSb + UTb, [pS[ri].b])
        a, b_, c, d_, Rr, Ri, Wr, Wi = tmp2[Pp % 2]
        rhob = rhob2[Pp % 2]
        tc, ts = Tc[:, Pp, :], Ts[:, Pp, :]
        sr, si = pS[0][:, 0:NCH], pS[1][:, 0:NCH]
        Vv(lambda e: e.tensor_tensor(out=a[:], in0=sr, in1=tc, op=ALU.mult), [pS[0].b, Tc.b], [a.b])
        Vv(lambda e: e.tensor_tensor(out=b_[:], in0=si, in1=ts, op=ALU.mult), [pS[1].b, Ts.b], [b_.b])
        Vv(lambda e: e.tensor_tensor(out=c[:], in0=si, in1=tc, op=ALU.mult), [pS[1].b, Tc.b], [c.b])
        Vv(lambda e: e.tensor_tensor(out=d_[:], in0=sr, in1=ts, op=ALU.mult), [pS[0].b, Ts.b], [d_.b])
        Gg(lambda e: e.tensor_tensor(out=Rr[:], in0=a[:], in1=b_[:], op=ALU.add), [a.b, b_.b], [Rr.b])
        Gg(lambda e: e.tensor_tensor(out=Ri[:], in0=c[:], in1=d_[:], op=ALU.subtract), [c.b, d_.b], [Ri.b])
        Vv(lambda e: e.tensor_copy(out=rhob[:], in_=rho8[:, Pp:Pp + 1].to_broadcast([128, NCH])), [rho8.b], [rhob.b])
        Vv(lambda e: e.tensor_tensor_scan(out=Wr[:], data0=rhob[:], data1=Rr[:], initial=s5_state[:, Pp, 0:1], op0=ALU.mult, op1=ALU.add),
           [rhob.b, Rr.b, s5_state.b], [Wr.b])
        Vv(lambda e: e.tensor_tensor_scan(out=Wi[:], data0=rhob[:], data1=Ri[:], initial=s5_state[:, Pp, 1:2], op0=ALU.mult, op1=ALU.add),
           [rhob.b, Ri.b, s5_state.b], [Wi.b])
        Vv(lambda e: e.tensor_copy(out=Xp[:, Pp, :, 0:1], in_=s5_state[:, Pp, :].unsqueeze(2)), [s5_state.b], [Xpb[Pp]])
        Gg(lambda e: e.tensor_tensor(out=a[:], in0=Wr[:], in1=tc, op=ALU.mult), [Wr.b, Tc.b], [a.b])
        Gg(lambda e: e.tensor_tensor(out=b_[:], in0=Wi[:], in1=ts, op=ALU.mult), [Wi.b, Ts.b], [b_.b])
        Vv(lambda e: e.tensor_tensor(out=c[:], in0=Wr[:], in1=ts, op=ALU.mult), [Wr.b, Ts.b], [c.b])
        Vv(lambda e: e.tensor_tensor(out=d_[:], in0=Wi[:], in1=tc, op=ALU.mult), [Wi.b, Tc.b], [d_.b])
        Vv(lambda e: e.tensor_tensor(out=Rr[:], in0=a[:], in1=b_[:], op=ALU.subtract), [a.b, b_.b], [Rr.b])
        Vv(lambda e: e.tensor_tensor(out=Ri[:], in0=c[:], in1=d_[:], op=ALU.add), [c.b, d_.b], [Ri.b])
        Gg(lambda e: e.tensor_copy(out=Xp[:, Pp, 0, 1:NCH + 1], in_=Rr[:]), [Rr.b], [Xpb[Pp]])
        Gg(lambda e: e.tensor_copy(out=Xp[:, Pp, 1, 1:NCH + 1], in_=Ri[:]), [Ri.b], [Xpb[Pp]])
        Vv(lambda e: e.tensor_copy(out=s5_state[:, Pp, 0:1], in_=Rr[:, NCH - 1:NCH]), [Rr.b], [s5_state.b])
        Vv(lambda e: e.tensor_copy(out=s5_state[:, Pp, 1:2], in_=Ri[:, NCH - 1:NCH]), [Ri.b], [s5_state.b])
    for Pp in range(8):
        do_pair(Pp)
    P.barrier()
    sba.release(mk_L1)
    yg = sba.tile("yg", [128, 2, SEG], BF16)
    ygb = [Buf("yg%d" % c) for c in range(2)]
    for ct in range(2):
        for i in range(8):
            ps = psum()

            def mmy(e, ct=ct, i=i, ps=ps):
                first = True
                for q in (2 * ct, 2 * ct + 1):
                    for j in range(i + 1):
                        e.matmul(ps[:, 0:NCH], lhsT=BD[:, q, i - j, :], rhs=UT[:, q, j * NCH:(j + 1) * NCH], start=first, stop=False)
                        first = False
                for Pp in range(4 * ct, 4 * ct + 4):
                    for ri in range(2):
                        last = (Pp == 4 * ct + 3 and ri == 1)
                        ins = e.matmul(ps[:, 0:NCH], lhsT=CA[:, i, ri, Pp, :], rhs=Xp[:, Pp, ri, 0:NCH], start=False, stop=last)
                return ins
            Tt(mmy, BDb + CAb + UTb + Xpb, [ps.b])
            Aa(lambda e, ct=ct, i=i, ps=ps: e.activation(out=yg[:, ct, i:SEG:8], in_=ps[:, 0:NCH], func=AF.Gelu_apprx_tanh), [ps.b], [ygb[ct]])
    BW = min(512, SEG)
    sg2 = [sba.tile("sg%d" % k, [128, BW], F32) for k in range(2)]
    obf2 = [sba.tile("obf%d" % k, [128, BW], F32) for k in range(2)]
    sqb2 = [sba.tile("sqb%d" % k, [128, BW], BF16) for k in range(2)]
    rs2 = [sba.tile("rs_%d" % k, [128, BW], F32) for k in range(2)]
    dbt = sba.tile("dbt", [128, BW], F32) if dbg_d else None
    its = [(ct2, blk) for ct2 in range(2) for blk in range(SEG // BW)]
    nglu_b = sba.tile("nglu_b", [128, 2], F32)
    Vv(lambda e: e.tensor_scalar(out=nglu_b[:], in0=glu_b[:], scalar1=-1.0, scalar2=None, op0=ALU.mult), [glu_b.b], [nglu_b.b])
    ps2s = {}

    def gluA(n):
        ct2, blk = its[n]
        cs_ = slice(blk * BW, (blk + 1) * BW)
        sg, obf, sqb = sg2[n % 2], obf2[n % 2], sqb2[n % 2]
        ps = psum()

        def mmg(e, ct2=ct2, cs_=cs_, ps=ps):
            for ct in range(2):
                ins = e.matmul(ps[:, 0:BW], lhsT=glu_w[:, ct, ct2 * 128:(ct2 + 1) * 128], rhs=yg[:, ct, cs_], start=(ct == 0), stop=(ct == 1))
            return ins
        Tt(mmg, [glu_w.b] + ygb, [ps.b])
        Aa(lambda e: e.activation(out=sg[:], in_=ps[:, 0:BW], func=AF.Exp, bias=nglu_b[:, ct2:ct2 + 1], scale=-1.0), [ps.b, nglu_b.b], [sg.b])
        Gg(lambda e: e.tensor_scalar(out=sg[:], in0=sg[:], scalar1=1.0, scalar2=None, op0=ALU.add), [sg.b], [sg.b])
        Vv(lambda e: e.reciprocal(out=sg[:], in_=sg[:]), [sg.b], [sg.b])
        Vv(lambda e: e.tensor_tensor(out=obf[:], in0=yg[:, ct2, cs_], in1=sg[:], op=ALU.mult), [sg.b] + ygb, [obf.b])
        Gg(lambda e: e.tensor_tensor(out=sqb[:], in0=obf[:], in1=obf[:], op=ALU.mult), [obf.b], [sqb.b])
        ps2 = psum()
        ps2s[n] = ps2
        Tt(lambda e: e.matmul(ps2[:, 0:BW], lhsT=blk64[:], rhs=sqb[:], start=True, stop=True), [blk64.b, sqb.b], [ps2.b])

    def gluB(n):
        ct2, blk = its[n]
        cs_ = slice(blk * BW, (blk + 1) * BW)
        obf, rs_ = obf2[n % 2], rs2[n % 2]
        ps2 = ps2s[n]
        Aa(lambda e: e.activation(out=rs_[:], in_=ps2[:, 0:BW], func=AF.Ln, bias=eps_t[:], scale=1.0 / 64.0), [ps2.b, eps_t.b], [rs_.b])
        Aa(lambda e: e.activation(out=rs_[:], in_=rs_[:], func=AF.Exp, scale=-0.5), [rs_.b], [rs_.b])
        Vv(lambda e: e.scalar_tensor_tensor(out=obT[:, ct2, cs_], in0=obf[:], scalar=g_ob[:, ct2:ct2 + 1], in1=rs_[:], op0=ALU.mult, op1=ALU.mult),
           [obf.b, g_ob.b, rs_.b], [obT.b])
        if dbg_d:
            Vv(lambda e: e.tensor_copy(out=dbt[:], in_=obT[:, ct2, cs_]), [obT.b], [dbt.b])
            DMA("sync", dbg_d["ob"][:, ct2, cs_], dbt[:], [dbt.b], [dbg_d["_b"]], store=True)

    gluA(0)
    for n in range(len(its)):
        if n + 1 < len(its):
            gluA(n + 1)
        gluB(n)


_dummy_tiles = {}


def cbZ_dummy(sba, name):
    if name not in _dummy_tiles:
        _dummy_tiles[name] = sba.tile(name, [128, 4, 2, 64], F32)
    return _dummy_tiles[name]


def phase_c(nc, P, sba, dr, l, seg, cfg, obT, oac_d, oac_bufs, x_in, x_out, xin_bufs, banks, eps_t, ident_f, ident_b,
            dbg_d, SEG, NT, NB, NBT, t0):
    Vv, Aa, Gg, Tt, DMA = _mk(P)
    bi = [0]

    def psum():
        t = banks[4 + bi[0] % 4]
        bi[0] += 1
        return t
    bf_ = [0]

    def psum_f():
        t = banks[bf_[0] % 8]
        bf_[0] += 1
        return t
    bo = [0]

    def psum_o():
        t = (banks[(bo[0] % 2) * 2], banks[(bo[0] % 2) * 2 + 1])
        bo[0] += 1
        return t
    L_last = (x_out is not None)
    w_out = sba.tile("w_out", [128, 8, 1024], BF16)
    wov = dr["w_out"][l].rearrange("(kc p) c -> p kc c", p=128)
    for kc in range(0, 8, 2):
        DMA("gpsimd", w_out[:, kc:kc + 2, :], wov[:, kc:kc + 2, :], [], [w_out.sb(kc)])
    w_out_b = [w_out.sb(kc) for kc in range(0, 8, 2)]
    gTf = sba.tile("gTf", [128, 1024], F32)
    DMA("sync", gTf[:], dr["gT_ffn"][l].rearrange("p a b -> p (a b)"), [], [gTf.b])
    w_r = sba.tile("w_r", [128, 8, 20], F32)
    DMA("sync", w_r[:], dr["w_r"][l].rearrange("(dc p) c -> p dc c", p=128), [], [w_r.b])
    b_r = sba.tile("b_r", [128, 20], F32)
    DMA("sync", b_r[:], dr["b_r"][l], [], [b_r.b])
    wgu = [sba.tile("wgu%d" % i, [128, 4, 8, 512], BF16) for i in range(2)]
    wdn = [sba.tile("wdn%d" % i, [128, 4, 2, 1024], BF16) for i in range(2)]
    acc = sba.tile("acc", [128, NBT, 1024], F32)
    accb = [Buf("acc%d" % i) for i in range(NBT)]
    h2T = sba.tile("h2T", [128, 8, NB], BF16)
    h2Tb = [Buf("h2T%d" % i) for i in range(NBT)]
    lg = sba.tile("lg", [128, NBT, 20], F32)
    lgb = [Buf("lg%d" % i) for i in range(NBT)]
    gate = sba.tile("gate", [128, NBT, 16], F32)
    xts = [sba.tile("cxt%d" % i, [128, 1024], F32) for i in range(2)]
    oacs = [sba.tile("oact%d" % i, [128, 768], BF16) for i in range(1)]
    oT = sba.tile("oT", [128, 6, 128], BF16)
    st = [sba.tile("cst%d" % i, [128, 8], F32) for i in range(2)]
    h2Tfs = [sba.tile("h2Tf%d" % i, [128, 8, 128], F32) for i in range(2)]
    sl = [sba.tile("sl%d" % i, [128, 256], BF16) for i in range(2)]
    hid = [sba.tile("hid%d" % i, [128, 256], BF16) for i in range(2)]
    hidT = [sba.tile("hidT%d" % i, [128, 2, 128], BF16) for i in range(2)]
    rt = {k: sba.tile("rt_" + k, [128, NBT, n], F32) for k, n in
          (("m", 1), ("gs", 4), ("eg", 4), ("sg", 1), ("pg", 1), ("ohg", 4), ("tmp", 16), ("es", 4), ("m1", 1), ("d1", 4), ("oh1", 4),
           ("msk", 4), ("m2", 1), ("oh2", 4), ("e2", 1), ("w1", 1), ("w2", 1), ("gin", 4), ("gi2", 4))}

    wq = [0]

    def load_group(gidx):
        buf = gidx % 2
        g = gidx % 4
        for e_ in range(4):
            E = 4 * g + e_
            DMA("gpsimd", wgu[buf][:, e_, :, 0:256], dr["w_gate"][l, E].rearrange("(dc p) f -> p dc f", p=128), [], [wgu[buf].sb((e_, 0))])
            DMA("gpsimd", wgu[buf][:, e_, :, 256:512], dr["w_up"][l, E].rearrange("(dc p) f -> p dc f", p=128), [], [wgu[buf].sb((e_, 1))])
            DMA("gpsimd", wdn[buf][:, e_, :, :], dr["w_down"][l, E].rearrange("(fc p) c -> p fc c", p=128), [], [wdn[buf].sb(e_)])

    nblk = SEG // NB
    load_group(0)
    gctr = 0
    for blk in range(nblk):
        def tile_front(tb, blk=blk):
            tl = blk * NBT + tb
            T = t0 + tl
            xt = xts[tb % 2]
            oc_ = oacs[0]
            s_ = st[tb % 2]
            h2Tf = h2Tfs[tb % 2]
            DMA("sync", xt[:], x_in[T * 128:(T + 1) * 128, :], [xin_bufs[l][T]], [xt.b])
            DMA("sync", oc_[:], oac_d[tl * 128:(tl + 1) * 128, :], [oac_bufs[tl]], [oc_.b])
            pt = psum_f()
            ptv = pt[:].bitcast(BF16)

            def tr(e):
                for c in range(6):
                    ins = e.transpose(out=ptv[:, c * 128:(c + 1) * 128], in_=oc_[:, c * 128:(c + 1) * 128], identity=ident_b[:])
                return ins
            Tt(tr, [oc_.b, ident_b.b], [pt.b])
            yield
            Aa(lambda e: e.activation(out=oT[:].rearrange("p a b -> p (a b)"), in_=ptv[:, 0:768], func=AF.Copy), [pt.b], [oT.b])
            yield
            for half in range(2):
                ps = psum_f()

                def mmo(e, half=half, ps=ps):
                    for kc in range(8):
                        if kc < 2:
                            lh = oT[:, kc, :]
                        elif kc < 4:
                            lh = obT[:, kc - 2, tl * 128:(tl + 1) * 128]
                        else:
                            lh = oT[:, kc - 2, :]
                        ins = e.matmul(ps[:, 0:512], lhsT=lh, rhs=w_out[:, kc, half * 512:(half + 1) * 512], start=(kc == 0), stop=(kc == 7))
                    return ins
                Tt(mmo, [oT.b, obT.b] + w_out_b, [ps.b])
                yield
                Vv(lambda e, half=half, ps=ps: e.tensor_tensor(out=acc[:, tb, half * 512:(half + 1) * 512], in0=ps[:, 0:512], in1=xt[:, half * 512:(half + 1) * 512], op=ALU.add),
                   [ps.b, xt.b], [accb[tb]])
                yield
            if dbg_d:
                DMA("sync", dbg_d["xm"][T * 128:(T + 1) * 128, :], acc[:, tb, :], [accb[tb]], [dbg_d["_b"]], store=True)
            Aa(lambda e: e.activation(out=xt[:], in_=acc[:, tb, :], func=AF.Square, scale=1.0 / 32.0, accum_out=s_[:, 0:1]), [accb[tb]], [xt.b, s_.sb("a")])
            Aa(lambda e: e.activation(out=s_[:, 1:2], in_=s_[:, 0:1], func=AF.Ln, bias=eps_t[:], scale=1.0), [s_.sb("a"), eps_t.b], [s_.sb("b")])
            Aa(lambda e: e.activation(out=s_[:, 2:3], in_=s_[:, 1:2], func=AF.Exp, scale=-0.5), [s_.sb("b")], [s_.sb("c")])
            yield
            Vv(lambda e: e.tensor_scalar(out=xt[:], in0=acc[:, tb, :], scalar1=s_[:, 2:3], scalar2=None, op0=ALU.mult), [accb[tb], s_.sb("c")], [xt.b])
            yield
            pa, pb = psum_f(), psum_f()

            def trf(e):
                for dc in range(8):
                    pp = pa if dc < 4 else pb
                    ins = e.transpose(out=pp[:, (dc % 4) * 128:(dc % 4 + 1) * 128], in_=xt[:, dc * 128:(dc + 1) * 128], identity=ident_f[:])
                return ins
            Tt(trf, [xt.b, ident_f.b], [pa.b, pb.b])
            yield
            Vv(lambda e: e.tensor_tensor(out=h2Tf[:, 0:4, :].rearrange("p a b -> p (a b)"), in0=pa[:, 0:512], in1=gTf[:, 0:512], op=ALU.mult), [pa.b, gTf.b], [h2Tf.sb(0)])
            Vv(lambda e: e.tensor_tensor(out=h2Tf[:, 4:8, :].rearrange("p a b -> p (a b)"), in0=pb[:, 0:512], in1=gTf[:, 512:1024], op=ALU.mult), [pb.b, gTf.b], [h2Tf.sb(1)])
            yield
            Gg(lambda e: e.tensor_copy(out=h2T[:, :, tb * 128:(tb + 1) * 128], in_=h2Tf[:]), [h2Tf.sb(0), h2Tf.sb(1)], [h2Tb[tb]])
            pr_ = psum_f()

            def mmr(e):
                for dc in range(8):
                    ins = e.matmul(pr_[:, 0:20], lhsT=h2Tf[:, dc, :], rhs=w_r[:, dc, :], start=(dc == 0), stop=(dc == 7))
                return ins
            Tt(mmr, [h2Tf.sb(0), h2Tf.sb(1), w_r.b], [pr_.b])
            yield
            Vv(lambda e: e.tensor_tensor(out=lg[:, tb, :], in0=pr_[:, 0:20], in1=b_r[:], op=ALU.add), [pr_.b, b_r.b], [lgb[tb]])

        act_f = []
        nxt_tb = 0
        steps = {}
        while nxt_tb < NBT or act_f:
            if nxt_tb < NBT and (not act_f or (len(act_f) == 1 and steps[act_f[0][0]] >= 5)):
                act_f.append((nxt_tb, tile_front(nxt_tb)))
                steps[nxt_tb] = 0
                nxt_tb += 1
            for (tb_, g_) in list(act_f):
                try:
                    next(g_)
                    steps[tb_] += 1
                except StopIteration:
                    act_f.remove((tb_, g_))

        def R(fn, r, w, eng="vector"):
            P.op(eng, fn, [rt[k].b if isinstance(k, str) else k for k in r], [rt[k].b if isinstance(k, str) else k for k in w])
        g4 = lg[:, :, 0:4]
        el = lg[:, :, 4:20].rearrange("p n (g e) -> p n g e", g=4)
        bc = lambda k, n: rt[k][:].to_broadcast([128, NBT, n])
        R(lambda e: e.tensor_reduce(out=rt["m"][:], in_=g4, axis=AX.X, op=ALU.max), lgb, ["m"])
        R(lambda e: e.tensor_tensor(out=rt["gs"][:], in0=g4, in1=bc("m", 4), op=ALU.subtract), lgb + ["m"], ["gs"])
        R(lambda e: e.activation(out=rt["eg"][:], in_=rt["gs"][:], func=AF.Exp), ["gs"], ["eg"], "scalar")
        R(lambda e: e.tensor_reduce(out=rt["sg"][:], in_=rt["eg"][:], axis=AX.X, op=ALU.add), ["eg"], ["sg"])
        R(lambda e: e.reciprocal(out=rt["pg"][:], in_=rt["sg"][:]), ["sg"], ["pg"])
        R(lambda e: e.tensor_single_scalar(out=rt["ohg"][:], in_=rt["gs"][:], scalar=0.0, op=ALU.is_equal), ["gs"], ["ohg"])
        R(lambda e: e.tensor_tensor(out=rt["tmp"][:].rearrange("p n (g e) -> p n g e", g=4), in0=el,
                                    in1=rt["ohg"][:].unsqueeze(3).to_broadcast([128, NBT, 4, 4]), op=ALU.mult), lgb + ["ohg"], ["tmp"])
        R(lambda e: e.tensor_reduce(out=rt["es"][:], in_=rt["tmp"][:].rearrange("p n (g e) -> p n e g", g=4), axis=AX.X, op=ALU.add), ["tmp"], ["es"])
        R(lambda e: e.tensor_reduce(out=rt["m1"][:], in_=rt["es"][:], axis=AX.X, op=ALU.max), ["es"], ["m1"])
        R(lambda e: e.tensor_tensor(out=rt["d1"][:], in0=rt["es"][:], in1=bc("m1", 4), op=ALU.subtract), ["es", "m1"], ["d1"])
        R(lambda e: e.tensor_single_scalar(out=rt["oh1"][:], in_=rt["d1"][:], scalar=0.0, op=ALU.is_equal), ["d1"], ["oh1"])
        R(lambda e: e.scalar_tensor_tensor(out=rt["msk"][:], in0=rt["oh1"][:], scalar=-1e30, in1=rt["d1"][:], op0=ALU.mult, op1=ALU.add), ["oh1", "d1"], ["msk"])
        R(lambda e: e.tensor_reduce(out=rt["m2"][:], in_=rt["msk"][:], axis=AX.X, op=ALU.max), ["msk"], ["m2"])
        R(lambda e: e.tensor_tensor(out=rt["oh2"][:], in0=rt["msk"][:], in1=bc("m2", 4), op=ALU.is_equal), ["msk", "m2"], ["oh2"])
        R(lambda e: e.activation(out=rt["e2"][:], in_=rt["m2"][:], func=AF.Exp), ["m2"], ["e2"], "scalar")
        R(lambda e: e.tensor_scalar(out=rt["w1"][:], in0=rt["e2"][:], scalar1=1.0, scalar2=None, op0=ALU.add), ["e2"], ["w1"])
        R(lambda e: e.reciprocal(out=rt["w1"][:], in_=rt["w1"][:]), ["w1"], ["w1"])
        R(lambda e: e.tensor_tensor(out=rt["w2"][:], in0=rt["e2"][:], in1=rt["w1"][:], op=ALU.mult), ["e2", "w1"], ["w2"])
        R(lambda e: e.tensor_tensor(out=rt["gin"][:], in0=rt["oh1"][:], in1=bc("w1", 4), op=ALU.mult), ["oh1", "w1"], ["gin"])
        R(lambda e: e.tensor_tensor(out=rt["gi2"][:], in0=rt["oh2"][:], in1=bc("w2", 4), op=ALU.mult), ["oh2", "w2"], ["gi2"])
        R(lambda e: e.tensor_tensor(out=rt["gin"][:], in0=rt["gin"][:], in1=rt["gi2"][:], op=ALU.add), ["gin", "gi2"], ["gin"])
        R(lambda e: e.tensor_tensor(out=rt["gin"][:], in0=rt["gin"][:], in1=bc("pg", 4), op=ALU.mult), ["gin", "pg"], ["gin"])
        R(lambda e: e.tensor_tensor(out=gate[:].rearrange("p n (g e) -> p n g e", g=4), in0=rt["ohg"][:].unsqueeze(3).to_broadcast([128, NBT, 4, 4]),
                                    in1=rt["gin"][:].unsqueeze(2).to_broadcast([128, NBT, 4, 4]), op=ALU.mult), ["ohg", "gin"], [gate.b])

        for g in range(4):
            buf = gctr % 2
            last_load = (blk == nblk - 1 and g == 3)
            if not last_load:
                load_group(gctr + 1)
            wbs = [[wgu[buf].sb((e_, 0)), wgu[buf].sb((e_, 1)), wdn[buf].sb(e_)] for e_ in range(4)]
            units = [(tb, e_) for tb in range(NBT) for e_ in range(4)]
            ust = {}

            def stA(i, g=g, buf=buf, wbs=wbs, ust=ust):
                tb, e_ = units[i]
                E = 4 * g + e_
                k = i % 2
                if e_ == 0:
                    ust[("po", tb)] = psum_o()
                pg_ = psum()

                def mm1(e):
                    for dc in range(8):
                        ins = e.matmul(pg_[:, 0:512], lhsT=h2T[:, dc, tb * 128:(tb + 1) * 128], rhs=wgu[buf][:, e_, dc, :], start=(dc == 0), stop=(dc == 7))
                    return ins
                Tt(mm1, [h2Tb[tb]] + wbs[e_], [pg_.b])
                Aa(lambda e: e.activation(out=sl[k][:], in_=pg_[:, 0:256], func=AF.Silu), [pg_.b], [sl[k].b])
                Vv(lambda e: e.scalar_tensor_tensor(out=hid[k][:], in0=pg_[:, 256:512], scalar=gate[:, tb, E:E + 1], in1=sl[k][:],
                                                    op0=ALU.mult, op1=ALU.mult), [pg_.b, gate.b, sl[k].b], [hid[k].b])

            def stB1(i):
                k = i % 2
                pt = psum()
                ptv = pt[:].bitcast(BF16)

                def tr2(e):
                    for fc in range(2):
                        ins = e.transpose(out=ptv[:, fc * 128:(fc + 1) * 128], in_=hid[k][:, fc * 128:(fc + 1) * 128], identity=ident_b[:])
                    return ins
                Tt(tr2, [hid[k].b, ident_b.b], [pt.b])
                Aa(lambda e: e.activation(out=hidT[k][:].rearrange("p a b -> p (a b)"), in_=ptv[:, 0:256], func=AF.Copy), [pt.b], [hidT[k].b])

            def stB2(i, g=g, buf=buf, wbs=wbs, ust=ust, blk=blk):
                tb, e_ = units[i]
                k = i % 2
                po = ust[("po", tb)]

                def mm2(e):
                    for half in range(2):
                        for fc in range(2):
                            ins = e.matmul(po[half][:, 0:512], lhsT=hidT[k][:, fc, :], rhs=wdn[buf][:, e_, fc, half * 512:(half + 1) * 512],
                                           start=(e_ == 0 and fc == 0), stop=(e_ == 3 and fc == 1))
                    return ins
                Tt(mm2, [hidT[k].b] + wbs[e_], [po[0].b, po[1].b])
                if e_ == 3:
                    for half in range(2):
                        Vv(lambda e, half=half: e.tensor_tensor(out=acc[:, tb, half * 512:(half + 1) * 512], in0=acc[:, tb, half * 512:(half + 1) * 512],
                                                                in1=po[half][:, 0:512], op=ALU.add), [po[half].b, accb[tb]], [accb[tb]])
                    if g == 3:
                        T = t0 + blk * NBT + tb
                        DMA("sync", x_out[T * 128:(T + 1) * 128, :], acc[:, tb, :], [accb[tb]], [xin_bufs[l + 1][T]], store=True)

            stA(0)
            for i in range(len(units)):
                if i + 1 < len(units):
                    stA(i + 1)
                stB1(i)
                if i >= 1:
                    stB2(i - 1)
            stB2(len(units) - 1)
            gctr += 1


_CACHE = {}


def run(inputs, S, L, n_cores, debug=False, SEG=None):
    lay = host_layouts(inputs, L)
    shapes = {k: v.shape for k, v in lay.items()}
    shapes["x"] = (S, D)
    key = (S, L, n_cores, debug, SEG)
    if key not in _CACHE:
        _CACHE[key] = build(dict(S=S, L=L, debug=debug, SEG=SEG), shapes)
    nc, peak, nops = _CACHE[key]
    x = np.ascontiguousarray(inputs["x"], dtype=np.float32)
    in_maps = []
    for c in range(n_cores):
        m = dict(lay)
        m["x"] = np.ascontiguousarray(x[c])
        in_maps.append(m)
    res = run_bass_kernel_spmd(nc, in_maps, core_ids=list(range(n_cores)))
    return res.results


def kernel(**inputs):
    x = inputs["x"]
    B, S, _ = x.shape
    L = inputs["w_in"].shape[0]
    results = run(inputs, S, L, B)
    return np.stack([np.asarray(r["out"], dtype=np.float32) for r in results], axis=0)
```

```python
import math
from contextlib import ExitStack
import numpy as np
import concourse.bass as bass
import concourse.mybir as mybir
from concourse.bass_utils import run_bass_kernel_spmd

F32 = mybir.dt.float32
BF16 = mybir.dt.bfloat16
I32 = mybir.dt.int32
AF = mybir.ActivationFunctionType
ALU = mybir.AluOpType
AX = mybir.AxisListType

D = 1024
IN_COLS = 2304
EPS = 1e-6
NEG = -30000.0
TWO_PI = 2.0 * math.pi


class Sem:
    def __init__(self, h):
        self.h = h
        self.count = 0


class Buf:
    __slots__ = ("name", "w", "r", "sem")

    def __init__(self, name=""):
        self.name = name
        self.w = {}
        self.r = {}
        self.sem = None


ENGS = ["tensor", "vector", "scalar", "gpsimd", "sync"]


class Prog:
    def __init__(self, nc, stack, npool=94):
        self.nc = nc
        self.esem = {e: Sem(stack.enter_context(nc.semaphore("e_" + e))) for e in ENGS}
        nsw = 40
        self.pools = {"sw": [Sem(stack.enter_context(nc.semaphore("w%d" % i))) for i in range(nsw)],
                      "hw": [Sem(stack.enter_context(nc.semaphore("d%d" % i))) for i in range(npool - nsw)]}
        self.pool_i = {"sw": 0, "hw": 0}
        self.ops = {e: [] for e in ENGS}
        self.seen = {e: {} for e in ENGS}
        self.nops = 0

    def dsem(self, buf, eng):
        if buf.sem is None:
            k = "sw" if eng == "gpsimd" else "hw"
            assert self.pool_i[k] < len(self.pools[k]), "out of %s semaphores" % k
            buf.sem = self.pools[k][self.pool_i[k]]
            self.pool_i[k] += 1
        return buf.sem

    def op(self, eng, fn, reads=(), writes=(), dma=False, store=False):
        need = {}
        es = self.esem[eng]

        def add(d, skip_own):
            for sm, v in d.items():
                if skip_own and sm is es:
                    continue
                if need.get(sm, 0) < v:
                    need[sm] = v

        for b in reads:
            add(b.w, False)
        so = (eng == "tensor") and not dma
        for b in writes:
            if not store:
                add(b.w, so)
            add(b.r, so)
        waits = []
        seen = self.seen[eng]
        for sm, v in need.items():
            if seen.get(sm, 0) >= v:
                continue
            seen[sm] = v
            waits.append((sm, v))
        if fn is None:
            self.ops[eng].append((waits, None, None, 0))
            return
        if dma:
            sm = self.dsem(reads[0] if store else writes[0], eng)
            sm.count += 16
            tok = (sm, sm.count)
            inc = 16
        else:
            es.count += 1
            tok = (es, es.count)
            inc = 1
        for b in reads:
            if b.r.get(tok[0], 0) < tok[1]:
                b.r[tok[0]] = tok[1]
        for b in writes:
            if store:
                b.w[tok[0]] = tok[1]
            else:
                b.w = {tok[0]: tok[1]}
                b.r = {}
        self.ops[eng].append((waits, fn, tok[0], inc))
        self.nops += 1

    def barrier(self):
        for e in ENGS:
            waits = []
            seen = self.seen[e]
            for sm in list(self.esem.values()) + self.pools["sw"][: self.pool_i["sw"]] + self.pools["hw"][: self.pool_i["hw"]]:
                if sm.count == 0:
                    continue
                if seen.get(sm, 0) >= sm.count:
                    continue
                seen[sm] = sm.count
                waits.append((sm, sm.count))
            self.ops[e].append((waits, None, None, 0))

    def emit(self, block):
        for e in ENGS:
            ops = self.ops[e]

            def body(eng, ops=ops):
                for waits, fn, sm, inc in ops:
                    for (w, v) in waits:
                        eng.wait_ge(w.h, v)
                    if fn is not None:
                        fn(eng).then_inc(sm.h, inc)

            getattr(block, e)(body)


class Tile:
    def __init__(self, h, name):
        self.h = h
        self.b = Buf(name)
        self.sub = {}

    def __getitem__(self, k):
        return self.h[k]

    def sb(self, key):
        if key not in self.sub:
            self.sub[key] = Buf("%s/%s" % (self.b.name, key))
        return self.sub[key]


class SBAlloc:
    def __init__(self, nc):
        self.nc = nc
        self.top = 16640
        self.lim = 229376 - 64
        self.top2 = self.lim
        self.n = 0
        self.peak = 0
        self.P = None

    def tile(self, name, shape, dt, top=False):
        esz = {F32: 4, BF16: 2, I32: 4}[dt]
        nb = esz
        for s in shape[1:]:
            nb *= s
        if top:
            off = (self.top2 - nb) // 64 * 64
            self.top2 = off
        else:
            off = (self.top + 63) // 64 * 64
            self.top = off + nb
        self.peak = max(self.peak, self.top + (self.lim - self.top2))
        assert self.top <= self.top2, "SBUF overflow at %s: %d/%d" % (name, self.top, self.top2)
        self.n += 1
        h = self.nc.alloc_sbuf_tensor_at("%s_%d" % (name, self.n), list(shape), dt, offset=off)
        return Tile(h, name)

    def mark(self):
        return (self.top, dict(self.P.pool_i) if self.P else None, self.top2)

    def release(self, m):
        self.top = m[0]
        self.top2 = m[2]
        if self.P:
            self.P.pool_i = dict(m[1])


def _rep(a, n=128):
    return np.ascontiguousarray(np.broadcast_to(a[None], (n,) + a.shape)).astype(np.float32)


def host_layouts(inp, L):
    f = np.float32
    o = {}
    o["w_in"] = np.ascontiguousarray(inp["w_in"], dtype=f)
    ws5 = np.zeros((L, D, 4, 4, 32), f)
    ws5[:, :, :, :, :16] = inp["w_in"][:, :, 512:768].reshape(L, D, 4, 4, 16)
    o["w_s5"] = ws5.reshape(L, D, 512)
    gT = lambda g: np.ascontiguousarray(np.broadcast_to(g.reshape(L, 8, 128).transpose(0, 2, 1)[:, :, :, None], (L, 128, 8, 128))).astype(f)
    o["gT_mix"] = gT(inp["norm_mix"])
    o["gT_ffn"] = gT(inp["norm_ffn"])
    o["g_sgu"] = np.stack([_rep(inp["sgu_norm"][l]) for l in range(L)])
    o["wsT"] = np.ascontiguousarray(inp["sgu_w"].transpose(0, 3, 1, 2)).astype(f)
    o["bsT"] = np.ascontiguousarray(inp["sgu_b"].transpose(0, 2, 1)).astype(f)
    o["g_q"] = np.stack([_rep(np.tile(inp["q_norm"][l], 8)) for l in range(L)])
    o["g_k"] = np.stack([_rep(np.tile(inp["k_norm"][l], 8)) for l in range(L)])
    ki = np.arange(128)[:, None, None]
    kt = np.arange(5)[None, :, None]
    qi = np.arange(128)[None, None, :]
    idx = np.clip(128 * (4 - kt) + qi - ki, -256, 256) + 256
    o["biasT"] = np.ascontiguousarray(inp["rel_bias"][:, :, idx].transpose(0, 2, 1, 3, 4)).astype(f)
    o["g_oa"] = np.stack([_rep(inp["out_norm"][l, 0:256]) for l in range(L)])
    o["g_oc"] = np.stack([_rep(inp["out_norm"][l, 512:1024]) for l in range(L)])
    o["g_ob"] = np.ascontiguousarray(inp["out_norm"][:, 256:512].reshape(L, 2, 128).transpose(0, 2, 1)).astype(f)
    o["w_out"] = np.ascontiguousarray(inp["w_out"], dtype=f)
    wr = np.concatenate([inp["router_group_w"], inp["router_expert_w"].transpose(0, 2, 1, 3).reshape(L, D, 16)], axis=2)
    o["w_r"] = np.ascontiguousarray(wr).astype(f)
    br = np.concatenate([inp["router_group_b"], inp["router_expert_b"].reshape(L, 16)], axis=1)
    o["b_r"] = np.stack([_rep(br[l]) for l in range(L)])
    o["w_gate"] = np.ascontiguousarray(inp["w_gate"], dtype=f)
    o["w_up"] = np.ascontiguousarray(inp["w_up"], dtype=f)
    o["w_down"] = np.ascontiguousarray(inp["w_down"], dtype=f)
    lre, lim, ldt = inp["s5_lambda_re"], inp["s5_lambda_im"], inp["s5_log_dt"]
    bre, bim, cre, cim = inp["s5_b_re"], inp["s5_b_im"], inp["s5_c_re"], inp["s5_c_im"]
    def layA(v):
        return np.ascontiguousarray(v.reshape(L, 8, 2, 64).transpose(0, 2, 3, 1).reshape(L, 128, 8)).astype(f)
    o["lamA"] = np.stack([layA(lre), layA(lim), layA(np.broadcast_to(ldt[:, :, None], (L, 16, 64)))], axis=2)
    cA = np.zeros((L, 2, 2, 64, 8, 128), f)
    bZ = np.zeros((L, 2, 2, 64, 8, 64), f)
    for g in range(16):
        P_, gl2 = g // 2, g % 2
        c0 = 16 * (g % 8)
        cA[:, 0, gl2, :, P_, c0:c0 + 16] = cre[:, g].transpose(0, 2, 1)
        cA[:, 1, gl2, :, P_, c0:c0 + 16] = cim[:, g].transpose(0, 2, 1)
        bZ[:, 0, gl2, :, P_, 32 * gl2:32 * gl2 + 16] = bre[:, g]
        bZ[:, 1, gl2, :, P_, 32 * gl2:32 * gl2 + 16] = bim[:, g]
    o["cA"] = np.ascontiguousarray(cA.reshape(L, 2, 128, 8, 128).transpose(0, 2, 1, 3, 4))
    o["bZ"] = np.ascontiguousarray(bZ.reshape(L, 2, 128, 8, 64).transpose(0, 2, 1, 3, 4))
    lamB = np.zeros((L, 3, 4, 32, 4, 2, 64), f)
    bB = np.zeros((L, 2, 4, 32, 4, 2, 64), f)
    for g in range(16):
        q, gl = g // 4, g % 4
        lamB[:, 0, gl, :, q, :, :] = lre[:, g][:, None, None, :]
        lamB[:, 1, gl, :, q, :, :] = lim[:, g][:, None, None, :]
        lamB[:, 2, gl, :, q, :, :] = ldt[:, g][:, None, None, None]
        bB[:, 0, gl, :16, q, gl % 2, :] = bre[:, g].transpose(0, 2, 1)
        bB[:, 1, gl, :16, q, gl % 2, :] = bim[:, g].transpose(0, 2, 1)
    o["lamB"] = np.ascontiguousarray(lamB.reshape(L, 3, 128, 512).transpose(0, 2, 1, 3))
    o["bB"] = np.ascontiguousarray(bB.reshape(L, 2, 128, 512).transpose(0, 2, 1, 3))
    o["d_bc"] = np.stack([_rep(inp["s5_d"][l]) for l in range(L)])
    o["glu_w"] = np.ascontiguousarray(inp["s5_glu_w"], dtype=f)
    o["glu_b"] = np.ascontiguousarray(inp["s5_glu_b"].reshape(L, 2, 128).transpose(0, 2, 1)).astype(f)
    o["c_ident"] = np.eye(128, dtype=f)
    ch = np.arange(128) // 64
    o["c_maskT"] = (ch[:, None] <= ch[None, :]).astype(f)
    am = np.zeros((128, 2, 128), f)
    am[:64, 0, 64:] = NEG
    am[64:, 1, :64] = NEG
    o["c_amask"] = am
    E = np.zeros((128, 4, 128), f)
    for g in range(16):
        q, gl = g // 4, g % 4
        for h in range(16):
            E[32 * gl + h, q, 16 * (g % 8) + h] = 1.0
    o["c_E"] = E
    kv = np.zeros((128, 9, 1), f)
    kv[:, :, 0] = np.arange(9)[None, :]
    o["c_kvec"] = kv
    blk = np.arange(128) // 64
    o["c_blk64"] = (blk[:, None] == blk[None, :]).astype(f)
    return o


def build(cfg, shapes):
    S = cfg["S"]
    L = cfg["L"]
    SEG = cfg.get("SEG") or min(S, 4096)
    NSEG = S // SEG
    NT = SEG // 128
    NB = min(SEG, 1024)
    NBT = NB // 128
    NCH = SEG // 8
    dbg = cfg.get("debug")

    nc = bass.Bass("TRN2", target_bir_lowering=False)
    dr = {k: nc.dram_tensor(k, list(v), F32, kind="ExternalInput").ap() for k, v in shapes.items()}
    out_d = nc.dram_tensor("out", [S, D], F32, kind="ExternalOutput").ap()
    x1_d = nc.dram_tensor("x1s", [S, D], F32, kind="Internal").ap() if L > 1 else None
    oac_d = nc.dram_tensor("oac", [SEG, 768], BF16, kind=("ExternalOutput" if dbg else "Internal")).ap()
    dbg_d = {}
    if dbg:
        dbg_d["ob"] = nc.dram_tensor("dbg_ob", [128, 2, SEG], F32, kind="ExternalOutput").ap()
        dbg_d["xm"] = nc.dram_tensor("dbg_xm", [S, D], F32, kind="ExternalOutput").ap()

    NCH_ = SEG // 8
    dr["_s5cache"] = {
        "WSk": nc.dram_tensor("s5c_WSk", [128, 8 * 2 * 512], BF16, kind="Internal").ap(),
        "BD": nc.dram_tensor("s5c_BD", [128, 4 * 8 * 128], BF16, kind="Internal").ap(),
        "CA": nc.dram_tensor("s5c_CA", [128, 8 * 2 * 8 * 128], BF16, kind="Internal").ap(),
        "Tc": nc.dram_tensor("s5c_Tc", [128, 8 * NCH_], F32, kind="Internal").ap(),
        "Ts": nc.dram_tensor("s5c_Ts", [128, 8 * NCH_], F32, kind="Internal").ap(),
        "rho8": nc.dram_tensor("s5c_rho8", [128, 8], F32, kind="Internal").ap(),
        "buf": Buf("s5cache"),
    }
    stack = ExitStack()
    P = Prog(nc, stack)
    sba = SBAlloc(nc)
    sba.P = P
    banks = []
    for i in range(8):
        banks.append(Tile(nc.alloc_psum_tensor("bank%d" % i, [128, 512], F32), "bank%d" % i))
    bank_i = [0]

    def psum():
        t = banks[bank_i[0] % 6]
        bank_i[0] += 1
        return t

    def Vv(fn, r, w):
        P.op("vector", fn, r, w)

    def Aa(fn, r, w):
        P.op("scalar", fn, r, w)

    def Gg(fn, r, w):
        P.op("gpsimd", fn, r, w)

    def Tt(fn, r, w):
        P.op("tensor", fn, r, w)

    def DMA(eng, out_ap, in_ap, r, w, store=False):
        P.op(eng, lambda e: e.dma_start(out=out_ap, in_=in_ap), r, w, dma=True, store=store)

    xin_bufs = []
    for l_ in range(L + 1):
        grp = [Buf("x%d_%d" % (l_, t)) for t in range((S // 128 + 7) // 8)]
        xin_bufs.append([grp[t // 8] for t in range(S // 128)])
    ogrp = [Buf("oac%d" % t) for t in range((NT + 3) // 4)]
    oac_bufs = [ogrp[t // 4] for t in range(NT)]
    x_aps = [dr["x"]] + [x1_d] * (L - 1) + [out_d]
    cb = Buf("consts")

    ident_f = sba.tile("ident_f", [128, 128], F32)
    ident_b = sba.tile("ident_b", [128, 128], BF16)
    eps_t = sba.tile("eps", [128, 1], F32)
    DMA("sync", ident_f[:], dr["c_ident"], [], [ident_f.b])
    DMA("gpsimd", ident_b[:], dr["c_ident"], [], [ident_b.b])
    Vv(lambda e: e.memset(eps_t[:], EPS), [], [eps_t.b])
    s5_state = sba.tile("s5state", [128, 8, 2], F32)
    if dbg:
        dbg_d["_b"] = Buf("dbg")

    for l in range(L):
        x_in = x_aps[l]
        x_out = x_aps[l + 1]
        def do_seg(l, seg, x_in, x_out):
            t0 = seg * NT
            mk_seg = sba.mark()
            obT = sba.tile("obT", [128, 2, SEG], BF16)
            mk_UT = sba.mark()
            UT = sba.tile("UT", [128, 4, SEG], BF16)
            UTb = [Buf("UT%d" % t) for t in range(NT)]
            mk_A = sba.mark()
            w_in_sb = sba.tile("w_in", [128, 8, IN_COLS], BF16)
            w_s5_sb = sba.tile("w_s5", [128, 8, 512], BF16)
            w_in_v = dr["w_in"][l].rearrange("(dc p) c -> p dc c", p=128)
            w_s5_v = dr["w_s5"][l].rearrange("(dc p) c -> p dc c", p=128)
            for dc in range(0, 8, 2):
                DMA("gpsimd", w_in_sb[:, dc:dc + 2, :], w_in_v[:, dc:dc + 2, :], [], [w_in_sb.sb(dc)])
            DMA("gpsimd", w_s5_sb[:], w_s5_v, [], [w_s5_sb.b])
            w_in_bufs = [w_in_sb.sb(dc) for dc in range(0, 8, 2)]
            gT = sba.tile("gT", [128, 1024], F32)
            DMA("sync", gT[:], dr["gT_mix"][l].rearrange("p a b -> p (a b)"), [], [gT.b])
            g_sgu = sba.tile("g_sgu", [128, 256], F32)
            DMA("sync", g_sgu[:], dr["g_sgu"][l], [], [g_sgu.b])
            wsT_f = sba.tile("wsT_f", [128, 4, 128], F32)
            DMA("sync", wsT_f[:], dr["wsT"][l], [], [wsT_f.b])
            maskT = sba.tile("maskT", [128, 128], F32)
            DMA("sync", maskT[:], dr["c_maskT"], [], [maskT.b])
            wsT_b = sba.tile("wsT_b", [128, 4, 128], BF16)
            Vv(lambda e: e.tensor_tensor(out=wsT_b[:], in0=wsT_f[:], in1=maskT[:].unsqueeze(1).to_broadcast([128, 4, 128]), op=ALU.mult),
               [wsT_f.b, maskT.b], [wsT_b.b])
            bsT = sba.tile("bsT", [128, 4], F32)
            DMA("sync", bsT[:], dr["bsT"][l], [], [bsT.b])
            g_q = sba.tile("g_q", [128, 512], F32)
            g_k = sba.tile("g_k", [128, 512], F32)
            g_oa = sba.tile("g_oa", [128, 256], F32)
            g_oc = sba.tile("g_oc", [128, 512], F32)
            DMA("sync", g_q[:], dr["g_q"][l], [], [g_q.b])
            DMA("sync", g_k[:], dr["g_k"][l], [], [g_k.b])
            DMA("sync", g_oa[:], dr["g_oa"][l], [], [g_oa.b])
            DMA("sync", g_oc[:], dr["g_oc"][l], [], [g_oc.b])
            biasT = sba.tile("biasT", [128, 8, 5, 128], BF16)
            mk_tmp = sba.mark()
            biasT_f = sba.tile("biasT_f", [128, 8, 5, 128], F32)
            amask = sba.tile("amask", [128, 2, 128], F32)
            DMA("sync", biasT_f[:], dr["biasT"][l], [], [biasT_f.b])
            DMA("sync", amask[:], dr["c_amask"], [], [amask.b])
            Vv(lambda e: e.tensor_tensor(out=biasT_f[:, :, 0, :], in0=biasT_f[:, :, 0, :], in1=amask[:, 0:1, :].to_broadcast([128, 8, 128]), op=ALU.add),
               [biasT_f.b, amask.b], [biasT_f.b])
            Vv(lambda e: e.tensor_tensor(out=biasT_f[:, :, 4, :], in0=biasT_f[:, :, 4, :], in1=amask[:, 1:2, :].to_broadcast([128, 8, 128]), op=ALU.add),
               [biasT_f.b, amask.b], [biasT_f.b])
            Aa(lambda e: e.activation(out=biasT[:], in_=biasT_f[:], func=AF.Exp), [biasT_f.b], [biasT.b])
            P.barrier()
            sba.release(mk_tmp)
            KT = sba.tile("KT", [128, 4, 8 * 128], BF16)
            KTb = [Buf("KT%d" % s) for s in range(8)]
            Vx = sba.tile("Vx", [128, 8, 8, 65], BF16)
            Vxb = [Buf("Vx%d" % s) for s in range(8)]
            Vv(lambda e: e.memset(Vx[:], 1.0), [], [Vx.b] + Vxb)
            xts = [sba.tile("xt%d" % i, [128, 1024], F32) for i in range(2)]
            junk = sba.tile("junk", [128, 1024], BF16)
            stat = [sba.tile("stat%d" % i, [128, 96], F32) for i in range(2)]
            for st_ in stat:
                Vv(lambda e, st_=st_: e.memset(st_[:], 1.0), [], [st_.b] + [st_.sb(k_) for k_ in ("ssq", "ln", "rstd", "s2", "ln2", "r2", "s3", "ln3", "r3", "rs0", "rs1", "s4", "ln4", "r4")])
            hb = sba.tile("hb", [128, 1024], BF16)
            hTr = sba.tile("hTr", [128, 8, 512], BF16)
            hTb = [Buf("hT%d" % i) for i in range(4)]
            gz = sba.tile("gz", [128, 512], F32)
            vn = sba.tile("vn", [128, 256], BF16)
            oa = sba.tile("oa", [128, 256], F32)
            oa2 = sba.tile("oa2", [128, 256], F32)
            oan = sba.tile("oan", [128, 256], BF16)
            qraw = [sba.tile("qraw%d" % i, [128, 512], F32) for i in range(2)]
            kraw = [sba.tile("kraw%d" % i, [128, 512], F32) for i in range(2)]
            sq = sba.tile("sq", [128, 512], F32)
            sq2 = sba.tile("sq2", [128, 512], F32)
            qn = sba.tile("qn", [128, 512], BF16)
            kn = sba.tile("kn", [128, 512], BF16)
            qTs = [sba.tile("qT%d" % i, [128, 4, 128], BF16) for i in range(3)]
            PTs = [sba.tile("PT%d" % i, [128, 5, 128], BF16) for i in range(4)]
            oc = sba.tile("oc", [128, 8, 64], F32)
            oc2 = sba.tile("oc2", [128, 8, 64], F32)
            ocn = sba.tile("ocn", [128, 512], BF16)
            pt_i = [0]

            def ut_batch(tl0):
                for q in range(4):
                    pu = psum()

                    def mmu(e, q=q, pu=pu):
                        for dc in range(8):
                            ins = e.matmul(pu[:, 0:512], lhsT=w_s5_sb[:, dc, q * 128:(q + 1) * 128], rhs=hTr[:, dc, :], start=(dc == 0), stop=(dc == 7))
                        return ins
                    Tt(mmu, hTb + [w_s5_sb.b], [pu.b])
                    P.op("vector" if q % 2 == 0 else "scalar",
                         (lambda e, q=q, pu=pu: e.tensor_copy(out=UT[:, q, :].rearrange("p (j n) -> p j n", j=8)[:, :, tl0 * 16:tl0 * 16 + 64], in_=pu[:, 0:512].rearrange("p (n j) -> p j n", j=8))) if q % 2 == 0 else
                         (lambda e, q=q, pu=pu: e.activation(out=UT[:, q, :].rearrange("p (j n) -> p j n", j=8)[:, :, tl0 * 16:tl0 * 16 + 64], in_=pu[:, 0:512].rearrange("p (n j) -> p j n", j=8), func=AF.Copy)),
                         [pu.b], [UTb[tl0 + q]])

            def stage1(T, full):
                tl = T - t0
                pend_mix = []
                xt = xts[T % 2]
                st = stat[T % 2]
                hs = T % 4
                slot = T % 8
                DMA("sync", xt[:], x_in[T * 128:(T + 1) * 128, :], [xin_bufs[l][T]], [xt.b])
                Aa(lambda e: e.activation(out=junk[:], in_=xt[:], func=AF.Square, scale=1.0 / 32.0, accum_out=st[:, 0:1]),
                   [xt.b], [junk.b, st.sb("ssq")])
                Aa(lambda e: e.activation(out=st[:, 1:2], in_=st[:, 0:1], func=AF.Ln, bias=eps_t[:], scale=1.0),
                   [st.sb("ssq"), eps_t.b], [st.sb("ln")])
                Aa(lambda e: e.activation(out=st[:, 2:3], in_=st[:, 1:2], func=AF.Exp, scale=-0.5),
                   [st.sb("ln")], [st.sb("rstd")])
                Vv(lambda e: e.tensor_scalar(out=hb[:], in0=xt[:], scalar1=st[:, 2:3], scalar2=None, op0=ALU.mult),
                   [xt.b, st.sb("rstd")], [hb.b])
                yield
                pT = psum()
                pTv = pT[:].bitcast(BF16)

                def tr(e):
                    for dc in range(8):
                        ins = e.transpose(out=pTv[:, dc * 128:(dc + 1) * 128], in_=hb[:, dc * 128:(dc + 1) * 128], identity=ident_b[:])
                    return ins
                Tt(tr, [hb.b, ident_b.b], [pT.b])
                yield
                Vv(lambda e: e.tensor_tensor(out=hTr[:, :, hs * 128:(hs + 1) * 128], in0=pTv[:, 0:1024].rearrange("p (a b) -> p a b", a=8),
                                             in1=gT[:].rearrange("p (a b) -> p a b", a=8), op=ALU.mult),
                   [pT.b, gT.b], [hTb[hs]])
                yield

                def proj(c0, n=512):
                    ps = psum()

                    def mm(e):
                        for dc in range(8):
                            ins = e.matmul(ps[:, 0:n], lhsT=hTr[:, dc, hs * 128:(hs + 1) * 128], rhs=w_in_sb[:, dc, c0:c0 + n], start=(dc == 0), stop=(dc == 7))
                        return ins
                    Tt(mm, [hTb[hs]] + w_in_bufs, [ps.b])
                    return ps

                if full:
                    pz = proj(0)
                    yield
                    Aa(lambda e: e.activation(out=gz[:], in_=pz[:, 0:512], func=AF.Gelu_apprx_tanh), [pz.b], [gz.b])
                    yield
                    pq = proj(768)
                    yield
                    qr = qraw[T % 2]
                    Aa(lambda e: e.activation(out=qr[:], in_=pq[:, 0:512], func=AF.Copy), [pq.b], [qr.b])
                    yield
                pk = proj(1280)
                yield
                kr = kraw[T % 2]
                Aa(lambda e: e.activation(out=kr[:], in_=pk[:, 0:512], func=AF.Copy), [pk.b], [kr.b])
                yield
                pv = proj(1792)
                yield
                Aa(lambda e: e.activation(out=Vx[:, slot, :, 0:64], in_=pv[:, 0:512].rearrange("p (h d) -> p h d", h=8), func=AF.Copy),
                   [pv.b], [Vxb[slot]])
                yield
                rd = []
                if full:
                    Aa(lambda e: e.activation(out=junk[:, 0:256], in_=gz[:, 256:512], func=AF.Square, scale=1.0 / 16.0, accum_out=st[:, 8:9]),
                       [gz.b], [junk.b, st.sb("s2")])
                    Gg(lambda e: e.tensor_tensor(out=sq[:], in0=qr[:], in1=qr[:], op=ALU.mult), [qr.b], [sq.b])
                    Vv(lambda e: e.tensor_reduce(out=st[:, 16:24], in_=sq[:].rearrange("p (h d) -> p h d", h=8), axis=AX.X, op=ALU.add),
                       [sq.b], [st.sb("s2")])
                Gg(lambda e: e.tensor_tensor(out=sq2[:], in0=kr[:], in1=kr[:], op=ALU.mult), [kr.b], [sq2.b])
                Vv(lambda e: e.tensor_reduce(out=st[:, 24:32], in_=sq2[:].rearrange("p (h d) -> p h d", h=8), axis=AX.X, op=ALU.add),
                   [sq2.b], [st.sb("s2")])
                if not full:
                    Vv(lambda e: e.memset(st[:, 8:24], 1.0), [], [st.sb("s2")])
                if full:
                    Vv(lambda e: e.tensor_scalar(out=st[:, 8:9], in0=st[:, 8:9], scalar1=64.0, scalar2=None, op0=ALU.mult),
                       [st.sb("s2")], [st.sb("s2")])
                Aa(lambda e: e.activation(out=st[:, 32:56], in_=st[:, 8:32], func=AF.Ln, bias=eps_t[:], scale=1.0 / 64.0),
                   [st.sb("s2"), eps_t.b], [st.sb("ln2")])
                Aa(lambda e: e.activation(out=st[:, 32:56], in_=st[:, 32:56], func=AF.Exp, scale=-0.5),
                   [st.sb("ln2")], [st.sb("r2")])
                if full:
                    Vv(lambda e: e.scalar_tensor_tensor(out=vn[:], in0=gz[:, 256:512], scalar=st[:, 32:33], in1=g_sgu[:], op0=ALU.mult, op1=ALU.mult),
                       [gz.b, st.sb("r2"), g_sgu.b], [vn.b])
                    def sgu_mix():
                        pm = psum()

                        def mmx(e):
                            for hh in range(4):
                                ins = e.matmul(pm[:, hh * 64:(hh + 1) * 64], lhsT=wsT_b[:, hh, :], rhs=vn[:, hh * 64:(hh + 1) * 64], start=True, stop=True)
                            return ins
                        Tt(mmx, [wsT_b.b, vn.b], [pm.b])
                        yield

                        def sgu_out(e):
                            for hh in range(4):
                                ins = e.scalar_tensor_tensor(out=oa[:, hh * 64:(hh + 1) * 64], in0=pm[:, hh * 64:(hh + 1) * 64], scalar=bsT[:, hh:hh + 1],
                                                             in1=gz[:, hh * 64:(hh + 1) * 64], op0=ALU.add, op1=ALU.mult)
                            return ins
                        Vv(sgu_out, [pm.b, bsT.b, gz.b], [oa.b])
                        Gg(lambda e: e.tensor_tensor(out=oa2[:], in0=oa[:], in1=oa[:], op=ALU.mult), [oa.b], [oa2.b])
                        Vv(lambda e: e.tensor_reduce(out=st[:, 56:60], in_=oa2[:].rearrange("p (h d) -> p h d", h=4), axis=AX.X, op=ALU.add),
                           [oa2.b], [st.sb("s3")])
                        Aa(lambda e: e.activation(out=st[:, 60:64], in_=st[:, 56:60], func=AF.Ln, bias=eps_t[:], scale=1.0 / 64.0),
                           [st.sb("s3"), eps_t.b], [st.sb("ln3")])
                        Aa(lambda e: e.activation(out=st[:, 60:64], in_=st[:, 60:64], func=AF.Exp, scale=-0.5), [st.sb("ln3")], [st.sb("r3")])
                        Vv(lambda e: e.tensor_tensor(out=oa2[:].rearrange("p (h d) -> p h d", h=4), in0=oa[:].rearrange("p (h d) -> p h d", h=4),
                                                     in1=st[:, 60:64].unsqueeze(2).to_broadcast([128, 4, 64]), op=ALU.mult),
                           [oa.b, st.sb("r3")], [oa2.b])
                        Gg(lambda e: e.tensor_tensor(out=oan[:], in0=oa2[:], in1=g_oa[:], op=ALU.mult), [oa2.b, g_oa.b], [oan.b])
                        DMA("gpsimd", oac_d[tl * 128:(tl + 1) * 128, 0:256], oan[:], [oan.b], [oac_bufs[tl]], store=True)
                    pend_mix.append(sgu_mix)
                    Vv(lambda e: e.tensor_scalar(out=st[:, 40:48], in0=st[:, 40:48], scalar1=0.125, scalar2=None, op0=ALU.mult),
                       [st.sb("r2")], [st.sb("r2")])
                    Vv(lambda e: e.tensor_tensor(out=sq[:].rearrange("p (h d) -> p h d", h=8), in0=qr[:].rearrange("p (h d) -> p h d", h=8),
                                                 in1=st[:, 40:48].unsqueeze(2).to_broadcast([128, 8, 64]), op=ALU.mult),
                       [qr.b, st.sb("r2")], [sq.b])
                    Gg(lambda e: e.tensor_tensor(out=qn[:], in0=sq[:], in1=g_q[:], op=ALU.mult), [sq.b, g_q.b], [qn.b])
                Vv(lambda e: e.tensor_tensor(out=sq2[:].rearrange("p (h d) -> p h d", h=8), in0=kr[:].rearrange("p (h d) -> p h d", h=8),
                                             in1=st[:, 48:56].unsqueeze(2).to_broadcast([128, 8, 64]), op=ALU.mult),
                   [kr.b, st.sb("r2")], [sq2.b])
                Gg(lambda e: e.tensor_tensor(out=kn[:], in0=sq2[:], in1=g_k[:], op=ALU.mult), [sq2.b, g_k.b], [kn.b])
                yield
                def trans():
                    pkT = psum()
                    pkTv = pkT[:].bitcast(BF16)

                    def trk(e):
                        for hp in range(4):
                            ins = e.transpose(out=pkTv[:, hp * 128:(hp + 1) * 128], in_=kn[:, hp * 128:(hp + 1) * 128], identity=ident_b[:])
                        return ins
                    Tt(trk, [kn.b, ident_b.b], [pkT.b])
                    yield
                    Aa(lambda e: e.activation(out=KT[:, :, slot * 128:(slot + 1) * 128], in_=pkTv[:, 0:512].rearrange("p (a t) -> p a t", a=4), func=AF.Copy),
                       [pkT.b], [KTb[slot]])
                    if full:
                        qT = qTs[T % 3]
                        pqT = psum()
                        pqTv = pqT[:].bitcast(BF16)

                        def trq(e):
                            for hp in range(4):
                                ins = e.transpose(out=pqTv[:, hp * 128:(hp + 1) * 128], in_=qn[:, hp * 128:(hp + 1) * 128], identity=ident_b[:])
                            return ins
                        Tt(trq, [qn.b, ident_b.b], [pqT.b])
                        yield
                        Vv(lambda e: e.tensor_copy(out=qT[:].rearrange("p a t -> p (a t)"), in_=pqTv[:, 0:512]), [pqT.b], [qT.b])
                for f_ in pend_mix:
                    yield from f_()
                yield
                yield from trans()

            def stage2(T, gen):
                tl = T - t0
                qT = qTs[T % 3]
                st = stat[T % 2]
                kts = [kt for kt in range(5) if T - 4 + kt >= 0]
                po = [banks[6], banks[7]]
                pend = []
                for hh in range(8):
                    hp, hl = hh // 2, hh % 2
                    pr = slice(64 * hl, 64 * hl + 64)
                    psA = psum()
                    psD = psum()

                    def mms(e, hh=hh, hp=hp, pr=pr, psA=psA, psD=psD):
                        for kt in kts:
                            slot = (T - 4 + kt) % 8
                            dst = psD[:, 0:128] if kt == 4 else psA[:, kt * 128:(kt + 1) * 128]
                            ins = e.matmul(dst, lhsT=KT[pr, hp, slot * 128:(slot + 1) * 128], rhs=qT[pr, hp, :], start=True, stop=True)
                        return ins
                    Tt(mms, [qT.b] + [KTb[(T - 4 + kt) % 8] for kt in kts], [psA.b, psD.b])
                    gen()
                    PT = PTs[pt_i[0] % 4]
                    pt_i[0] += 1
                    ka = [kt for kt in kts if kt < 4]

                    def ex(e, psA=psA, psD=psD, PT=PT, ka=ka):
                        if ka:
                            e.activation(out=PT[:, ka[0]:4, :], in_=psA[:, ka[0] * 128:512].rearrange("p (k t) -> p k t", t=128), func=AF.Exp)
                        return e.activation(out=PT[:, 4, :], in_=psD[:, 0:128], func=AF.Exp)
                    Aa(ex, [psA.b, psD.b], [PT.b])
                    k0 = kts[0]
                    P.op("vector",
                         lambda e, PT=PT, hh=hh, k0=k0: e.tensor_tensor(out=PT[:, k0:5, :], in0=PT[:, k0:5, :], in1=biasT[:, hh, k0:5, :], op=ALU.mult),
                         [PT.b, biasT.b], [PT.b])
                    pob = po[hh // 4]

                    def mmo(e, hh=hh, PT=PT, pob=pob):
                        for i, kt in enumerate(kts):
                            slot = (T - 4 + kt) % 8
                            ins = e.matmul(pob[:, (hh % 4) * 65:(hh % 4) * 65 + 65], lhsT=PT[:, kt, :], rhs=Vx[:, slot, hh, :],
                                           start=(i == 0), stop=(i == len(kts) - 1))
                        return ins
                    pend.append((mmo, [PT.b] + [Vxb[(T - 4 + kt) % 8] for kt in kts], [pob.b]))
                    if len(pend) > 2:
                        Tt(*pend.pop(0))
                    gen()
                while pend:
                    Tt(*pend.pop(0))
                for half in range(2):
                    pob = po[half]
                    pv4 = pob[:, 0:260].rearrange("p (h d) -> p h d", d=65)
                    Vv(lambda e, pv4=pv4, half=half: e.reciprocal(out=st[:, 64 + half * 4: 68 + half * 4].unsqueeze(2), in_=pv4[:, :, 64:65]),
                       [pob.b], [st.sb("rs%d" % half)])
                    Vv(lambda e, pv4=pv4, half=half: e.tensor_tensor(out=oc[:, half * 4:(half + 1) * 4, :], in0=pv4[:, :, 0:64],
                                                                     in1=st[:, 64 + half * 4: 68 + half * 4].unsqueeze(2).to_broadcast([128, 4, 64]), op=ALU.mult),
                       [pob.b, st.sb("rs%d" % half)], [oc.b])
                Gg(lambda e: e.tensor_tensor(out=oc2[:], in0=oc[:], in1=oc[:], op=ALU.mult), [oc.b], [oc2.b])
                Vv(lambda e: e.tensor_reduce(out=st[:, 72:80], in_=oc2[:], axis=AX.X, op=ALU.add), [oc2.b], [st.sb("s4")])
                Aa(lambda e: e.activation(out=st[:, 80:88], in_=st[:, 72:80], func=AF.Ln, bias=eps_t[:], scale=1.0 / 64.0),
                   [st.sb("s4"), eps_t.b], [st.sb("ln4")])
                Aa(lambda e: e.activation(out=st[:, 80:88], in_=st[:, 80:88], func=AF.Exp, scale=-0.5), [st.sb("ln4")], [st.sb("r4")])
                Vv(lambda e: e.tensor_tensor(out=oc2[:], in0=oc[:], in1=st[:, 80:88].unsqueeze(2).to_broadcast([128, 8, 64]), op=ALU.mult),
                   [oc.b, st.sb("r4")], [oc2.b])
                Gg(lambda e: e.tensor_tensor(out=ocn[:], in0=oc2[:].rearrange("p h d -> p (h d)"), in1=g_oc[:], op=ALU.mult), [oc2.b, g_oc.b], [ocn.b])
                DMA("gpsimd", oac_d[tl * 128:(tl + 1) * 128, 256:768], ocn[:], [ocn.b], [oac_bufs[tl]], store=True)

            halo = [T for T in range(t0 - 4, t0) if T >= 0]
            for T in halo:
                for _ in stage1(T, False):
                    pass
            for T in (t0, t0 + 1):
                if T < t0 + NT:
                    for _ in stage1(T, True):
                        pass
            genq = []

            def adv():
                while genq:
                    try:
                        next(genq[0][1])
                        return
                    except StopIteration:
                        genq.pop(0)

            def drain_upto(Tmax):
                while genq and genq[0][0] <= Tmax:
                    for _ in genq[0][1]:
                        pass
                    genq.pop(0)

            for tl in range(NT):
                if tl % 4 == 2:
                    drain_upto(t0 + tl + 1)
                    ut_batch(tl - 2)
                if tl + 2 < NT:
                    genq.append((t0 + tl + 2, stage1(t0 + tl + 2, True)))
                stage2(t0 + tl, adv)
                drain_upto(t0 + tl + 1)
            drain_upto(t0 + NT)
            P.barrier()
            sba.release(mk_A)

            phase_s5(nc, P, sba, dr, l, seg, cfg, UT, UTb, obT, s5_state, banks, eps_t, ident_f, dbg_d, SEG, NCH)
            P.barrier()
            sba.release(mk_UT)

            phase_c(nc, P, sba, dr, l, seg, cfg, obT, oac_d, oac_bufs, x_in, x_out, xin_bufs, banks, eps_t, ident_f, ident_b,
                    dbg_d, SEG, NT, NB, NBT, t0)
            P.barrier()
            sba.release(mk_seg)

        for seg in range(NSEG):
            do_seg(l, seg, x_in, x_out)

    P.op("sync", None, reads=xin_bufs[L], writes=())
    if dbg:
        P.op("sync", None, reads=[dbg_d["_b"]], writes=())
    block = stack.enter_context(nc.Block())
    P.emit(block)
    stack.close()
    return nc, sba.peak, P.nops


def cmul(P, eng, o_re, o_im, a_re, a_im, b_re, b_im, t1, t2, rb, wb):
    def f(e):
        e.tensor_tensor(out=t1, in0=a_re, in1=b_re, op=ALU.mult)
        e.tensor_tensor(out=t2, in0=a_im, in1=b_im, op=ALU.mult)
        e.tensor_tensor(out=o_re, in0=t1, in1=t2, op=ALU.subtract)
        e.tensor_tensor(out=t1, in0=a_re, in1=b_im, op=ALU.mult)
        e.tensor_tensor(out=t2, in0=a_im, in1=b_re, op=ALU.mult)
        return e.tensor_tensor(out=o_im, in0=t1, in1=t2, op=ALU.add)
    P.op(eng, f, rb, wb)


def _mk(P):
    def Vv(fn, r, w):
        P.op("vector", fn, r, w)

    def Aa(fn, r, w):
        P.op("scalar", fn, r, w)

    def Gg(fn, r, w):
        P.op("gpsimd", fn, r, w)

    def Tt(fn, r, w):
        P.op("tensor", fn, r, w)

    def DMA(eng, out_ap, in_ap, r, w, store=False):
        P.op(eng, lambda e: e.dma_start(out=out_ap, in_=in_ap), r, w, dma=True, store=store)
    return Vv, Aa, Gg, Tt, DMA


def powers(P, sba, name, lre, lim, ldt, rb, n, nk, kvec, keep_unit=False):
    Vv, Aa, Gg, Tt, DMA = _mk(P)
    t = lambda nm, shp, dt=F32: sba.tile(name + nm, shp, dt)
    dt_ = t("dt", [128, n]); lm = t("lm", [128, n]); th = t("th", [128, n])
    Aa(lambda e: e.activation(out=dt_[:], in_=ldt, func=AF.Exp), rb, [dt_.b])
    Vv(lambda e: e.tensor_tensor(out=lm[:], in0=lre, in1=dt_[:], op=ALU.mult), rb + [dt_.b], [lm.b])
    Vv(lambda e: e.tensor_tensor(out=th[:], in0=lim, in1=dt_[:], op=ALU.mult), rb + [dt_.b], [th.b])
    Vv(lambda e: e.tensor_scalar(out=th[:], in0=th[:], scalar1=1.0 / TWO_PI, scalar2=None, op0=ALU.mult), [th.b], [th.b])
    kb = kvec[:, 0:nk, :].to_broadcast([128, nk, n])
    mag = t("mag", [128, nk, n]); y = t("y", [128, nk, n]); yi = t("yi", [128, nk, n], I32); yf = t("yf", [128, nk, n])
    sn = t("sn", [128, nk, n]); cs = t("cs", [128, nk, n])
    Vv(lambda e: e.tensor_tensor(out=mag[:], in0=kb, in1=lm[:].unsqueeze(1).to_broadcast([128, nk, n]), op=ALU.mult), [lm.b, kvec.b], [mag.b])
    Aa(lambda e: e.activation(out=mag[:], in_=mag[:], func=AF.Exp), [mag.b], [mag.b])
    Vv(lambda e: e.tensor_tensor(out=y[:], in0=kb, in1=th[:].unsqueeze(1).to_broadcast([128, nk, n]), op=ALU.mult), [th.b, kvec.b], [y.b])
    for (dst, shift) in ((sn, 0.0), (cs, 0.25)):
        if shift:
            Vv(lambda e: e.tensor_scalar(out=y[:], in0=y[:], scalar1=shift, scalar2=None, op0=ALU.add), [y.b], [y.b])
        Vv(lambda e: e.tensor_copy(out=yi[:], in_=y[:]), [y.b], [yi.b])
        Vv(lambda e: e.tensor_copy(out=yf[:], in_=yi[:]), [yi.b], [yf.b])
        Vv(lambda e: e.tensor_tensor(out=yf[:], in0=y[:], in1=yf[:], op=ALU.subtract), [y.b, yf.b], [yf.b])
        Aa(lambda e, dst=dst: e.activation(out=dst[:], in_=yf[:], func=AF.Sin, scale=TWO_PI), [yf.b], [dst.b])
    if keep_unit:
        ark = t("ark", [128, nk, n]); aik = t("aik", [128, nk, n])
    else:
        ark, aik = cs, sn
    Vv(lambda e: e.tensor_tensor(out=ark[:], in0=mag[:], in1=cs[:], op=ALU.mult), [mag.b, cs.b], [ark.b])
    Vv(lambda e: e.tensor_tensor(out=aik[:], in0=mag[:], in1=sn[:], op=ALU.mult), [mag.b, sn.b], [aik.b])
    nr = t("nr", [128, n]); den = t("den", [128, n]); t1 = t("t1", [128, n]); t2 = t("t2", [128, n])
    cr = t("cr", [128, n]); ci = t("ci", [128, n])
    Vv(lambda e: e.tensor_scalar(out=nr[:], in0=ark[:, 1, :], scalar1=-1.0, scalar2=None, op0=ALU.add), [ark.b], [nr.b])
    Vv(lambda e: e.tensor_tensor(out=t1[:], in0=lre, in1=lre, op=ALU.mult), rb, [t1.b])
    Vv(lambda e: e.tensor_tensor(out=t2[:], in0=lim, in1=lim, op=ALU.mult), rb, [t2.b])
    Vv(lambda e: e.tensor_tensor(out=den[:], in0=t1[:], in1=t2[:], op=ALU.add), [t1.b, t2.b], [den.b])
    Vv(lambda e: e.reciprocal(out=den[:], in_=den[:]), [den.b], [den.b])
    Vv(lambda e: e.tensor_tensor(out=t1[:], in0=nr[:], in1=lre, op=ALU.mult), rb + [nr.b, den.b], [t1.b])
    Vv(lambda e: e.tensor_tensor(out=t2[:], in0=aik[:, 1, :], in1=lim, op=ALU.mult), rb + [aik.b, den.b], [t2.b])
    Vv(lambda e: e.tensor_tensor(out=cr[:], in0=t1[:], in1=t2[:], op=ALU.add), [t1.b, t2.b], [cr.b])
    Vv(lambda e: e.tensor_tensor(out=cr[:], in0=cr[:], in1=den[:], op=ALU.mult), [cr.b, den.b], [cr.b])
    Vv(lambda e: e.tensor_tensor(out=t1[:], in0=aik[:, 1, :], in1=lre, op=ALU.mult), rb + [aik.b, cr.b], [t1.b])
    Vv(lambda e: e.tensor_tensor(out=t2[:], in0=nr[:], in1=lim, op=ALU.mult), rb + [nr.b, cr.b], [t2.b])
    Vv(lambda e: e.tensor_tensor(out=ci[:], in0=t1[:], in1=t2[:], op=ALU.subtract), [t1.b, t2.b], [ci.b])
    Vv(lambda e: e.tensor_tensor(out=ci[:], in0=ci[:], in1=den[:], op=ALU.mult), [ci.b, den.b], [ci.b])
    return ark, aik, mag, cs, sn, cr, ci


def cmul_ops(P, eng, o_re, o_im, a_re, a_im, b_re, b_im, t1, t2, rb, ob_re, ob_im, tb1, tb2, neg_im=False):
    def op(fn, r, w):
        P.op(eng, fn, r, w)
    op(lambda e: e.tensor_tensor(out=t1, in0=a_re, in1=b_re, op=ALU.mult), rb, [tb1])
    op(lambda e: e.tensor_tensor(out=t2, in0=a_im, in1=b_im, op=ALU.mult), rb, [tb2])
    op(lambda e: e.tensor_tensor(out=o_re, in0=t1, in1=t2, op=ALU.subtract), [tb1, tb2], [ob_re])
    op(lambda e: e.tensor_tensor(out=t1, in0=a_re, in1=b_im, op=ALU.mult), rb + [ob_re], [tb1])
    op(lambda e: e.tensor_tensor(out=t2, in0=a_im, in1=b_re, op=ALU.mult), rb + [ob_re], [tb2])
    if neg_im:
        op(lambda e: e.scalar_tensor_tensor(out=o_im, in0=t1, scalar=-1.0, in1=t2, op0=ALU.mult, op1=ALU.subtract), [tb1, tb2], [ob_im])
    else:
        op(lambda e: e.tensor_tensor(out=o_im, in0=t1, in1=t2, op=ALU.add), [tb1, tb2], [ob_im])


def phase_s5(nc, P, sba, dr, l, seg, cfg, UT, UTb, obT, s5_state, banks, eps_t, ident_f, dbg_d, SEG, NCH):
    Vv, Aa, Gg, Tt, DMA = _mk(P)
    bi = [0]

    def psum():
        t = banks[bi[0] % 8]
        bi[0] += 1
        return t
    ld = lambda name, shape, src, dt=F32, eng="sync": (lambda t: (DMA(eng, t[:], src, [], [t.b]), t)[1])(sba.tile(name, shape, dt))
    d_bc = ld("d_bc", [128, 256], dr["d_bc"][l])
    Em = ld("Em", [128, 4, 128], dr["c_E"])
    kvec = ld("kvec", [128, 9, 1], dr["c_kvec"])
    glu_b = ld("glu_b", [128, 2], dr["glu_b"][l])
    g_ob = ld("g_ob", [128, 2], dr["g_ob"][l])
    glu_w = ld("glu_w", [128, 2, 256], dr["glu_w"][l].rearrange("(ct p) c -> p ct c", p=128), BF16, "gpsimd")
    blk64 = ld("blk64", [128, 128], dr["c_blk64"], BF16, "gpsimd")
    if seg == 0:
        Vv(lambda e: e.memset(s5_state[:], 0.0), [], [s5_state.b])
    Xp = sba.tile("Xp", [128, 8, 2, NCH + 1], BF16)
    Xpb = [Buf("Xp%d" % i) for i in range(8)]
    BD = sba.tile("BD", [128, 4, 8, 128], BF16)
    CA = sba.tile("CA", [128, 8, 2, 8, 128], BF16)
    mk_L1 = sba.mark()
    if seg == 0:
        WSk = sba.tile("WSk", [128, 8, 2, 512], BF16, top=True)
        mk = sba.mark()
        lamB = ld("lamB", [128, 3, 512], dr["lamB"][l])
        bB = ld("bB", [128, 2, 512], dr["bB"][l])
        lre_c = sba.tile("lre_c", [128, 3, 256], F32)
        Vv(lambda e: e.tensor_copy(out=lre_c[:].rearrange("p w (q c) -> p w q c", q=4),
                                   in_=lamB[:].rearrange("p w (q g c) -> p w q g c", q=4, g=2)[:, :, :, 0, :]), [lamB.b], [lre_c.b])
        arB, aiB, magB, csB, snB, crB, ciB = powers(P, sba, "pB", lre_c[:, 0, :], lre_c[:, 1, :], lre_c[:, 2, :], [lre_c.b], 256, 8, kvec)
        cbr = sba.tile("cbr", [128, 4, 2, 64], F32)
        cbi = sba.tile("cbi", [128, 4, 2, 64], F32)
        wt1 = sba.tile("wt1", [128, 4, 2, 64], F32)
        wt2 = sba.tile("wt2", [128, 4, 2, 64], F32)
        b4 = lambda ap: ap.rearrange("p (q g c) -> p q g c", q=4, g=2)
        c4 = lambda ap: ap.rearrange("p (q c) -> p q c", q=4).unsqueeze(2).to_broadcast([128, 4, 2, 64])
        cmul_ops(P, "vector", cbr[:], cbi[:], b4(bB[:, 0, :]), b4(bB[:, 1, :]), c4(crB[:]), c4(ciB[:]), wt1[:], wt2[:],
                 [bB.b, crB.b, ciB.b], cbr.b, cbi.b, wt1.b, wt2.b)
        wt3 = sba.tile("wt3", [128, 4, 2, 64], F32)
        wt4 = sba.tile("wt4", [128, 4, 2, 64], F32)
        for k in range(8):
            ev = (k % 2 == 0)
            cmul_ops(P, "vector" if ev else "gpsimd", b4(WSk[:, k, 0, :]), b4(WSk[:, k, 1, :]), cbr[:], cbi[:], c4(arB[:, k, :]), c4(aiB[:, k, :]),
                     (wt1 if ev else wt3)[:], (wt2 if ev else wt4)[:], [cbr.b, cbi.b, arB.b, aiB.b], WSk.sb(("r", k)), WSk.sb(("i", k)),
                     (wt1 if ev else wt3).b, (wt2 if ev else wt4).b)
        WSb = [WSk.sb((r, k)) for r in ("r", "i") for k in range(8)]
        P.barrier()
        sba.release(mk)
        Tc = sba.tile("Tc", [128, 8, NCH], F32, top=True)
        Ts = sba.tile("Ts", [128, 8, NCH], F32, top=True)
        rho8 = sba.tile("rho8", [128, 8], F32, top=True)
        mk = sba.mark()
        lamA = ld("lamA", [128, 3, 8], dr["lamA"][l])
        cA = ld("cA", [128, 2, 8, 128], dr["cA"][l])
        bZ = ld("bZ", [128, 2, 8, 64], dr["bZ"][l])
        arA, aiA, magA, csA, snA, crA, ciA = powers(P, sba, "pA", lamA[:, 0, :], lamA[:, 1, :], lamA[:, 2, :], [lamA.b], 8, 9, kvec, keep_unit=True)
        Vv(lambda e: e.tensor_copy(out=rho8[:], in_=magA[:, 8, :]), [magA.b], [rho8.b])
        Vv(lambda e: e.tensor_copy(out=Tc[:, :, 0:1], in_=csA[:, 8, :].unsqueeze(2)), [csA.b], [Tc.b])
        Vv(lambda e: e.tensor_copy(out=Ts[:, :, 0:1], in_=snA[:, 8, :].unsqueeze(2)), [snA.b], [Ts.b])
        mk2 = sba.mark()
        tt1 = sba.tile("tt1", [128, 8, NCH // 2], F32)
        tt2 = sba.tile("tt2", [128, 8, NCH // 2], F32)
        n = 1
        while n < NCH:
            mr = Tc[:, :, n - 1:n].to_broadcast([128, 8, n])
            mi = Ts[:, :, n - 1:n].to_broadcast([128, 8, n])
            cmul_ops(P, "vector", Tc[:, :, n:2 * n], Ts[:, :, n:2 * n], Tc[:, :, 0:n], Ts[:, :, 0:n], mr, mi,
                     tt1[:, :, 0:n], tt2[:, :, 0:n], [Tc.b, Ts.b], Tc.b, Ts.b, tt1.b, tt2.b)
            n *= 2
        P.barrier()
        sba.release(mk2)
        ncim = sba.tile("ncim", [128, 8, 128], F32)
        Vv(lambda e: e.tensor_scalar(out=ncim[:], in0=cA[:, 1, :, :], scalar1=-1.0, scalar2=None, op0=ALU.mult), [cA.b], [ncim.b])
        ct1 = sba.tile("ct1", [128, 8, 128], F32)
        ct2 = sba.tile("ct2", [128, 8, 128], F32)
        for i in range(8):
            ar = arA[:, i + 1, :].unsqueeze(2).to_broadcast([128, 8, 128])
            ai = aiA[:, i + 1, :].unsqueeze(2).to_broadcast([128, 8, 128])
            cmul_ops(P, "vector", CA[:, i, 0, :, :], CA[:, i, 1, :, :], cA[:, 0, :, :], cA[:, 1, :, :], ar, ai,
                     ct1[:], ct2[:], [cA.b, arA.b, aiA.b], CA.sb(("r", i)), CA.sb(("i", i)), ct1.b, ct2.b, neg_im=True)
        CAb = [CA.sb((r, i)) for r in ("r", "i") for i in range(8)]
        cbZr = sba.tile("cbZr", [128, 8, 64], F32)
        cbZi = sba.tile("cbZi", [128, 8, 64], F32)
        zt1a = ct1[:, :, 0:64]
        zt2a = ct2[:, :, 0:64]
        cmul_ops(P, "vector", cbZr[:], cbZi[:], bZ[:, 0, :, :], bZ[:, 1, :, :], crA[:].unsqueeze(2).to_broadcast([128, 8, 64]),
                 ciA[:].unsqueeze(2).to_broadcast([128, 8, 64]), zt1a, zt2a, [bZ.b, crA.b, ciA.b], cbZr.b, cbZi.b, ct1.b, ct2.b)
        Zr = sba.tile("Zr", [128, 4, 8, 64], F32)
        Zi = sba.tile("Zi", [128, 4, 8, 64], F32)
        Ed = sba.tile("Ed", [128, 4, 128], F32)
        for q in range(4):
            ctq = q // 2
            Vv(lambda e, q=q, ctq=ctq: e.tensor_tensor(out=Ed[:, q, :], in0=Em[:, q, :], in1=d_bc[:, ctq * 128:(ctq + 1) * 128], op=ALU.mult),
               [Em.b, d_bc.b], [Ed.sb(q)])
        for half in range(2):
            for ts_ in range(4):
                tau = half * 4 + ts_
                ar = arA[:, tau, :].unsqueeze(2).to_broadcast([128, 8, 64])
                ai = aiA[:, tau, :].unsqueeze(2).to_broadcast([128, 8, 64])
                cmul_ops(P, "vector", Zr[:, ts_, :, :], Zi[:, ts_, :, :], cbZr[:], cbZi[:], ar, ai, zt1a, zt2a,
                         [cbZr.b, cbZi.b, arA.b, aiA.b], Zr.sb(ts_), Zi.sb(ts_), ct1.b, ct2.b)
            Zb = [Zr.sb(t) for t in range(4)] + [Zi.sb(t) for t in range(4)]
            for q in range(4):
                ps = psum()

                def mmk(e, q=q, ps=ps):
                    for pl in range(2):
                        Pp = 2 * q + pl
                        for ts_ in range(4):
                            dst = ps[64 * pl:64 * pl + 64, ts_ * 128:(ts_ + 1) * 128]
                            e.matmul(dst, lhsT=Zr[:, ts_, Pp, :], rhs=cA[:, 0, Pp, :], start=True, stop=False)
                            ins = e.matmul(dst, lhsT=Zi[:, ts_, Pp, :], rhs=ncim[:, Pp, :], start=False, stop=True)
                    return ins
                Tt(mmk, Zb + [cA.b, ncim.b], [ps.b])
                if half == 0:
                    Vv(lambda e, q=q, ps=ps: e.tensor_tensor(out=BD[:, q, 0, :], in0=ps[:, 0:128], in1=Ed[:, q, :], op=ALU.add), [ps.b, Ed.sb(q)], [BD.sb((q, 0))])
                    Vv(lambda e, q=q, ps=ps: e.tensor_copy(out=BD[:, q, 1:4, :], in_=ps[:, 128:512].rearrange("p (t c) -> p t c", t=3)), [ps.b], [BD.sb((q, 1))])
                else:
                    Vv(lambda e, q=q, ps=ps: e.tensor_copy(out=BD[:, q, 4:8, :], in_=ps[:, 0:512].rearrange("p (t c) -> p t c", t=4)), [ps.b], [BD.sb((q, 2))])
        BDb = [BD.sb((q, k)) for q in range(4) for k in range(3)]
        P.barrier()
        sba.release(mk)

        cd = dr["_s5cache"]
        DMA("sync", cd["WSk"], WSk[:].rearrange("p a b c -> p (a b c)"), WSb, [cd["buf"]], store=True)
        DMA("sync", cd["BD"], BD[:].rearrange("p a b c -> p (a b c)"), BDb, [cd["buf"]], store=True)
        DMA("sync", cd["CA"], CA[:].rearrange("p a b c d -> p (a b c d)"), CAb, [cd["buf"]], store=True)
        DMA("sync", cd["Tc"], Tc[:].rearrange("p a b -> p (a b)"), [Tc.b], [cd["buf"]], store=True)
        DMA("sync", cd["Ts"], Ts[:].rearrange("p a b -> p (a b)"), [Ts.b], [cd["buf"]], store=True)
        DMA("sync", cd["rho8"], rho8[:], [rho8.b], [cd["buf"]], store=True)
    else:
        cd = dr["_s5cache"]
        WSk = sba.tile("WSk", [128, 8, 2, 512], BF16, top=True)
        Tc = sba.tile("Tc", [128, 8, NCH], F32, top=True)
        Ts = sba.tile("Ts", [128, 8, NCH], F32, top=True)
        rho8 = sba.tile("rho8", [128, 8], F32, top=True)
        DMA("sync", WSk[:].rearrange("p a b c -> p (a b c)"), cd["WSk"], [cd["buf"]], [WSk.b])
        DMA("sync", BD[:].rearrange("p a b c -> p (a b c)"), cd["BD"], [cd["buf"]], [BD.b])
        DMA("sync", CA[:].rearrange("p a b c d -> p (a b c d)"), cd["CA"], [cd["buf"]], [CA.b])
        DMA("sync", Tc[:].rearrange("p a b -> p (a b)"), cd["Tc"], [cd["buf"]], [Tc.b])
        DMA("sync", Ts[:].rearrange("p a b -> p (a b)"), cd["Ts"], [cd["buf"]], [Ts.b])
        DMA("sync", rho8[:], cd["rho8"], [cd["buf"]], [rho8.b])
        WSb, BDb, CAb = [WSk.b], [BD.b], [CA.b]

    tmp2 = [[sba.tile("s5t%d_%d" % (k, i), [128, NCH], F32) for i in range(8)] for k in range(2)]
    rhob2 = [sba.tile("rhob%d" % k, [128, NCH], F32) for k in range(2)]
    def do_pair(Pp):
        q, pl = Pp // 2, Pp % 2
        pr = slice(64 * pl, 64 * pl + 64)
        pS = [psum(), psum()]
        for ri in range(2):
            def mms(e, ri=ri, ps=pS[ri]):
                for j in range(8):
                    ins = e.matmul(ps[:, 0:NCH], lhsT=WSk[pr, 7 - j, ri, q * 128:(q + 1) * 128], rhs=UT[pr, q, j * NCH:(j + 1) * NCH], start=(j == 0), stop=(j == 7))
                return ins
            Tt(mms, WSb + UTb, [pS[ri].b])
        a, b_, c, d_, Rr, Ri, Wr, Wi = tmp2[Pp % 2]
        rhob = rhob2[Pp % 2]
        tc, ts = Tc[:, Pp, :], Ts[:, Pp, :]
        sr, si = pS[0][:, 0:NCH], pS[1][:, 0:NCH]
        Vv(lambda e: e.tensor_tensor(out=a[:], in0=sr, in1=tc, op=ALU.mult), [pS[0].b, Tc.b], [a.b])
        Vv(lambda e: e.tensor_tensor(out=b_[:], in0=si, in1=ts, op=ALU.mult), [pS[1].b, Ts.b], [b_.b])
        Vv(lambda e: e.tensor_tensor(out=c[:], in0=si, in1=tc, op=ALU.mult), [pS[1].b, Tc.b], [c.b])
        Vv(lambda e: e.tensor_tensor(out=d_[:], in0=sr, in1=ts, op=ALU.mult), [pS[0].b, Ts.b], [d_.b])
        Gg(lambda e: e.tensor_tensor(out=Rr[:], in0=a[:], in1=b_[:], op=ALU.add), [a.b, b_.b], [Rr.b])
        Gg(lambda e: e.tensor_tensor(out=Ri[:], in0=c[:], in1=d_[:], op=ALU.subtract), [c.b, d_.b], [Ri.b])
        Vv(lambda e: e.tensor_copy(out=rhob[:], in_=rho8[:, Pp:Pp + 1].to_broadcast([128, NCH])), [rho8.b], [rhob.b])
        Vv(lambda e: e.tensor_tensor_scan(out=Wr[:], data0=rhob[:], data1=Rr[:], initial=s5_state[:, Pp, 0:1], op0=ALU.mult, op1=ALU.add),
           [rhob.b, Rr.b, s5_state.b], [Wr.b])
        Vv(lambda e: e.tensor_tensor_scan(out=Wi[:], data0=rhob[:], data1=Ri[:], initial=s5_state[:, Pp, 1:2], op0=ALU.mult, op1=ALU.add),
           [rhob.b, Ri.b, s5_state.b], [Wi.b])
        Vv(lambda e: e.tensor_copy(out=Xp[:, Pp, :, 0:1], in_=s5_state[:, Pp, :].unsqueeze(2)), [s5_state.b], [Xpb[Pp]])
        Gg(lambda e: e.tensor_tensor(out=a[:], in0=Wr[:], in1=tc, op=ALU.mult), [Wr.b, Tc.b], [a.b])
        Gg(lambda e: e.tensor_tensor(out=b_[:], in0=Wi[:], in1=ts, op=ALU.mult), [Wi.b, Ts.b], [b_.b])
        Vv(lambda e: e.tensor_tensor(out=c[:], in0=Wr[:], in1=ts, op=ALU.mult), [Wr.b, Ts.b], [c.b])
        Vv(lambda e: e.tensor_tensor(out=d_[:], in0=Wi[:], in1=tc, op=ALU.mult), [Wi.b, Tc.b], [d_.b])
        Vv(lambda e: e.tensor_tensor(out=Rr[:], in0=a[:], in1=b_[:], op=ALU.subtract), [a.b, b_.b], [Rr.b])
        Vv(lambda e: e.tensor_tensor(out=Ri[:], in0=c[:], in1=d_[:], op=ALU.add), [c.b, d_.b], [Ri.b])
        Gg(lambda e: e.tensor_copy(out=Xp[:, Pp, 0, 1:NCH + 1], in_=Rr[:]), [Rr.b], [Xpb[Pp]])
        Gg(lambda e: e.tensor_copy(out=Xp[:, Pp, 1, 1:NCH + 1], in_=Ri[:]), [Ri.b], [Xpb[Pp]])
        Vv(lambda e: e.tensor_copy(out=s5_state[:, Pp, 0:1], in_=Rr[:, NCH - 1:NCH]), [Rr.b], [s5_state.b])
        Vv(lambda e: e.tensor_copy(out=s5_state[:, Pp, 1:2], in_=Ri[:, NCH - 1:NCH]), [Ri.b], [s5_state.b])
    for Pp in range(8):
        do_pair(Pp)
    P.barrier()
    sba.release(mk_L1)
    yg = sba.tile("yg", [128, 2, SEG], BF16)
    ygb = [Buf("yg%d" % c) for c in range(2)]
    for ct in range(2):
        for i in range(8):
            ps = psum()

            def mmy(e, ct=ct, i=i, ps=ps):
                first = True
                for q in (2 * ct, 2 * ct + 1):
                    for j in range(i + 1):
                        e.matmul(ps[:, 0:NCH], lhsT=BD[:, q, i - j, :], rhs=UT[:, q, j * NCH:(j + 1) * NCH], start=first, stop=False)
                        first = False
                for Pp in range(4 * ct, 4 * ct + 4):
                    for ri in range(2):
                        last = (Pp == 4 * ct + 3 and ri == 1)
                        ins = e.matmul(ps[:, 0:NCH], lhsT=CA[:, i, ri, Pp, :], rhs=Xp[:, Pp, ri, 0:NCH], start=False, stop=last)
                return ins
            Tt(mmy, BDb + CAb + UTb + Xpb, [ps.b])
            Aa(lambda e, ct=ct, i=i, ps=ps: e.activation(out=yg[:, ct, i:SEG:8], in_=ps[:, 0:NCH], func=AF.Gelu_apprx_tanh), [ps.b], [ygb[ct]])
    BW = min(512, SEG)
    sg2 = [sba.tile("sg%d" % k, [128, BW], F32) for k in range(2)]
    obf2 = [sba.tile("obf%d" % k, [128, BW], F32) for k in range(2)]
    sqb2 = [sba.tile("sqb%d" % k, [128, BW], BF16) for k in range(2)]
    rs2 = [sba.tile("rs_%d" % k, [128, BW], F32) for k in range(2)]
    dbt = sba.tile("dbt", [128, BW], F32) if dbg_d else None
    its = [(ct2, blk) for ct2 in range(2) for blk in range(SEG // BW)]
    ps2s = {}

    def gluA(n):
        ct2, blk = its[n]
        cs_ = slice(blk * BW, (blk + 1) * BW)
        sg, obf, sqb = sg2[n % 2], obf2[n % 2], sqb2[n % 2]
        ps = psum()

        def mmg(e, ct2=ct2, cs_=cs_, ps=ps):
            for ct in range(2):
                ins = e.matmul(ps[:, 0:BW], lhsT=glu_w[:, ct, ct2 * 128:(ct2 + 1) * 128], rhs=yg[:, ct, cs_], start=(ct == 0), stop=(ct == 1))
            return ins
        Tt(mmg, [glu_w.b] + ygb, [ps.b])
        Aa(lambda e: e.activation(out=sg[:], in_=ps[:, 0:BW], func=AF.Sigmoid, bias=glu_b[:, ct2:ct2 + 1]), [ps.b, glu_b.b], [sg.b])
        Vv(lambda e: e.tensor_tensor(out=obf[:], in0=yg[:, ct2, cs_], in1=sg[:], op=ALU.mult), [sg.b] + ygb, [obf.b])
        Gg(lambda e: e.tensor_tensor(out=sqb[:], in0=obf[:], in1=obf[:], op=ALU.mult), [obf.b], [sqb.b])
        ps2 = psum()
        ps2s[n] = ps2
        Tt(lambda e: e.matmul(ps2[:, 0:BW], lhsT=blk64[:], rhs=sqb[:], start=True, stop=True), [blk64.b, sqb.b], [ps2.b])

    def gluB(n):
        ct2, blk = its[n]
        cs_ = slice(blk * BW, (blk + 1) * BW)
        obf, rs_ = obf2[n % 2], rs2[n % 2]
        ps2 = ps2s[n]
        Aa(lambda e: e.activation(out=rs_[:], in_=ps2[:, 0:BW], func=AF.Ln, bias=eps_t[:], scale=1.0 / 64.0), [ps2.b, eps_t.b], [rs_.b])
        Aa(lambda e: e.activation(out=rs_[:], in_=rs_[:], func=AF.Exp, scale=-0.5), [rs_.b], [rs_.b])
        Vv(lambda e: e.scalar_tensor_tensor(out=obT[:, ct2, cs_], in0=obf[:], scalar=g_ob[:, ct2:ct2 + 1], in1=rs_[:], op0=ALU.mult, op1=ALU.mult),
           [obf.b, g_ob.b, rs_.b], [obT.b])
        if dbg_d:
            Vv(lambda e: e.tensor_copy(out=dbt[:], in_=obT[:, ct2, cs_]), [obT.b], [dbt.b])
            DMA("sync", dbg_d["ob"][:, ct2, cs_], dbt[:], [dbt.b], [dbg_d["_b"]], store=True)

    gluA(0)
    for n in range(len(its)):
        if n + 1 < len(its):
            gluA(n + 1)
        gluB(n)


_dummy_tiles = {}


def cbZ_dummy(sba, name):
    if name not in _dummy_tiles:
        _dummy_tiles[name] = sba.tile(name, [128, 4, 2, 64], F32)
    return _dummy_tiles[name]


def phase_c(nc, P, sba, dr, l, seg, cfg, obT, oac_d, oac_bufs, x_in, x_out, xin_bufs, banks, eps_t, ident_f, ident_b,
            dbg_d, SEG, NT, NB, NBT, t0):
    Vv, Aa, Gg, Tt, DMA = _mk(P)
    bi = [0]

    def psum():
        t = banks[4 + bi[0] % 4]
        bi[0] += 1
        return t
    bf_ = [0]

    def psum_f():
        t = banks[bf_[0] % 8]
        bf_[0] += 1
        return t
    bo = [0]

    def psum_o():
        t = (banks[(bo[0] % 2) * 2], banks[(bo[0] % 2) * 2 + 1])
        bo[0] += 1
        return t
    L_last = (x_out is not None)
    w_out = sba.tile("w_out", [128, 8, 1024], BF16)
    wov = dr["w_out"][l].rearrange("(kc p) c -> p kc c", p=128)
    for kc in range(0, 8, 2):
        DMA("gpsimd", w_out[:, kc:kc + 2, :], wov[:, kc:kc + 2, :], [], [w_out.sb(kc)])
    w_out_b = [w_out.sb(kc) for kc in range(0, 8, 2)]
    gTf = sba.tile("gTf", [128, 1024], F32)
    DMA("sync", gTf[:], dr["gT_ffn"][l].rearrange("p a b -> p (a b)"), [], [gTf.b])
    w_r = sba.tile("w_r", [128, 8, 20], F32)
    DMA("sync", w_r[:], dr["w_r"][l].rearrange("(dc p) c -> p dc c", p=128), [], [w_r.b])
    b_r = sba.tile("b_r", [128, 20], F32)
    DMA("sync", b_r[:], dr["b_r"][l], [], [b_r.b])
    wgu = [sba.tile("wgu%d" % i, [128, 4, 8, 512], BF16) for i in range(2)]
    wdn = [sba.tile("wdn%d" % i, [128, 4, 2, 1024], BF16) for i in range(2)]
    acc = sba.tile("acc", [128, NBT, 1024], F32)
    accb = [Buf("acc%d" % i) for i in range(NBT)]
    h2T = sba.tile("h2T", [128, 8, NB], BF16)
    h2Tb = [Buf("h2T%d" % i) for i in range(NBT)]
    lg = sba.tile("lg", [128, NBT, 20], F32)
    lgb = [Buf("lg%d" % i) for i in range(NBT)]
    gate = sba.tile("gate", [128, NBT, 16], F32)
    xts = [sba.tile("cxt%d" % i, [128, 1024], F32) for i in range(2)]
    oacs = [sba.tile("oact%d" % i, [128, 768], BF16) for i in range(1)]
    oT = sba.tile("oT", [128, 6, 128], BF16)
    st = [sba.tile("cst%d" % i, [128, 8], F32) for i in range(2)]
    h2Tfs = [sba.tile("h2Tf%d" % i, [128, 8, 128], F32) for i in range(2)]
    sl = [sba.tile("sl%d" % i, [128, 256], BF16) for i in range(2)]
    hid = [sba.tile("hid%d" % i, [128, 256], BF16) for i in range(2)]
    hidT = [sba.tile("hidT%d" % i, [128, 2, 128], BF16) for i in range(2)]
    rt = {k: sba.tile("rt_" + k, [128, NBT, n], F32) for k, n in
          (("m", 1), ("gs", 4), ("eg", 4), ("sg", 1), ("pg", 1), ("ohg", 4), ("tmp", 16), ("es", 4), ("m1", 1), ("d1", 4), ("oh1", 4),
           ("msk", 4), ("m2", 1), ("oh2", 4), ("e2", 1), ("w1", 1), ("w2", 1), ("gin", 4), ("gi2", 4))}

    wq = [0]

    def load_group(gidx):
        buf = gidx % 2
        g = gidx % 4
        for e_ in range(4):
            E = 4 * g + e_
            DMA("gpsimd", wgu[buf][:, e_, :, 0:256], dr["w_gate"][l, E].rearrange("(dc p) f -> p dc f", p=128), [], [wgu[buf].sb((e_, 0))])
            DMA("gpsimd", wgu[buf][:, e_, :, 256:512], dr["w_up"][l, E].rearrange("(dc p) f -> p dc f", p=128), [], [wgu[buf].sb((e_, 1))])
            DMA("gpsimd", wdn[buf][:, e_, :, :], dr["w_down"][l, E].rearrange("(fc p) c -> p fc c", p=128), [], [wdn[buf].sb(e_)])

    nblk = SEG // NB
    load_group(0)
    gctr = 0
    for blk in range(nblk):
        def tile_front(tb, blk=blk):
            tl = blk * NBT + tb
            T = t0 + tl
            xt = xts[tb % 2]
            oc_ = oacs[0]
            s_ = st[tb % 2]
            h2Tf = h2Tfs[tb % 2]
            DMA("sync", xt[:], x_in[T * 128:(T + 1) * 128, :], [xin_bufs[l][T]], [xt.b])
            DMA("sync", oc_[:], oac_d[tl * 128:(tl + 1) * 128, :], [oac_bufs[tl]], [oc_.b])
            pt = psum_f()
            ptv = pt[:].bitcast(BF16)

            def tr(e):
                for c in range(6):
                    ins = e.transpose(out=ptv[:, c * 128:(c + 1) * 128], in_=oc_[:, c * 128:(c + 1) * 128], identity=ident_b[:])
                return ins
            Tt(tr, [oc_.b, ident_b.b], [pt.b])
            yield
            Aa(lambda e: e.activation(out=oT[:].rearrange("p a b -> p (a b)"), in_=ptv[:, 0:768], func=AF.Copy), [pt.b], [oT.b])
            yield
            for half in range(2):
                ps = psum_f()

                def mmo(e, half=half, ps=ps):
                    for kc in range(8):
                        if kc < 2:
                            lh = oT[:, kc, :]
                        elif kc < 4:
                            lh = obT[:, kc - 2, tl * 128:(tl + 1) * 128]
                        else:
                            lh = oT[:, kc - 2, :]
                        ins = e.matmul(ps[:, 0:512], lhsT=lh, rhs=w_out[:, kc, half * 512:(half + 1) * 512], start=(kc == 0), stop=(kc == 7))
                    return ins
                Tt(mmo, [oT.b, obT.b] + w_out_b, [ps.b])
                yield
                Vv(lambda e, half=half, ps=ps: e.tensor_tensor(out=acc[:, tb, half * 512:(half + 1) * 512], in0=ps[:, 0:512], in1=xt[:, half * 512:(half + 1) * 512], op=ALU.add),
                   [ps.b, xt.b], [accb[tb]])
                yield
            if dbg_d:
                DMA("sync", dbg_d["xm"][T * 128:(T + 1) * 128, :], acc[:, tb, :], [accb[tb]], [dbg_d["_b"]], store=True)
            Aa(lambda e: e.activation(out=xt[:], in_=acc[:, tb, :], func=AF.Square, scale=1.0 / 32.0, accum_out=s_[:, 0:1]), [accb[tb]], [xt.b, s_.sb("a")])
            Aa(lambda e: e.activation(out=s_[:, 1:2], in_=s_[:, 0:1], func=AF.Ln, bias=eps_t[:], scale=1.0), [s_.sb("a"), eps_t.b], [s_.sb("b")])
            Aa(lambda e: e.activation(out=s_[:, 2:3], in_=s_[:, 1:2], func=AF.Exp, scale=-0.5), [s_.sb("b")], [s_.sb("c")])
            yield
            Vv(lambda e: e.tensor_scalar(out=xt[:], in0=acc[:, tb, :], scalar1=s_[:, 2:3], scalar2=None, op0=ALU.mult), [accb[tb], s_.sb("c")], [xt.b])
            yield
            pa, pb = psum_f(), psum_f()

            def trf(e):
                for dc in range(8):
                    pp = pa if dc < 4 else pb
                    ins = e.transpose(out=pp[:, (dc % 4) * 128:(dc % 4 + 1) * 128], in_=xt[:, dc * 128:(dc + 1) * 128], identity=ident_f[:])
                return ins
            Tt(trf, [xt.b, ident_f.b], [pa.b, pb.b])
            yield
            Vv(lambda e: e.tensor_tensor(out=h2Tf[:, 0:4, :].rearrange("p a b -> p (a b)"), in0=pa[:, 0:512], in1=gTf[:, 0:512], op=ALU.mult), [pa.b, gTf.b], [h2Tf.sb(0)])
            Vv(lambda e: e.tensor_tensor(out=h2Tf[:, 4:8, :].rearrange("p a b -> p (a b)"), in0=pb[:, 0:512], in1=gTf[:, 512:1024], op=ALU.mult), [pb.b, gTf.b], [h2Tf.sb(1)])
            yield
            Vv(lambda e: e.tensor_copy(out=h2T[:, :, tb * 128:(tb + 1) * 128], in_=h2Tf[:]), [h2Tf.sb(0), h2Tf.sb(1)], [h2Tb[tb]])
            pr_ = psum_f()

            def mmr(e):
                for dc in range(8):
                    ins = e.matmul(pr_[:, 0:20], lhsT=h2Tf[:, dc, :], rhs=w_r[:, dc, :], start=(dc == 0), stop=(dc == 7))
                return ins
            Tt(mmr, [h2Tf.sb(0), h2Tf.sb(1), w_r.b], [pr_.b])
            yield
            Vv(lambda e: e.tensor_tensor(out=lg[:, tb, :], in0=pr_[:, 0:20], in1=b_r[:], op=ALU.add), [pr_.b, b_r.b], [lgb[tb]])

        act_f = []
        nxt_tb = 0
        steps = {}
        while nxt_tb < NBT or act_f:
            if nxt_tb < NBT and (not act_f or (len(act_f) == 1 and steps[act_f[0][0]] >= 5)):
                act_f.append((nxt_tb, tile_front(nxt_tb)))
                steps[nxt_tb] = 0
                nxt_tb += 1
            for (tb_, g_) in list(act_f):
                try:
                    next(g_)
                    steps[tb_] += 1
                except StopIteration:
                    act_f.remove((tb_, g_))

        def R(fn, r, w, eng="vector"):
            P.op(eng, fn, [rt[k].b if isinstance(k, str) else k for k in r], [rt[k].b if isinstance(k, str) else k for k in w])
        g4 = lg[:, :, 0:4]
        el = lg[:, :, 4:20].rearrange("p n (g e) -> p n g e", g=4)
        bc = lambda k, n: rt[k][:].to_broadcast([128, NBT, n])
        R(lambda e: e.tensor_reduce(out=rt["m"][:], in_=g4, axis=AX.X, op=ALU.max), lgb, ["m"])
        R(lambda e: e.tensor_tensor(out=rt["gs"][:], in0=g4, in1=bc("m", 4), op=ALU.subtract), lgb + ["m"], ["gs"])
        R(lambda e: e.activation(out=rt["eg"][:], in_=rt["gs"][:], func=AF.Exp), ["gs"], ["eg"], "scalar")
        R(lambda e: e.tensor_reduce(out=rt["sg"][:], in_=rt["eg"][:], axis=AX.X, op=ALU.add), ["eg"], ["sg"])
        R(lambda e: e.reciprocal(out=rt["pg"][:], in_=rt["sg"][:]), ["sg"], ["pg"])
        R(lambda e: e.tensor_single_scalar(out=rt["ohg"][:], in_=rt["gs"][:], scalar=0.0, op=ALU.is_equal), ["gs"], ["ohg"])
        R(lambda e: e.tensor_tensor(out=rt["tmp"][:].rearrange("p n (g e) -> p n g e", g=4), in0=el,
                                    in1=rt["ohg"][:].unsqueeze(3).to_broadcast([128, NBT, 4, 4]), op=ALU.mult), lgb + ["ohg"], ["tmp"])
        R(lambda e: e.tensor_reduce(out=rt["es"][:], in_=rt["tmp"][:].rearrange("p n (g e) -> p n e g", g=4), axis=AX.X, op=ALU.add), ["tmp"], ["es"])
        R(lambda e: e.tensor_reduce(out=rt["m1"][:], in_=rt["es"][:], axis=AX.X, op=ALU.max), ["es"], ["m1"])
        R(lambda e: e.tensor_tensor(out=rt["d1"][:], in0=rt["es"][:], in1=bc("m1", 4), op=ALU.subtract), ["es", "m1"], ["d1"])
        R(lambda e: e.tensor_single_scalar(out=rt["oh1"][:], in_=rt["d1"][:], scalar=0.0, op=ALU.is_equal), ["d1"], ["oh1"])
        R(lambda e: e.scalar_tensor_tensor(out=rt["msk"][:], in0=rt["oh1"][:], scalar=-1e30, in1=rt["d1"][:], op0=ALU.mult, op1=ALU.add), ["oh1", "d1"], ["msk"])
        R(lambda e: e.tensor_reduce(out=rt["m2"][:], in_=rt["msk"][:], axis=AX.X, op=ALU.max), ["msk"], ["m2"])
        R(lambda e: e.tensor_tensor(out=rt["oh2"][:], in0=rt["msk"][:], in1=bc("m2", 4), op=ALU.is_equal), ["msk", "m2"], ["oh2"])
        R(lambda e: e.activation(out=rt["e2"][:], in_=rt["m2"][:], func=AF.Exp), ["m2"], ["e2"], "scalar")
        R(lambda e: e.tensor_scalar(out=rt["w1"][:], in0=rt["e2"][:], scalar1=1.0, scalar2=None, op0=ALU.add), ["e2"], ["w1"])
        R(lambda e: e.reciprocal(out=rt["w1"][:], in_=rt["w1"][:]), ["w1"], ["w1"])
        R(lambda e: e.tensor_tensor(out=rt["w2"][:], in0=rt["e2"][:], in1=rt["w1"][:], op=ALU.mult), ["e2", "w1"], ["w2"])
        R(lambda e: e.tensor_tensor(out=rt["gin"][:], in0=rt["oh1"][:], in1=bc("w1", 4), op=ALU.mult), ["oh1", "w1"], ["gin"])
        R(lambda e: e.tensor_tensor(out=rt["gi2"][:], in0=rt["oh2"][:], in1=bc("w2", 4), op=ALU.mult), ["oh2", "w2"], ["gi2"])
        R(lambda e: e.tensor_tensor(out=rt["gin"][:], in0=rt["gin"][:], in1=rt["gi2"][:], op=ALU.add), ["gin", "gi2"], ["gin"])
        R(lambda e: e.tensor_tensor(out=rt["gin"][:], in0=rt["gin"][:], in1=bc("pg", 4), op=ALU.mult), ["gin", "pg"], ["gin"])
        R(lambda e: e.tensor_tensor(out=gate[:].rearrange("p n (g e) -> p n g e", g=4), in0=rt["ohg"][:].unsqueeze(3).to_broadcast([128, NBT, 4, 4]),
                                    in1=rt["gin"][:].unsqueeze(2).to_broadcast([128, NBT, 4, 4]), op=ALU.mult), ["ohg", "gin"], [gate.b])

        for g in range(4):
            buf = gctr % 2
            last_load = (blk == nblk - 1 and g == 3)
            if not last_load:
                load_group(gctr + 1)
            wbs = [[wgu[buf].sb((e_, 0)), wgu[buf].sb((e_, 1)), wdn[buf].sb(e_)] for e_ in range(4)]
            units = [(tb, e_) for tb in range(NBT) for e_ in range(4)]
            ust = {}

            def stA(i, g=g, buf=buf, wbs=wbs, ust=ust):
                tb, e_ = units[i]
                E = 4 * g + e_
                k = i % 2
                if e_ == 0:
                    ust[("po", tb)] = psum_o()
                pg_ = psum()

                def mm1(e):
                    for dc in range(8):
                        ins = e.matmul(pg_[:, 0:512], lhsT=h2T[:, dc, tb * 128:(tb + 1) * 128], rhs=wgu[buf][:, e_, dc, :], start=(dc == 0), stop=(dc == 7))
                    return ins
                Tt(mm1, [h2Tb[tb]] + wbs[e_], [pg_.b])
                Aa(lambda e: e.activation(out=sl[k][:], in_=pg_[:, 0:256], func=AF.Silu), [pg_.b], [sl[k].b])
                Vv(lambda e: e.scalar_tensor_tensor(out=hid[k][:], in0=pg_[:, 256:512], scalar=gate[:, tb, E:E + 1], in1=sl[k][:],
                                                    op0=ALU.mult, op1=ALU.mult), [pg_.b, gate.b, sl[k].b], [hid[k].b])

            def stB1(i):
                k = i % 2
                pt = psum()
                ptv = pt[:].bitcast(BF16)

                def tr2(e):
                    for fc in range(2):
                        ins = e.transpose(out=ptv[:, fc * 128:(fc + 1) * 128], in_=hid[k][:, fc * 128:(fc + 1) * 128], identity=ident_b[:])
                    return ins
                Tt(tr2, [hid[k].b, ident_b.b], [pt.b])
                Aa(lambda e: e.activation(out=hidT[k][:].rearrange("p a b -> p (a b)"), in_=ptv[:, 0:256], func=AF.Copy), [pt.b], [hidT[k].b])

            def stB2(i, g=g, buf=buf, wbs=wbs, ust=ust, blk=blk):
                tb, e_ = units[i]
                k = i % 2
                po = ust[("po", tb)]

                def mm2(e):
                    for half in range(2):
                        for fc in range(2):
                            ins = e.matmul(po[half][:, 0:512], lhsT=hidT[k][:, fc, :], rhs=wdn[buf][:, e_, fc, half * 512:(half + 1) * 512],
                                           start=(e_ == 0 and fc == 0), stop=(e_ == 3 and fc == 1))
                    return ins
                Tt(mm2, [hidT[k].b] + wbs[e_], [po[0].b, po[1].b])
                if e_ == 3:
                    for half in range(2):
                        Vv(lambda e, half=half: e.tensor_tensor(out=acc[:, tb, half * 512:(half + 1) * 512], in0=acc[:, tb, half * 512:(half + 1) * 512],
                                                                in1=po[half][:, 0:512], op=ALU.add), [po[half].b, accb[tb]], [accb[tb]])
                    if g == 3:
                        T = t0 + blk * NBT + tb
                        DMA("sync", x_out[T * 128:(T + 1) * 128, :], acc[:, tb, :], [accb[tb]], [xin_bufs[l + 1][T]], store=True)

            stA(0)
            for i in range(len(units)):
                if i + 1 < len(units):
                    stA(i + 1)
                stB1(i)
                if i >= 1:
                    stB2(i - 1)
            stB2(len(units) - 1)
            gctr += 1


_CACHE = {}


def run(inputs, S, L, n_cores, debug=False, SEG=None):
    lay = host_layouts(inputs, L)
    shapes = {k: v.shape for k, v in lay.items()}
    shapes["x"] = (S, D)
    key = (S, L, n_cores, debug, SEG)
    if key not in _CACHE:
        _CACHE[key] = build(dict(S=S, L=L, debug=debug, SEG=SEG), shapes)
    nc, peak, nops = _CACHE[key]
    x = np.ascontiguousarray(inputs["x"], dtype=np.float32)
    in_maps = []
    for c in range(n_cores):
        m = dict(lay)
        m["x"] = np.ascontiguousarray(x[c])
        in_maps.append(m)
    res = run_bass_kernel_spmd(nc, in_maps, core_ids=list(range(n_cores)))
    return res.results


def kernel(**inputs):
    x = inputs["x"]
    B, S, _ = x.shape
    L = inputs["w_in"].shape[0]
    results = run(inputs, S, L, B)
    return np.stack([np.asarray(r["out"], dtype=np.float32) for r in results], axis=0)
```

```python
import math
from contextlib import ExitStack
import numpy as np
import concourse.bass as bass
import concourse.mybir as mybir
from concourse.bass_utils import run_bass_kernel_spmd

F32 = mybir.dt.float32
BF16 = mybir.dt.bfloat16
I32 = mybir.dt.int32
AF = mybir.ActivationFunctionType
ALU = mybir.AluOpType
AX = mybir.AxisListType

D = 1024
IN_COLS = 2304
EPS = 1e-6
NEG = -30000.0
TWO_PI = 2.0 * math.pi


class Sem:
    def __init__(self, h):
        self.h = h
        self.count = 0


class Buf:
    __slots__ = ("name", "w", "r", "sem")

    def __init__(self, name=""):
        self.name = name
        self.w = {}
        self.r = {}
        self.sem = None


ENGS = ["tensor", "vector", "scalar", "gpsimd", "sync"]


class Prog:
    def __init__(self, nc, stack, npool=94):
        self.nc = nc
        self.esem = {e: Sem(stack.enter_context(nc.semaphore("e_" + e))) for e in ENGS}
        nsw = 40
        self.pools = {"sw": [Sem(stack.enter_context(nc.semaphore("w%d" % i))) for i in range(nsw)],
                      "hw": [Sem(stack.enter_context(nc.semaphore("d%d" % i))) for i in range(npool - nsw)]}
        self.pool_i = {"sw": 0, "hw": 0}
        self.ops = {e: [] for e in ENGS}
        self.seen = {e: {} for e in ENGS}
        self.nops = 0

    def dsem(self, buf, eng):
        if buf.sem is None:
            k = "sw" if eng == "gpsimd" else "hw"
            assert self.pool_i[k] < len(self.pools[k]), "out of %s semaphores" % k
            buf.sem = self.pools[k][self.pool_i[k]]
            self.pool_i[k] += 1
        return buf.sem

    def op(self, eng, fn, reads=(), writes=(), dma=False, store=False):
        need = {}
        es = self.esem[eng]

        def add(d, skip_own):
            for sm, v in d.items():
                if skip_own and sm is es:
                    continue
                if need.get(sm, 0) < v:
                    need[sm] = v

        for b in reads:
            add(b.w, False)
        so = (eng == "tensor") and not dma
        for b in writes:
            if not store:
                add(b.w, so)
            add(b.r, so)
        waits = []
        seen = self.seen[eng]
        for sm, v in need.items():
            if seen.get(sm, 0) >= v:
                continue
            seen[sm] = v
            waits.append((sm, v))
        if fn is None:
            self.ops[eng].append((waits, None, None, 0))
            return
        if dma:
            sm = self.dsem(reads[0] if store else writes[0], eng)
            sm.count += 16
            tok = (sm, sm.count)
            inc = 16
        else:
            es.count += 1
            tok = (es, es.count)
            inc = 1
        for b in reads:
            if b.r.get(tok[0], 0) < tok[1]:
                b.r[tok[0]] = tok[1]
        for b in writes:
            if store:
                b.w[tok[0]] = tok[1]
            else:
                b.w = {tok[0]: tok[1]}
                b.r = {}
        self.ops[eng].append((waits, fn, tok[0], inc))
        self.nops += 1

    def barrier(self):
        for e in ENGS:
            waits = []
            seen = self.seen[e]
            for sm in list(self.esem.values()) + self.pools["sw"][: self.pool_i["sw"]] + self.pools["hw"][: self.pool_i["hw"]]:
                if sm.count == 0:
                    continue
                if seen.get(sm, 0) >= sm.count:
                    continue
                seen[sm] = sm.count
                waits.append((sm, sm.count))
            self.ops[e].append((waits, None, None, 0))

    def emit(self, block):
        for e in ENGS:
            ops = self.ops[e]

            def body(eng, ops=ops):
                for waits, fn, sm, inc in ops:
                    for (w, v) in waits:
                        eng.wait_ge(w.h, v)
                    if fn is not None:
                        fn(eng).then_inc(sm.h, inc)

            getattr(block, e)(body)


class Tile:
    def __init__(self, h, name):
        self.h = h
        self.b = Buf(name)
        self.sub = {}

    def __getitem__(self, k):
        return self.h[k]

    def sb(self, key):
        if key not in self.sub:
            self.sub[key] = Buf("%s/%s" % (self.b.name, key))
        return self.sub[key]


class SBAlloc:
    def __init__(self, nc):
        self.nc = nc
        self.top = 16640
        self.lim = 229376 - 64
        self.top2 = self.lim
        self.n = 0
        self.peak = 0
        self.P = None

    def tile(self, name, shape, dt, top=False):
        esz = {F32: 4, BF16: 2, I32: 4}[dt]
        nb = esz
        for s in shape[1:]:
            nb *= s
        if top:
            off = (self.top2 - nb) // 64 * 64
            self.top2 = off
        else:
            off = (self.top + 63) // 64 * 64
            self.top = off + nb
        self.peak = max(self.peak, self.top + (self.lim - self.top2))
        assert self.top <= self.top2, "SBUF overflow at %s: %d/%d" % (name, self.top, self.top2)
        self.n += 1
        h = self.nc.alloc_sbuf_tensor_at("%s_%d" % (name, self.n), list(shape), dt, offset=off)
        return Tile(h, name)

    def mark(self):
        return (self.top, dict(self.P.pool_i) if self.P else None, self.top2)

    def release(self, m):
        self.top = m[0]
        self.top2 = m[2]
        if self.P:
            self.P.pool_i = dict(m[1])


def _rep(a, n=128):
    return np.ascontiguousarray(np.broadcast_to(a[None], (n,) + a.shape)).astype(np.float32)


def host_layouts(inp, L):
    f = np.float32
    o = {}
    o["w_in"] = np.ascontiguousarray(inp["w_in"], dtype=f)
    ws5 = np.zeros((L, D, 4, 4, 32), f)
    ws5[:, :, :, :, :16] = inp["w_in"][:, :, 512:768].reshape(L, D, 4, 4, 16)
    o["w_s5"] = ws5.reshape(L, D, 512)
    gT = lambda g: np.ascontiguousarray(np.broadcast_to(g.reshape(L, 8, 128).transpose(0, 2, 1)[:, :, :, None], (L, 128, 8, 128))).astype(f)
    o["gT_mix"] = gT(inp["norm_mix"])
    o["gT_ffn"] = gT(inp["norm_ffn"])
    o["g_sgu"] = np.stack([_rep(inp["sgu_norm"][l]) for l in range(L)])
    o["wsT"] = np.ascontiguousarray(inp["sgu_w"].transpose(0, 3, 1, 2)).astype(f)
    o["bsT"] = np.ascontiguousarray(inp["sgu_b"].transpose(0, 2, 1)).astype(f)
    o["g_q"] = np.stack([_rep(np.tile(inp["q_norm"][l], 8)) for l in range(L)])
    o["g_k"] = np.stack([_rep(np.tile(inp["k_norm"][l], 8)) for l in range(L)])
    ki = np.arange(128)[:, None, None]
    kt = np.arange(5)[None, :, None]
    qi = np.arange(128)[None, None, :]
    idx = np.clip(128 * (4 - kt) + qi - ki, -256, 256) + 256
    o["biasT"] = np.ascontiguousarray(inp["rel_bias"][:, :, idx].transpose(0, 2, 1, 3, 4)).astype(f)
    o["g_oa"] = np.stack([_rep(inp["out_norm"][l, 0:256]) for l in range(L)])
    o["g_oc"] = np.stack([_rep(inp["out_norm"][l, 512:1024]) for l in range(L)])
    o["g_ob"] = np.ascontiguousarray(inp["out_norm"][:, 256:512].reshape(L, 2, 128).transpose(0, 2, 1)).astype(f)
    o["w_out"] = np.ascontiguousarray(inp["w_out"], dtype=f)
    wr = np.concatenate([inp["router_group_w"], inp["router_expert_w"].transpose(0, 2, 1, 3).reshape(L, D, 16)], axis=2)
    o["w_r"] = np.ascontiguousarray(wr).astype(f)
    br = np.concatenate([inp["router_group_b"], inp["router_expert_b"].reshape(L, 16)], axis=1)
    o["b_r"] = np.stack([_rep(br[l]) for l in range(L)])
    o["w_gate"] = np.ascontiguousarray(inp["w_gate"], dtype=f)
    o["w_up"] = np.ascontiguousarray(inp["w_up"], dtype=f)
    o["w_down"] = np.ascontiguousarray(inp["w_down"], dtype=f)
    lre, lim, ldt = inp["s5_lambda_re"], inp["s5_lambda_im"], inp["s5_log_dt"]
    bre, bim, cre, cim = inp["s5_b_re"], inp["s5_b_im"], inp["s5_c_re"], inp["s5_c_im"]
    def layA(v):
        return np.ascontiguousarray(v.reshape(L, 8, 2, 64).transpose(0, 2, 3, 1).reshape(L, 128, 8)).astype(f)
    o["lamA"] = np.stack([layA(lre), layA(lim), layA(np.broadcast_to(ldt[:, :, None], (L, 16, 64)))], axis=2)
    cA = np.zeros((L, 2, 2, 64, 8, 128), f)
    bZ = np.zeros((L, 2, 2, 64, 8, 64), f)
    for g in range(16):
        P_, gl2 = g // 2, g % 2
        c0 = 16 * (g % 8)
        cA[:, 0, gl2, :, P_, c0:c0 + 16] = cre[:, g].transpose(0, 2, 1)
        cA[:, 1, gl2, :, P_, c0:c0 + 16] = cim[:, g].transpose(0, 2, 1)
        bZ[:, 0, gl2, :, P_, 32 * gl2:32 * gl2 + 16] = bre[:, g]
        bZ[:, 1, gl2, :, P_, 32 * gl2:32 * gl2 + 16] = bim[:, g]
    o["cA"] = np.ascontiguousarray(cA.reshape(L, 2, 128, 8, 128).transpose(0, 2, 1, 3, 4))
    o["bZ"] = np.ascontiguousarray(bZ.reshape(L, 2, 128, 8, 64).transpose(0, 2, 1, 3, 4))
    lamB = np.zeros((L, 3, 4, 32, 4, 2, 64), f)
    bB = np.zeros((L, 2, 4, 32, 4, 2, 64), f)
    for g in range(16):
        q, gl = g // 4, g % 4
        lamB[:, 0, gl, :, q, :, :] = lre[:, g][:, None, None, :]
        lamB[:, 1, gl, :, q, :, :] = lim[:, g][:, None, None, :]
        lamB[:, 2, gl, :, q, :, :] = ldt[:, g][:, None, None, None]
        bB[:, 0, gl, :16, q, gl % 2, :] = bre[:, g].transpose(0, 2, 1)
        bB[:, 1, gl, :16, q, gl % 2, :] = bim[:, g].transpose(0, 2, 1)
    o["lamB"] = np.ascontiguousarray(lamB.reshape(L, 3, 128, 512).transpose(0, 2, 1, 3))
    o["bB"] = np.ascontiguousarray(bB.reshape(L, 2, 128, 512).transpose(0, 2, 1, 3))
    o["d_bc"] = np.stack([_rep(inp["s5_d"][l]) for l in range(L)])
    o["glu_w"] = np.ascontiguousarray(inp["s5_glu_w"], dtype=f)
    o["glu_b"] = np.ascontiguousarray(inp["s5_glu_b"].reshape(L, 2, 128).transpose(0, 2, 1)).astype(f)
    o["c_ident"] = np.eye(128, dtype=f)
    ch = np.arange(128) // 64
    o["c_maskT"] = (ch[:, None] <= ch[None, :]).astype(f)
    am = np.zeros((128, 2, 128), f)
    am[:64, 0, 64:] = NEG
    am[64:, 1, :64] = NEG
    o["c_amask"] = am
    E = np.zeros((128, 4, 128), f)
    for g in range(16):
        q, gl = g // 4, g % 4
        for h in range(16):
            E[32 * gl + h, q, 16 * (g % 8) + h] = 1.0
    o["c_E"] = E
    kv = np.zeros((128, 9, 1), f)
    kv[:, :, 0] = np.arange(9)[None, :]
    o["c_kvec"] = kv
    blk = np.arange(128) // 64
    o["c_blk64"] = (blk[:, None] == blk[None, :]).astype(f)
    return o


def build(cfg, shapes):
    S = cfg["S"]
    L = cfg["L"]
    SEG = cfg.get("SEG") or min(S, 4096)
    NSEG = S // SEG
    NT = SEG // 128
    NB = min(SEG, 1024)
    NBT = NB // 128
    NCH = SEG // 8
    dbg = cfg.get("debug")

    nc = bass.Bass("TRN2", target_bir_lowering=False)
    dr = {k: nc.dram_tensor(k, list(v), F32, kind="ExternalInput").ap() for k, v in shapes.items()}
    out_d = nc.dram_tensor("out", [S, D], F32, kind="ExternalOutput").ap()
    x1_d = nc.dram_tensor("x1s", [S, D], F32, kind="Internal").ap() if L > 1 else None
    oac_d = nc.dram_tensor("oac", [SEG, 768], BF16, kind=("ExternalOutput" if dbg else "Internal")).ap()
    dbg_d = {}
    if dbg:
        dbg_d["ob"] = nc.dram_tensor("dbg_ob", [128, 2, SEG], F32, kind="ExternalOutput").ap()
        dbg_d["xm"] = nc.dram_tensor("dbg_xm", [S, D], F32, kind="ExternalOutput").ap()

    NCH_ = SEG // 8
    dr["_s5cache"] = {
        "WSk": nc.dram_tensor("s5c_WSk", [128, 8 * 2 * 512], BF16, kind="Internal").ap(),
        "BD": nc.dram_tensor("s5c_BD", [128, 4 * 8 * 128], BF16, kind="Internal").ap(),
        "CA": nc.dram_tensor("s5c_CA", [128, 8 * 2 * 8 * 128], BF16, kind="Internal").ap(),
        "Tc": nc.dram_tensor("s5c_Tc", [128, 8 * NCH_], F32, kind="Internal").ap(),
        "Ts": nc.dram_tensor("s5c_Ts", [128, 8 * NCH_], F32, kind="Internal").ap(),
        "rho8": nc.dram_tensor("s5c_rho8", [128, 8], F32, kind="Internal").ap(),
        "buf": Buf("s5cache"),
    }
    stack = ExitStack()
    P = Prog(nc, stack)
    sba = SBAlloc(nc)
    sba.P = P
    banks = []
    for i in range(8):
        banks.append(Tile(nc.alloc_psum_tensor("bank%d" % i, [128, 512], F32), "bank%d" % i))
    bank_i = [0]

    def psum():
        t = banks[bank_i[0] % 6]
        bank_i[0] += 1
        return t

    def Vv(fn, r, w):
        P.op("vector", fn, r, w)

    def Aa(fn, r, w):
        P.op("scalar", fn, r, w)

    def Gg(fn, r, w):
        P.op("gpsimd", fn, r, w)

    def Tt(fn, r, w):
        P.op("tensor", fn, r, w)

    def DMA(eng, out_ap, in_ap, r, w, store=False):
        P.op(eng, lambda e: e.dma_start(out=out_ap, in_=in_ap), r, w, dma=True, store=store)

    xin_bufs = []
    for l_ in range(L + 1):
        grp = [Buf("x%d_%d" % (l_, t)) for t in range((S // 128 + 7) // 8)]
        xin_bufs.append([grp[t // 8] for t in range(S // 128)])
    ogrp = [Buf("oac%d" % t) for t in range((NT + 3) // 4)]
    oac_bufs = [ogrp[t // 4] for t in range(NT)]
    x_aps = [dr["x"]] + [x1_d] * (L - 1) + [out_d]
    cb = Buf("consts")

    ident_f = sba.tile("ident_f", [128, 128], F32)
    ident_b = sba.tile("ident_b", [128, 128], BF16)
    eps_t = sba.tile("eps", [128, 1], F32)
    DMA("sync", ident_f[:], dr["c_ident"], [], [ident_f.b])
    DMA("gpsimd", ident_b[:], dr["c_ident"], [], [ident_b.b])
    Vv(lambda e: e.memset(eps_t[:], EPS), [], [eps_t.b])
    s5_state = sba.tile("s5state", [128, 8, 2], F32)
    if dbg:
        dbg_d["_b"] = Buf("dbg")

    for l in range(L):
        x_in = x_aps[l]
        x_out = x_aps[l + 1]
        def do_seg(l, seg, x_in, x_out):
            t0 = seg * NT
            mk_seg = sba.mark()
            obT = sba.tile("obT", [128, 2, SEG], BF16)
            mk_UT = sba.mark()
            UT = sba.tile("UT", [128, 4, SEG], BF16)
            UTb = [Buf("UT%d" % t) for t in range(NT)]
            mk_A = sba.mark()
            w_in_sb = sba.tile("w_in", [128, 8, IN_COLS], BF16)
            w_s5_sb = sba.tile("w_s5", [128, 8, 512], BF16)
            w_in_v = dr["w_in"][l].rearrange("(dc p) c -> p dc c", p=128)
            w_s5_v = dr["w_s5"][l].rearrange("(dc p) c -> p dc c", p=128)
            for dc in range(0, 8, 2):
                DMA("gpsimd", w_in_sb[:, dc:dc + 2, :], w_in_v[:, dc:dc + 2, :], [], [w_in_sb.sb(dc)])
            DMA("gpsimd", w_s5_sb[:], w_s5_v, [], [w_s5_sb.b])
            w_in_bufs = [w_in_sb.sb(dc) for dc in range(0, 8, 2)]
            gT = sba.tile("gT", [128, 1024], F32)
            DMA("sync", gT[:], dr["gT_mix"][l].rearrange("p a b -> p (a b)"), [], [gT.b])
            g_sgu = sba.tile("g_sgu", [128, 256], F32)
            DMA("sync", g_sgu[:], dr["g_sgu"][l], [], [g_sgu.b])
            wsT_f = sba.tile("wsT_f", [128, 4, 128], F32)
            DMA("sync", wsT_f[:], dr["wsT"][l], [], [wsT_f.b])
            maskT = sba.tile("maskT", [128, 128], F32)
            DMA("sync", maskT[:], dr["c_maskT"], [], [maskT.b])
            wsT_b = sba.tile("wsT_b", [128, 4, 128], BF16)
            Vv(lambda e: e.tensor_tensor(out=wsT_b[:], in0=wsT_f[:], in1=maskT[:].unsqueeze(1).to_broadcast([128, 4, 128]), op=ALU.mult),
               [wsT_f.b, maskT.b], [wsT_b.b])
            bsT = sba.tile("bsT", [128, 4], F32)
            DMA("sync", bsT[:], dr["bsT"][l], [], [bsT.b])
            g_q = sba.tile("g_q", [128, 512], F32)
            g_k = sba.tile("g_k", [128, 512], F32)
            g_oa = sba.tile("g_oa", [128, 256], F32)
            g_oc = sba.tile("g_oc", [128, 512], F32)
            DMA("sync", g_q[:], dr["g_q"][l], [], [g_q.b])
            DMA("sync", g_k[:], dr["g_k"][l], [], [g_k.b])
            DMA("sync", g_oa[:], dr["g_oa"][l], [], [g_oa.b])
            DMA("sync", g_oc[:], dr["g_oc"][l], [], [g_oc.b])
            biasT = sba.tile("biasT", [128, 8, 5, 128], BF16)
            mk_tmp = sba.mark()
            biasT_f = sba.tile("biasT_f", [128, 8, 5, 128], F32)
            amask = sba.tile("amask", [128, 2, 128], F32)
            DMA("sync", biasT_f[:], dr["biasT"][l], [], [biasT_f.b])
            DMA("sync", amask[:], dr["c_amask"], [], [amask.b])
            Vv(lambda e: e.tensor_tensor(out=biasT_f[:, :, 0, :], in0=biasT_f[:, :, 0, :], in1=amask[:, 0:1, :].to_broadcast([128, 8, 128]), op=ALU.add),
               [biasT_f.b, amask.b], [biasT_f.b])
            Vv(lambda e: e.tensor_tensor(out=biasT_f[:, :, 4, :], in0=biasT_f[:, :, 4, :], in1=amask[:, 1:2, :].to_broadcast([128, 8, 128]), op=ALU.add),
               [biasT_f.b, amask.b], [biasT_f.b])
            Aa(lambda e: e.activation(out=biasT[:], in_=biasT_f[:], func=AF.Exp), [biasT_f.b], [biasT.b])
            P.barrier()
            sba.release(mk_tmp)
            KT = sba.tile("KT", [128, 4, 8 * 128], BF16)
            KTb = [Buf("KT%d" % s) for s in range(8)]
            Vx = sba.tile("Vx", [128, 8, 8, 65], BF16)
            Vxb = [Buf("Vx%d" % s) for s in range(8)]
            Vv(lambda e: e.memset(Vx[:], 1.0), [], [Vx.b] + Vxb)
            xts = [sba.tile("xt%d" % i, [128, 1024], F32) for i in range(2)]
            junk = sba.tile("junk", [128, 1024], BF16)
            stat = [sba.tile("stat%d" % i, [128, 96], F32) for i in range(2)]
            for st_ in stat:
                Vv(lambda e, st_=st_: e.memset(st_[:], 1.0), [], [st_.b] + [st_.sb(k_) for k_ in ("ssq", "ln", "rstd", "s2", "ln2", "r2", "s3", "ln3", "r3", "rs0", "rs1", "s4", "ln4", "r4")])
            hb = sba.tile("hb", [128, 1024], BF16)
            hTr = sba.tile("hTr", [128, 8, 512], BF16)
            hTb = [Buf("hT%d" % i) for i in range(4)]
            gz = sba.tile("gz", [128, 512], F32)
            vn = sba.tile("vn", [128, 256], BF16)
            oa = sba.tile("oa", [128, 256], F32)
            oa2 = sba.tile("oa2", [128, 256], F32)
            oan = sba.tile("oan", [128, 256], BF16)
            qraw = [sba.tile("qraw%d" % i, [128, 512], F32) for i in range(2)]
            kraw = [sba.tile("kraw%d" % i, [128, 512], F32) for i in range(2)]
            sq = sba.tile("sq", [128, 512], F32)
            sq2 = sba.tile("sq2", [128, 512], F32)
            qn = sba.tile("qn", [128, 512], BF16)
            kn = sba.tile("kn", [128, 512], BF16)
            qTs = [sba.tile("qT%d" % i, [128, 4, 128], BF16) for i in range(3)]
            PTs = [sba.tile("PT%d" % i, [128, 5, 128], BF16) for i in range(4)]
            oc = sba.tile("oc", [128, 8, 64], F32)
            oc2 = sba.tile("oc2", [128, 8, 64], F32)
            ocn = sba.tile("ocn", [128, 512], BF16)
            pt_i = [0]

            def ut_batch(tl0):
                for q in range(4):
                    pu = psum()

                    def mmu(e, q=q, pu=pu):
                        for dc in range(8):
                            ins = e.matmul(pu[:, 0:512], lhsT=w_s5_sb[:, dc, q * 128:(q + 1) * 128], rhs=hTr[:, dc, :], start=(dc == 0), stop=(dc == 7))
                        return ins
                    Tt(mmu, hTb + [w_s5_sb.b], [pu.b])
                    P.op("vector" if q % 2 == 0 else "scalar",
                         (lambda e, q=q, pu=pu: e.tensor_copy(out=UT[:, q, :].rearrange("p (j n) -> p j n", j=8)[:, :, tl0 * 16:tl0 * 16 + 64], in_=pu[:, 0:512].rearrange("p (n j) -> p j n", j=8))) if q % 2 == 0 else
                         (lambda e, q=q, pu=pu: e.activation(out=UT[:, q, :].rearrange("p (j n) -> p j n", j=8)[:, :, tl0 * 16:tl0 * 16 + 64], in_=pu[:, 0:512].rearrange("p (n j) -> p j n", j=8), func=AF.Copy)),
                         [pu.b], [UTb[tl0 + q]])

            def stage1(T, full):
                tl = T - t0
                pend_mix = []
                xt = xts[T % 2]
                st = stat[T % 2]
                hs = T % 4
                slot = T % 8
                DMA("sync", xt[:], x_in[T * 128:(T + 1) * 128, :], [xin_bufs[l][T]], [xt.b])
                Aa(lambda e: e.activation(out=junk[:], in_=xt[:], func=AF.Square, scale=1.0 / 32.0, accum_out=st[:, 0:1]),
                   [xt.b], [junk.b, st.sb("ssq")])
                Aa(lambda e: e.activation(out=st[:, 1:2], in_=st[:, 0:1], func=AF.Ln, bias=eps_t[:], scale=1.0),
                   [st.sb("ssq"), eps_t.b], [st.sb("ln")])
                Aa(lambda e: e.activation(out=st[:, 2:3], in_=st[:, 1:2], func=AF.Exp, scale=-0.5),
                   [st.sb("ln")], [st.sb("rstd")])
                Vv(lambda e: e.tensor_scalar(out=hb[:], in0=xt[:], scalar1=st[:, 2:3], scalar2=None, op0=ALU.mult),
                   [xt.b, st.sb("rstd")], [hb.b])
                yield
                pT = psum()
                pTv = pT[:].bitcast(BF16)

                def tr(e):
                    for dc in range(8):
                        ins = e.transpose(out=pTv[:, dc * 128:(dc + 1) * 128], in_=hb[:, dc * 128:(dc + 1) * 128], identity=ident_b[:])
                    return ins
                Tt(tr, [hb.b, ident_b.b], [pT.b])
                yield
                Vv(lambda e: e.tensor_tensor(out=hTr[:, :, hs * 128:(hs + 1) * 128], in0=pTv[:, 0:1024].rearrange("p (a b) -> p a b", a=8),
                                             in1=gT[:].rearrange("p (a b) -> p a b", a=8), op=ALU.mult),
                   [pT.b, gT.b], [hTb[hs]])
                yield

                def proj(c0, n=512):
                    ps = psum()

                    def mm(e):
                        for dc in range(8):
                            ins = e.matmul(ps[:, 0:n], lhsT=hTr[:, dc, hs * 128:(hs + 1) * 128], rhs=w_in_sb[:, dc, c0:c0 + n], start=(dc == 0), stop=(dc == 7))
                        return ins
                    Tt(mm, [hTb[hs]] + w_in_bufs, [ps.b])
                    return ps

                if full:
                    pz = proj(0)
                    yield
                    Aa(lambda e: e.activation(out=gz[:], in_=pz[:, 0:512], func=AF.Gelu_apprx_tanh), [pz.b], [gz.b])
                    yield
                    pq = proj(768)
                    yield
                    qr = qraw[T % 2]
                    Aa(lambda e: e.activation(out=qr[:], in_=pq[:, 0:512], func=AF.Copy), [pq.b], [qr.b])
                    yield
                pk = proj(1280)
                yield
                kr = kraw[T % 2]
                Aa(lambda e: e.activation(out=kr[:], in_=pk[:, 0:512], func=AF.Copy), [pk.b], [kr.b])
                yield
                pv = proj(1792)
                yield
                Aa(lambda e: e.activation(out=Vx[:, slot, :, 0:64], in_=pv[:, 0:512].rearrange("p (h d) -> p h d", h=8), func=AF.Copy),
                   [pv.b], [Vxb[slot]])
                yield
                rd = []
                if full:
                    Aa(lambda e: e.activation(out=junk[:, 0:256], in_=gz[:, 256:512], func=AF.Square, scale=1.0 / 16.0, accum_out=st[:, 8:9]),
                       [gz.b], [junk.b, st.sb("s2")])
                    Gg(lambda e: e.tensor_tensor(out=sq[:], in0=qr[:], in1=qr[:], op=ALU.mult), [qr.b], [sq.b])
                    Vv(lambda e: e.tensor_reduce(out=st[:, 16:24], in_=sq[:].rearrange("p (h d) -> p h d", h=8), axis=AX.X, op=ALU.add),
                       [sq.b], [st.sb("s2")])
                Gg(lambda e: e.tensor_tensor(out=sq2[:], in0=kr[:], in1=kr[:], op=ALU.mult), [kr.b], [sq2.b])
                Vv(lambda e: e.tensor_reduce(out=st[:, 24:32], in_=sq2[:].rearrange("p (h d) -> p h d", h=8), axis=AX.X, op=ALU.add),
                   [sq2.b], [st.sb("s2")])
                if not full:
                    Vv(lambda e: e.memset(st[:, 8:24], 1.0), [], [st.sb("s2")])
                if full:
                    Vv(lambda e: e.tensor_scalar(out=st[:, 8:9], in0=st[:, 8:9], scalar1=64.0, scalar2=None, op0=ALU.mult),
                       [st.sb("s2")], [st.sb("s2")])
                Aa(lambda e: e.activation(out=st[:, 32:56], in_=st[:, 8:32], func=AF.Ln, bias=eps_t[:], scale=1.0 / 64.0),
                   [st.sb("s2"), eps_t.b], [st.sb("ln2")])
                Aa(lambda e: e.activation(out=st[:, 32:56], in_=st[:, 32:56], func=AF.Exp, scale=-0.5),
                   [st.sb("ln2")], [st.sb("r2")])
                if full:
                    Vv(lambda e: e.scalar_tensor_tensor(out=vn[:], in0=gz[:, 256:512], scalar=st[:, 32:33], in1=g_sgu[:], op0=ALU.mult, op1=ALU.mult),
                       [gz.b, st.sb("r2"), g_sgu.b], [vn.b])
                    def sgu_mix():
                        pm = psum()

                        def mmx(e):
                            for hh in range(4):
                                ins = e.matmul(pm[:, hh * 64:(hh + 1) * 64], lhsT=wsT_b[:, hh, :], rhs=vn[:, hh * 64:(hh + 1) * 64], start=True, stop=True)
                            return ins
                        Tt(mmx, [wsT_b.b, vn.b], [pm.b])
                        yield

                        def sgu_out(e):
                            for hh in range(4):
                                ins = e.scalar_tensor_tensor(out=oa[:, hh * 64:(hh + 1) * 64], in0=pm[:, hh * 64:(hh + 1) * 64], scalar=bsT[:, hh:hh + 1],
                                                             in1=gz[:, hh * 64:(hh + 1) * 64], op0=ALU.add, op1=ALU.mult)
                            return ins
                        Vv(sgu_out, [pm.b, bsT.b, gz.b], [oa.b])
                        Gg(lambda e: e.tensor_tensor(out=oa2[:], in0=oa[:], in1=oa[:], op=ALU.mult), [oa.b], [oa2.b])
                        Vv(lambda e: e.tensor_reduce(out=st[:, 56:60], in_=oa2[:].rearrange("p (h d) -> p h d", h=4), axis=AX.X, op=ALU.add),
                           [oa2.b], [st.sb("s3")])
                        Aa(lambda e: e.activation(out=st[:, 60:64], in_=st[:, 56:60], func=AF.Ln, bias=eps_t[:], scale=1.0 / 64.0),
                           [st.sb("s3"), eps_t.b], [st.sb("ln3")])
                        Aa(lambda e: e.activation(out=st[:, 60:64], in_=st[:, 60:64], func=AF.Exp, scale=-0.5), [st.sb("ln3")], [st.sb("r3")])
                        Vv(lambda e: e.tensor_tensor(out=oa2[:].rearrange("p (h d) -> p h d", h=4), in0=oa[:].rearrange("p (h d) -> p h d", h=4),
                                                     in1=st[:, 60:64].unsqueeze(2).to_broadcast([128, 4, 64]), op=ALU.mult),
                           [oa.b, st.sb("r3")], [oa2.b])
                        Gg(lambda e: e.tensor_tensor(out=oan[:], in0=oa2[:], in1=g_oa[:], op=ALU.mult), [oa2.b, g_oa.b], [oan.b])
                        DMA("gpsimd", oac_d[tl * 128:(tl + 1) * 128, 0:256], oan[:], [oan.b], [oac_bufs[tl]], store=True)
                    pend_mix.append(sgu_mix)
                    Vv(lambda e: e.tensor_scalar(out=st[:, 40:48], in0=st[:, 40:48], scalar1=0.125, scalar2=None, op0=ALU.mult),
                       [st.sb("r2")], [st.sb("r2")])
                    Vv(lambda e: e.tensor_tensor(out=sq[:].rearrange("p (h d) -> p h d", h=8), in0=qr[:].rearrange("p (h d) -> p h d", h=8),
                                                 in1=st[:, 40:48].unsqueeze(2).to_broadcast([128, 8, 64]), op=ALU.mult),
                       [qr.b, st.sb("r2")], [sq.b])
                    Gg(lambda e: e.tensor_tensor(out=qn[:], in0=sq[:], in1=g_q[:], op=ALU.mult), [sq.b, g_q.b], [qn.b])
                Vv(lambda e: e.tensor_tensor(out=sq2[:].rearrange("p (h d) -> p h d", h=8), in0=kr[:].rearrange("p (h d) -> p h d", h=8),
                                             in1=st[:, 48:56].unsqueeze(2).to_broadcast([128, 8, 64]), op=ALU.mult),
                   [kr.b, st.sb("r2")], [sq2.b])
                Gg(lambda e: e.tensor_tensor(out=kn[:], in0=sq2[:], in1=g_k[:], op=ALU.mult), [sq2.b, g_k.b], [kn.b])
                yield
                def trans():
                    pkT = psum()
                    pkTv = pkT[:].bitcast(BF16)

                    def trk(e):
                        for hp in range(4):
                            ins = e.transpose(out=pkTv[:, hp * 128:(hp + 1) * 128], in_=kn[:, hp * 128:(hp + 1) * 128], identity=ident_b[:])
                        return ins
                    Tt(trk, [kn.b, ident_b.b], [pkT.b])
                    yield
                    Aa(lambda e: e.activation(out=KT[:, :, slot * 128:(slot + 1) * 128], in_=pkTv[:, 0:512].rearrange("p (a t) -> p a t", a=4), func=AF.Copy),
                       [pkT.b], [KTb[slot]])
                    if full:
                        qT = qTs[T % 3]
                        pqT = psum()
                        pqTv = pqT[:].bitcast(BF16)

                        def trq(e):
                            for hp in range(4):
                                ins = e.transpose(out=pqTv[:, hp * 128:(hp + 1) * 128], in_=qn[:, hp * 128:(hp + 1) * 128], identity=ident_b[:])
                            return ins
                        Tt(trq, [qn.b, ident_b.b], [pqT.b])
                        yield
                        Vv(lambda e: e.tensor_copy(out=qT[:].rearrange("p a t -> p (a t)"), in_=pqTv[:, 0:512]), [pqT.b], [qT.b])
                for f_ in pend_mix:
                    yield from f_()
                yield
                yield from trans()

            def stage2(T, gen):
                tl = T - t0
                qT = qTs[T % 3]
                st = stat[T % 2]
                kts = [kt for kt in range(5) if T - 4 + kt >= 0]
                po = [banks[6], banks[7]]
                pend = []
                for hh in range(8):
                    hp, hl = hh // 2, hh % 2
                    pr = slice(64 * hl, 64 * hl + 64)
                    psA = psum()
                    psD = psum()

                    def mms(e, hh=hh, hp=hp, pr=pr, psA=psA, psD=psD):
                        for kt in kts:
                            slot = (T - 4 + kt) % 8
                            dst = psD[:, 0:128] if kt == 4 else psA[:, kt * 128:(kt + 1) * 128]
                            ins = e.matmul(dst, lhsT=KT[pr, hp, slot * 128:(slot + 1) * 128], rhs=qT[pr, hp, :], start=True, stop=True)
                        return ins
                    Tt(mms, [qT.b] + [KTb[(T - 4 + kt) % 8] for kt in kts], [psA.b, psD.b])
                    gen()
                    PT = PTs[pt_i[0] % 4]
                    pt_i[0] += 1
                    ka = [kt for kt in kts if kt < 4]

                    def ex(e, psA=psA, psD=psD, PT=PT, ka=ka):
                        if ka:
                            e.activation(out=PT[:, ka[0]:4, :], in_=psA[:, ka[0] * 128:512].rearrange("p (k t) -> p k t", t=128), func=AF.Exp)
                        return e.activation(out=PT[:, 4, :], in_=psD[:, 0:128], func=AF.Exp)
                    Aa(ex, [psA.b, psD.b], [PT.b])
                    k0 = kts[0]
                    P.op("vector",
                         lambda e, PT=PT, hh=hh, k0=k0: e.tensor_tensor(out=PT[:, k0:5, :], in0=PT[:, k0:5, :], in1=biasT[:, hh, k0:5, :], op=ALU.mult),
                         [PT.b, biasT.b], [PT.b])
                    pob = po[hh // 4]

                    def mmo(e, hh=hh, PT=PT, pob=pob):
                        for i, kt in enumerate(kts):
                            slot = (T - 4 + kt) % 8
                            ins = e.matmul(pob[:, (hh % 4) * 65:(hh % 4) * 65 + 65], lhsT=PT[:, kt, :], rhs=Vx[:, slot, hh, :],
                                           start=(i == 0), stop=(i == len(kts) - 1))
                        return ins
                    pend.append((mmo, [PT.b] + [Vxb[(T - 4 + kt) % 8] for kt in kts], [pob.b]))
                    if len(pend) > 2:
                        Tt(*pend.pop(0))
                    gen()
                while pend:
                    Tt(*pend.pop(0))
                for half in range(2):
                    pob = po[half]
                    pv4 = pob[:, 0:260].rearrange("p (h d) -> p h d", d=65)
                    Vv(lambda e, pv4=pv4, half=half: e.reciprocal(out=st[:, 64 + half * 4: 68 + half * 4].unsqueeze(2), in_=pv4[:, :, 64:65]),
                       [pob.b], [st.sb("rs%d" % half)])
                    Vv(lambda e, pv4=pv4, half=half: e.tensor_tensor(out=oc[:, half * 4:(half + 1) * 4, :], in0=pv4[:, :, 0:64],
                                                                     in1=st[:, 64 + half * 4: 68 + half * 4].unsqueeze(2).to_broadcast([128, 4, 64]), op=ALU.mult),
                       [pob.b, st.sb("rs%d" % half)], [oc.b])
                Gg(lambda e: e.tensor_tensor(out=oc2[:], in0=oc[:], in1=oc[:], op=ALU.mult), [oc.b], [oc2.b])
                Vv(lambda e: e.tensor_reduce(out=st[:, 72:80], in_=oc2[:], axis=AX.X, op=ALU.add), [oc2.b], [st.sb("s4")])
                Aa(lambda e: e.activation(out=st[:, 80:88], in_=st[:, 72:80], func=AF.Ln, bias=eps_t[:], scale=1.0 / 64.0),
                   [st.sb("s4"), eps_t.b], [st.sb("ln4")])
                Aa(lambda e: e.activation(out=st[:, 80:88], in_=st[:, 80:88], func=AF.Exp, scale=-0.5), [st.sb("ln4")], [st.sb("r4")])
                Vv(lambda e: e.tensor_tensor(out=oc2[:], in0=oc[:], in1=st[:, 80:88].unsqueeze(2).to_broadcast([128, 8, 64]), op=ALU.mult),
                   [oc.b, st.sb("r4")], [oc2.b])
                Gg(lambda e: e.tensor_tensor(out=ocn[:], in0=oc2[:].rearrange("p h d -> p (h d)"), in1=g_oc[:], op=ALU.mult), [oc2.b, g_oc.b], [ocn.b])
                DMA("gpsimd", oac_d[tl * 128:(tl + 1) * 128, 256:768], ocn[:], [ocn.b], [oac_bufs[tl]], store=True)

            halo = [T for T in range(t0 - 4, t0) if T >= 0]
            for T in halo:
                for _ in stage1(T, False):
                    pass
            for T in (t0, t0 + 1):
                if T < t0 + NT:
                    for _ in stage1(T, True):
                        pass
            genq = []

            def adv():
                while genq:
                    try:
                        next(genq[0][1])
                        return
                    except StopIteration:
                        genq.pop(0)

            def drain_upto(Tmax):
                while genq and genq[0][0] <= Tmax:
                    for _ in genq[0][1]:
                        pass
                    genq.pop(0)

            for tl in range(NT):
                if tl % 4 == 2:
                    drain_upto(t0 + tl + 1)
                    ut_batch(tl - 2)
                if tl + 2 < NT:
                    genq.append((t0 + tl + 2, stage1(t0 + tl + 2, True)))
                stage2(t0 + tl, adv)
                drain_upto(t0 + tl + 1)
            drain_upto(t0 + NT)
            P.barrier()
            sba.release(mk_A)

            phase_s5(nc, P, sba, dr, l, seg, cfg, UT, UTb, obT, s5_state, banks, eps_t, ident_f, dbg_d, SEG, NCH)
            P.barrier()
            sba.release(mk_UT)

            phase_c(nc, P, sba, dr, l, seg, cfg, obT, oac_d, oac_bufs, x_in, x_out, xin_bufs, banks, eps_t, ident_f, ident_b,
                    dbg_d, SEG, NT, NB, NBT, t0)
            P.barrier()
            sba.release(mk_seg)

        for seg in range(NSEG):
            do_seg(l, seg, x_in, x_out)

    P.op("sync", None, reads=xin_bufs[L], writes=())
    if dbg:
        P.op("sync", None, reads=[dbg_d["_b"]], writes=())
    block = stack.enter_context(nc.Block())
    P.emit(block)
    stack.close()
    return nc, sba.peak, P.nops


def cmul(P, eng, o_re, o_im, a_re, a_im, b_re, b_im, t1, t2, rb, wb):
    def f(e):
        e.tensor_tensor(out=t1, in0=a_re, in1=b_re, op=ALU.mult)
        e.tensor_tensor(out=t2, in0=a_im, in1=b_im, op=ALU.mult)
        e.tensor_tensor(out=o_re, in0=t1, in1=t2, op=ALU.subtract)
        e.tensor_tensor(out=t1, in0=a_re, in1=b_im, op=ALU.mult)
        e.tensor_tensor(out=t2, in0=a_im, in1=b_re, op=ALU.mult)
        return e.tensor_tensor(out=o_im, in0=t1, in1=t2, op=ALU.add)
    P.op(eng, f, rb, wb)


def _mk(P):
    def Vv(fn, r, w):
        P.op("vector", fn, r, w)

    def Aa(fn, r, w):
        P.op("scalar", fn, r, w)

    def Gg(fn, r, w):
        P.op("gpsimd", fn, r, w)

    def Tt(fn, r, w):
        P.op("tensor", fn, r, w)

    def DMA(eng, out_ap, in_ap, r, w, store=False):
        P.op(eng, lambda e: e.dma_start(out=out_ap, in_=in_ap), r, w, dma=True, store=store)
    return Vv, Aa, Gg, Tt, DMA


def powers(P, sba, name, lre, lim, ldt, rb, n, nk, kvec, keep_unit=False):
    Vv, Aa, Gg, Tt, DMA = _mk(P)
    t = lambda nm, shp, dt=F32: sba.tile(name + nm, shp, dt)
    dt_ = t("dt", [128, n]); lm = t("lm", [128, n]); th = t("th", [128, n])
    Aa(lambda e: e.activation(out=dt_[:], in_=ldt, func=AF.Exp), rb, [dt_.b])
    Vv(lambda e: e.tensor_tensor(out=lm[:], in0=lre, in1=dt_[:], op=ALU.mult), rb + [dt_.b], [lm.b])
    Vv(lambda e: e.tensor_tensor(out=th[:], in0=lim, in1=dt_[:], op=ALU.mult), rb + [dt_.b], [th.b])
    Vv(lambda e: e.tensor_scalar(out=th[:], in0=th[:], scalar1=1.0 / TWO_PI, scalar2=None, op0=ALU.mult), [th.b], [th.b])
    kb = kvec[:, 0:nk, :].to_broadcast([128, nk, n])
    mag = t("mag", [128, nk, n]); y = t("y", [128, nk, n]); yi = t("yi", [128, nk, n], I32); yf = t("yf", [128, nk, n])
    sn = t("sn", [128, nk, n]); cs = t("cs", [128, nk, n])
    Vv(lambda e: e.tensor_tensor(out=mag[:], in0=kb, in1=lm[:].unsqueeze(1).to_broadcast([128, nk, n]), op=ALU.mult), [lm.b, kvec.b], [mag.b])
    Aa(lambda e: e.activation(out=mag[:], in_=mag[:], func=AF.Exp), [mag.b], [mag.b])
    Vv(lambda e: e.tensor_tensor(out=y[:], in0=kb, in1=th[:].unsqueeze(1).to_broadcast([128, nk, n]), op=ALU.mult), [th.b, kvec.b], [y.b])
    for (dst, shift) in ((sn, 0.0), (cs, 0.25)):
        if shift:
            Vv(lambda e: e.tensor_scalar(out=y[:], in0=y[:], scalar1=shift, scalar2=None, op0=ALU.add), [y.b], [y.b])
        Vv(lambda e: e.tensor_copy(out=yi[:], in_=y[:]), [y.b], [yi.b])
        Vv(lambda e: e.tensor_copy(out=yf[:], in_=yi[:]), [yi.b], [yf.b])
        Vv(lambda e: e.tensor_tensor(out=yf[:], in0=y[:], in1=yf[:], op=ALU.subtract), [y.b, yf.b], [yf.b])
        Aa(lambda e, dst=dst: e.activation(out=dst[:], in_=yf[:], func=AF.Sin, scale=TWO_PI), [yf.b], [dst.b])
    if keep_unit:
        ark = t("ark", [128, nk, n]); aik = t("aik", [128, nk, n])
    else:
        ark, aik = cs, sn
    Vv(lambda e: e.tensor_tensor(out=ark[:], in0=mag[:], in1=cs[:], op=ALU.mult), [mag.b, cs.b], [ark.b])
    Vv(lambda e: e.tensor_tensor(out=aik[:], in0=mag[:], in1=sn[:], op=ALU.mult), [mag.b, sn.b], [aik.b])
    nr = t("nr", [128, n]); den = t("den", [128, n]); t1 = t("t1", [128, n]); t2 = t("t2", [128, n])
    cr = t("cr", [128, n]); ci = t("ci", [128, n])
    Vv(lambda e: e.tensor_scalar(out=nr[:], in0=ark[:, 1, :], scalar1=-1.0, scalar2=None, op0=ALU.add), [ark.b], [nr.b])
    Vv(lambda e: e.tensor_tensor(out=t1[:], in0=lre, in1=lre, op=ALU.mult), rb, [t1.b])
    Vv(lambda e: e.tensor_tensor(out=t2[:], in0=lim, in1=lim, op=ALU.mult), rb, [t2.b])
    Vv(lambda e: e.tensor_tensor(out=den[:], in0=t1[:], in1=t2[:], op=ALU.add), [t1.b, t2.b], [den.b])
    Vv(lambda e: e.reciprocal(out=den[:], in_=den[:]), [den.b], [den.b])
    Vv(lambda e: e.tensor_tensor(out=t1[:], in0=nr[:], in1=lre, op=ALU.mult), rb + [nr.b, den.b], [t1.b])
    Vv(lambda e: e.tensor_tensor(out=t2[:], in0=aik[:, 1, :], in1=lim, op=ALU.mult), rb + [aik.b, den.b], [t2.b])
    Vv(lambda e: e.tensor_tensor(out=cr[:], in0=t1[:], in1=t2[:], op=ALU.add), [t1.b, t2.b], [cr.b])
    Vv(lambda e: e.tensor_tensor(out=cr[:], in0=cr[:], in1=den[:], op=ALU.mult), [cr.b, den.b], [cr.b])
    Vv(lambda e: e.tensor_tensor(out=t1[:], in0=aik[:, 1, :], in1=lre, op=ALU.mult), rb + [aik.b, cr.b], [t1.b])
    Vv(lambda e: e.tensor_tensor(out=t2[:], in0=nr[:], in1=lim, op=ALU.mult), rb + [nr.b, cr.b], [t2.b])
    Vv(lambda e: e.tensor_tensor(out=ci[:], in0=t1[:], in1=t2[:], op=ALU.subtract), [t1.b, t2.b], [ci.b])
    Vv(lambda e: e.tensor_tensor(out=ci[:], in0=ci[:], in1=den[:], op=ALU.mult), [ci.b, den.b], [ci.b])
    return ark, aik, mag, cs, sn, cr, ci


def cmul_ops(P, eng, o_re, o_im, a_re, a_im, b_re, b_im, t1, t2, rb, ob_re, ob_im, tb1, tb2, neg_im=False):
    def op(fn, r, w):
        P.op(eng, fn, r, w)
    op(lambda e: e.tensor_tensor(out=t1, in0=a_re, in1=b_re, op=ALU.mult), rb, [tb1])
    op(lambda e: e.tensor_tensor(out=t2, in0=a_im, in1=b_im, op=ALU.mult), rb, [tb2])
    op(lambda e: e.tensor_tensor(out=o_re, in0=t1, in1=t2, op=ALU.subtract), [tb1, tb2], [ob_re])
    op(lambda e: e.tensor_tensor(out=t1, in0=a_re, in1=b_im, op=ALU.mult), rb + [ob_re], [tb1])
    op(lambda e: e.tensor_tensor(out=t2, in0=a_im, in1=b_re, op=ALU.mult), rb + [ob_re], [tb2])
    if neg_im:
        op(lambda e: e.scalar_tensor_tensor(out=o_im, in0=t1, scalar=-1.0, in1=t2, op0=ALU.mult, op1=ALU.subtract), [tb1, tb2], [ob_im])
    else:
        op(lambda e: e.tensor_tensor(out=o_im, in0=t1, in1=t2, op=ALU.add), [tb1, tb2], [ob_im])


def phase_s5(nc, P, sba, dr, l, seg, cfg, UT, UTb, obT, s5_state, banks, eps_t, ident_f, dbg_d, SEG, NCH):
    Vv, Aa, Gg, Tt, DMA = _mk(P)
    bi = [0]

    def psum():
        t = banks[bi[0] % 8]
        bi[0] += 1
        return t
    ld = lambda name, shape, src, dt=F32, eng="sync": (lambda t: (DMA(eng, t[:], src, [], [t.b]), t)[1])(sba.tile(name, shape, dt))
    d_bc = ld("d_bc", [128, 256], dr["d_bc"][l])
    Em = ld("Em", [128, 4, 128], dr["c_E"])
    kvec = ld("kvec", [128, 9, 1], dr["c_kvec"])
    glu_b = ld("glu_b", [128, 2], dr["glu_b"][l])
    g_ob = ld("g_ob", [128, 2], dr["g_ob"][l])
    glu_w = ld("glu_w", [128, 2, 256], dr["glu_w"][l].rearrange("(ct p) c -> p ct c", p=128), BF16, "gpsimd")
    blk64 = ld("blk64", [128, 128], dr["c_blk64"], BF16, "gpsimd")
    if seg == 0:
        Vv(lambda e: e.memset(s5_state[:], 0.0), [], [s5_state.b])
    Xp = sba.tile("Xp", [128, 8, 2, NCH + 1], BF16)
    Xpb = [Buf("Xp%d" % i) for i in range(8)]
    BD = sba.tile("BD", [128, 4, 8, 128], BF16)
    CA = sba.tile("CA", [128, 8, 2, 8, 128], BF16)
    mk_L1 = sba.mark()
    if seg == 0:
        WSk = sba.tile("WSk", [128, 8, 2, 512], BF16, top=True)
        mk = sba.mark()
        lamB = ld("lamB", [128, 3, 512], dr["lamB"][l])
        bB = ld("bB", [128, 2, 512], dr["bB"][l])
        lre_c = sba.tile("lre_c", [128, 3, 256], F32)
        Vv(lambda e: e.tensor_copy(out=lre_c[:].rearrange("p w (q c) -> p w q c", q=4),
                                   in_=lamB[:].rearrange("p w (q g c) -> p w q g c", q=4, g=2)[:, :, :, 0, :]), [lamB.b], [lre_c.b])
        arB, aiB, magB, csB, snB, crB, ciB = powers(P, sba, "pB", lre_c[:, 0, :], lre_c[:, 1, :], lre_c[:, 2, :], [lre_c.b], 256, 8, kvec)
        cbr = sba.tile("cbr", [128, 4, 2, 64], F32)
        cbi = sba.tile("cbi", [128, 4, 2, 64], F32)
        wt1 = sba.tile("wt1", [128, 4, 2, 64], F32)
        wt2 = sba.tile("wt2", [128, 4, 2, 64], F32)
        b4 = lambda ap: ap.rearrange("p (q g c) -> p q g c", q=4, g=2)
        c4 = lambda ap: ap.rearrange("p (q c) -> p q c", q=4).unsqueeze(2).to_broadcast([128, 4, 2, 64])
        cmul_ops(P, "vector", cbr[:], cbi[:], b4(bB[:, 0, :]), b4(bB[:, 1, :]), c4(crB[:]), c4(ciB[:]), wt1[:], wt2[:],
                 [bB.b, crB.b, ciB.b], cbr.b, cbi.b, wt1.b, wt2.b)
        wt3 = sba.tile("wt3", [128, 4, 2, 64], F32)
        wt4 = sba.tile("wt4", [128, 4, 2, 64], F32)
        for k in range(8):
            ev = (k % 2 == 0)
            cmul_ops(P, "vector" if ev else "gpsimd", b4(WSk[:, k, 0, :]), b4(WSk[:, k, 1, :]), cbr[:], cbi[:], c4(arB[:, k, :]), c4(aiB[:, k, :]),
                     (wt1 if ev else wt3)[:], (wt2 if ev else wt4)[:], [cbr.b, cbi.b, arB.b, aiB.b], WSk.sb(("r", k)), WSk.sb(("i", k)),
                     (wt1 if ev else wt3).b, (wt2 if ev else wt4).b)
        WSb = [WSk.sb((r, k)) for r in ("r", "i") for k in range(8)]
        P.barrier()
        sba.release(mk)
        Tc = sba.tile("Tc", [128, 8, NCH], F32, top=True)
        Ts = sba.tile("Ts", [128, 8, NCH], F32, top=True)
        rho8 = sba.tile("rho8", [128, 8], F32, top=True)
        mk = sba.mark()
        lamA = ld("lamA", [128, 3, 8], dr["lamA"][l])
        cA = ld("cA", [128, 2, 8, 128], dr["cA"][l])
        bZ = ld("bZ", [128, 2, 8, 64], dr["bZ"][l])
        arA, aiA, magA, csA, snA, crA, ciA = powers(P, sba, "pA", lamA[:, 0, :], lamA[:, 1, :], lamA[:, 2, :], [lamA.b], 8, 9, kvec, keep_unit=True)
        Vv(lambda e: e.tensor_copy(out=rho8[:], in_=magA[:, 8, :]), [magA.b], [rho8.b])
        Vv(lambda e: e.tensor_copy(out=Tc[:, :, 0:1], in_=csA[:, 8, :].unsqueeze(2)), [csA.b], [Tc.b])
        Vv(lambda e: e.tensor_copy(out=Ts[:, :, 0:1], in_=snA[:, 8, :].unsqueeze(2)), [snA.b], [Ts.b])
        mk2 = sba.mark()
        tt1 = sba.tile("tt1", [128, 8, NCH // 2], F32)
        tt2 = sba.tile("tt2", [128, 8, NCH // 2], F32)
        n = 1
        while n < NCH:
            mr = Tc[:, :, n - 1:n].to_broadcast([128, 8, n])
            mi = Ts[:, :, n - 1:n].to_broadcast([128, 8, n])
            cmul_ops(P, "vector", Tc[:, :, n:2 * n], Ts[:, :, n:2 * n], Tc[:, :, 0:n], Ts[:, :, 0:n], mr, mi,
                     tt1[:, :, 0:n], tt2[:, :, 0:n], [Tc.b, Ts.b], Tc.b, Ts.b, tt1.b, tt2.b)
            n *= 2
        P.barrier()
        sba.release(mk2)
        ncim = sba.tile("ncim", [128, 8, 128], F32)
        Vv(lambda e: e.tensor_scalar(out=ncim[:], in0=cA[:, 1, :, :], scalar1=-1.0, scalar2=None, op0=ALU.mult), [cA.b], [ncim.b])
        ct1 = sba.tile("ct1", [128, 8, 128], F32)
        ct2 = sba.tile("ct2", [128, 8, 128], F32)
        for i in range(8):
            ar = arA[:, i + 1, :].unsqueeze(2).to_broadcast([128, 8, 128])
            ai = aiA[:, i + 1, :].unsqueeze(2).to_broadcast([128, 8, 128])
            cmul_ops(P, "vector", CA[:, i, 0, :, :], CA[:, i, 1, :, :], cA[:, 0, :, :], cA[:, 1, :, :], ar, ai,
                     ct1[:], ct2[:], [cA.b, arA.b, aiA.b], CA.sb(("r", i)), CA.sb(("i", i)), ct1.b, ct2.b, neg_im=True)
        CAb = [CA.sb((r, i)) for r in ("r", "i") for i in range(8)]
        cbZr = sba.tile("cbZr", [128, 8, 64], F32)
        cbZi = sba.tile("cbZi", [128, 8, 64], F32)
        zt1a = ct1[:, :, 0:64]
        zt2a = ct2[:, :, 0:64]
        cmul_ops(P, "vector", cbZr[:], cbZi[:], bZ[:, 0, :, :], bZ[:, 1, :, :], crA[:].unsqueeze(2).to_broadcast([128, 8, 64]),
                 ciA[:].unsqueeze(2).to_broadcast([128, 8, 64]), zt1a, zt2a, [bZ.b, crA.b, ciA.b], cbZr.b, cbZi.b, ct1.b, ct2.b)
        Zr = sba.tile("Zr", [128, 4, 8, 64], F32)
        Zi = sba.tile("Zi", [128, 4, 8, 64], F32)
        Ed = sba.tile("Ed", [128, 4, 128], F32)
        for q in range(4):
            ctq = q // 2
            Vv(lambda e, q=q, ctq=ctq: e.tensor_tensor(out=Ed[:, q, :], in0=Em[:, q, :], in1=d_bc[:, ctq * 128:(ctq + 1) * 128], op=ALU.mult),
               [Em.b, d_bc.b], [Ed.sb(q)])
        for half in range(2):
            for ts_ in range(4):
                tau = half * 4 + ts_
                ar = arA[:, tau, :].unsqueeze(2).to_broadcast([128, 8, 64])
                ai = aiA[:, tau, :].unsqueeze(2).to_broadcast([128, 8, 64])
                cmul_ops(P, "vector", Zr[:, ts_, :, :], Zi[:, ts_, :, :], cbZr[:], cbZi[:], ar, ai, zt1a, zt2a,
                         [cbZr.b, cbZi.b, arA.b, aiA.b], Zr.sb(ts_), Zi.sb(ts_), ct1.b, ct2.b)
            Zb = [Zr.sb(t) for t in range(4)] + [Zi.sb(t) for t in range(4)]
            for q in range(4):
                ps = psum()

                def mmk(e, q=q, ps=ps):
                    for pl in range(2):
                        Pp = 2 * q + pl
                        for ts_ in range(4):
                            dst = ps[64 * pl:64 * pl + 64, ts_ * 128:(ts_ + 1) * 128]
                            e.matmul(dst, lhsT=Zr[:, ts_, Pp, :], rhs=cA[:, 0, Pp, :], start=True, stop=False)
                            ins = e.matmul(dst, lhsT=Zi[:, ts_, Pp, :], rhs=ncim[:, Pp, :], start=False, stop=True)
                    return ins
                Tt(mmk, Zb + [cA.b, ncim.b], [ps.b])
                if half == 0:
                    Vv(lambda e, q=q, ps=ps: e.tensor_tensor(out=BD[:, q, 0, :], in0=ps[:, 0:128], in1=Ed[:, q, :], op=ALU.add), [ps.b, Ed.sb(q)], [BD.sb((q, 0))])
                    Vv(lambda e, q=q, ps=ps: e.tensor_copy(out=BD[:, q, 1:4, :], in_=ps[:, 128:512].rearrange("p (t c) -> p t c", t=3)), [ps.b], [BD.sb((q, 1))])
                else:
                    Vv(lambda e, q=q, ps=ps: e.tensor_copy(out=BD[:, q, 4:8, :], in_=ps[:, 0:512].rearrange("p (t c) -> p t c", t=4)), [ps.b], [BD.sb((q, 2))])
        BDb = [BD.sb((q, k)) for q in range(4) for k in range(3)]
        P.barrier()
        sba.release(mk)

        cd = dr["_s5cache"]
        DMA("sync", cd["WSk"], WSk[:].rearrange("p a b c -> p (a b c)"), WSb, [cd["buf"]], store=True)
        DMA("sync", cd["BD"], BD[:].rearrange("p a b c -> p (a b c)"), BDb, [cd["buf"]], store=True)
        DMA("sync", cd["CA"], CA[:].rearrange("p a b c d -> p (a b c d)"), CAb, [cd["buf"]], store=True)
        DMA("sync", cd["Tc"], Tc[:].rearrange("p a b -> p (a b)"), [Tc.b], [cd["buf"]], store=True)
        DMA("sync", cd["Ts"], Ts[:].rearrange("p a b -> p (a b)"), [Ts.b], [cd["buf"]], store=True)
        DMA("sync", cd["rho8"], rho8[:], [rho8.b], [cd["buf"]], store=True)
    else:
        cd = dr["_s5cache"]
        WSk = sba.tile("WSk", [128, 8, 2, 512], BF16, top=True)
        Tc = sba.tile("Tc", [128, 8, NCH], F32, top=True)
        Ts = sba.tile("Ts", [128, 8, NCH], F32, top=True)
        rho8 = sba.tile("rho8", [128, 8], F32, top=True)
        DMA("sync", WSk[:].rearrange("p a b c -> p (a b c)"), cd["WSk"], [cd["buf"]], [WSk.b])
        DMA("sync", BD[:].rearrange("p a b c -> p (a b c)"), cd["BD"], [cd["buf"]], [BD.b])
        DMA("sync", CA[:].rearrange("p a b c d -> p (a b c d)"), cd["CA"], [cd["buf"]], [CA.b])
        DMA("sync", Tc[:].rearrange("p a b -> p (a b)"), cd["Tc"], [cd["buf"]], [Tc.b])
        DMA("sync", Ts[:].rearrange("p a b -> p (a b)"), cd["Ts"], [cd["buf"]], [Ts.b])
        DMA("sync", rho8[:], cd["rho8"], [cd["buf"]], [rho8.b])
        WSb, BDb, CAb = [WSk.b], [BD.b], [CA.b]

    tmp2 = [[sba.tile("s5t%d_%d" % (k, i), [128, NCH], F32) for i in range(8)] for k in range(2)]
    rhob2 = [sba.tile("rhob%d" % k, [128, NCH], F32) for k in range(2)]
    def do_pair(Pp):
        q, pl = Pp // 2, Pp % 2
        pr = slice(64 * pl, 64 * pl + 64)
        pS = [psum(), psum()]
        for ri in range(2):
            def mms(e, ri=ri, ps=pS[ri]):
                for j in range(8):
                    ins = e.matmul(ps[:, 0:NCH], lhsT=WSk[pr, 7 - j, ri, q * 128:(q + 1) * 128], rhs=UT[pr, q, j * NCH:(j + 1) * NCH], start=(j == 0), stop=(j == 7))
                return ins
            Tt(mms, WSb + UTb, [pS[ri].b])
        a, b_, c, d_, Rr, Ri, Wr, Wi = tmp2[Pp % 2]
        rhob = rhob2[Pp % 2]
        tc, ts = Tc[:, Pp, :], Ts[:, Pp, :]
        sr, si = pS[0][:, 0:NCH], pS[1][:, 0:NCH]
        Vv(lambda e: e.tensor_tensor(out=a[:], in0=sr, in1=tc, op=ALU.mult), [pS[0].b, Tc.b], [a.b])
        Vv(lambda e: e.tensor_tensor(out=b_[:], in0=si, in1=ts, op=ALU.mult), [pS[1].b, Ts.b], [b_.b])
        Vv(lambda e: e.tensor_tensor(out=c[:], in0=si, in1=tc, op=ALU.mult), [pS[1].b, Tc.b], [c.b])
        Vv(lambda e: e.tensor_tensor(out=d_[:], in0=sr, in1=ts, op=ALU.mult), [pS[0].b, Ts.b], [d_.b])
        Gg(lambda e: e.tensor_tensor(out=Rr[:], in0=a[:], in1=b_[:], op=ALU.add), [a.b, b_.b], [Rr.b])
        Gg(lambda e: e.tensor_tensor(out=Ri[:], in0=c[:], in1=d_[:], op=ALU.subtract), [c.b, d_.b], [Ri.b])
        Vv(lambda e: e.tensor_copy(out=rhob[:], in_=rho8[:, Pp:Pp + 1].to_broadcast([128, NCH])), [rho8.b], [rhob.b])
        Vv(lambda e: e.tensor_tensor_scan(out=Wr[:], data0=rhob[:], data1=Rr[:], initial=s5_state[:, Pp, 0:1], op0=ALU.mult, op1=ALU.add),
           [rhob.b, Rr.b, s5_state.b], [Wr.b])
        Vv(lambda e: e.tensor_tensor_scan(out=Wi[:], data0=rhob[:], data1=Ri[:], initial=s5_state[:, Pp, 1:2], op0=ALU.mult, op1=ALU.add),
           [rhob.b, Ri.b, s5_state.b], [Wi.b])
        Vv(lambda e: e.tensor_copy(out=Xp[:, Pp, :, 0:1], in_=s5_state[:, Pp, :].unsqueeze(2)), [s5_state.b], [Xpb[Pp]])
        Gg(lambda e: e.tensor_tensor(out=a[:], in0=Wr[:], in1=tc, op=ALU.mult), [Wr.b, Tc.b], [a.b])
        Gg(lambda e: e.tensor_tensor(out=b_[:], in0=Wi[:], in1=ts, op=ALU.mult), [Wi.b, Ts.b], [b_.b])
        Vv(lambda e: e.tensor_tensor(out=c[:], in0=Wr[:], in1=ts, op=ALU.mult), [Wr.b, Ts.b], [c.b])
        Vv(lambda e: e.tensor_tensor(out=d_[:], in0=Wi[:], in1=tc, op=ALU.mult), [Wi.b, Tc.b], [d_.b])
        Gg(lambda e: e.tensor_tensor(out=Rr[:], in0=a[:], in1=b_[:], op=ALU.subtract), [a.b, b_.b], [Rr.b])
        Vv(lambda e: e.tensor_tensor(out=Ri[:], in0=c[:], in1=d_[:], op=ALU.add), [c.b, d_.b], [Ri.b])
        Aa(lambda e: e.activation(out=Xp[:, Pp, 0, 1:NCH + 1], in_=Rr[:], func=AF.Copy), [Rr.b], [Xpb[Pp]])
        Aa(lambda e: e.activation(out=Xp[:, Pp, 1, 1:NCH + 1], in_=Ri[:], func=AF.Copy), [Ri.b], [Xpb[Pp]])
        Vv(lambda e: e.tensor_copy(out=s5_state[:, Pp, 0:1], in_=Rr[:, NCH - 1:NCH]), [Rr.b], [s5_state.b])
        Vv(lambda e: e.tensor_copy(out=s5_state[:, Pp, 1:2], in_=Ri[:, NCH - 1:NCH]), [Ri.b], [s5_state.b])
    for Pp in range(8):
        do_pair(Pp)
    P.barrier()
    sba.release(mk_L1)
    yg = sba.tile("yg", [128, 2, SEG], BF16)
    ygb = [Buf("yg%d" % c) for c in range(2)]
    for ct in range(2):
        for i in range(8):
            ps = psum()

            def mmy(e, ct=ct, i=i, ps=ps):
                first = True
                for q in (2 * ct, 2 * ct + 1):
                    for j in range(i + 1):
                        e.matmul(ps[:, 0:NCH], lhsT=BD[:, q, i - j, :], rhs=UT[:, q, j * NCH:(j + 1) * NCH], start=first, stop=False)
                        first = False
                for Pp in range(4 * ct, 4 * ct + 4):
                    for ri in range(2):
                        last = (Pp == 4 * ct + 3 and ri == 1)
                        ins = e.matmul(ps[:, 0:NCH], lhsT=CA[:, i, ri, Pp, :], rhs=Xp[:, Pp, ri, 0:NCH], start=False, stop=last)
                return ins
            Tt(mmy, BDb + CAb + UTb + Xpb, [ps.b])
            Aa(lambda e, ct=ct, i=i, ps=ps: e.activation(out=yg[:, ct, i:SEG:8], in_=ps[:, 0:NCH], func=AF.Gelu_apprx_tanh), [ps.b], [ygb[ct]])
    BW = min(512, SEG)
    sg2 = [sba.tile("sg%d" % k, [128, BW], F32) for k in range(2)]
    obf2 = [sba.tile("obf%d" % k, [128, BW], F32) for k in range(2)]
    sqb2 = [sba.tile("sqb%d" % k, [128, BW], BF16) for k in range(2)]
    rs2 = [sba.tile("rs_%d" % k, [128, BW], F32) for k in range(2)]
    dbt = sba.tile("dbt", [128, BW], F32) if dbg_d else None
    its = [(ct2, blk) for ct2 in range(2) for blk in range(SEG // BW)]
    ps2s = {}

    def gluA(n):
        ct2, blk = its[n]
        cs_ = slice(blk * BW, (blk + 1) * BW)
        sg, obf, sqb = sg2[n % 2], obf2[n % 2], sqb2[n % 2]
        ps = psum()

        def mmg(e, ct2=ct2, cs_=cs_, ps=ps):
            for ct in range(2):
                ins = e.matmul(ps[:, 0:BW], lhsT=glu_w[:, ct, ct2 * 128:(ct2 + 1) * 128], rhs=yg[:, ct, cs_], start=(ct == 0), stop=(ct == 1))
            return ins
        Tt(mmg, [glu_w.b] + ygb, [ps.b])
        Aa(lambda e: e.activation(out=sg[:], in_=ps[:, 0:BW], func=AF.Sigmoid, bias=glu_b[:, ct2:ct2 + 1]), [ps.b, glu_b.b], [sg.b])
        Vv(lambda e: e.tensor_tensor(out=obf[:], in0=yg[:, ct2, cs_], in1=sg[:], op=ALU.mult), [sg.b] + ygb, [obf.b])
        Gg(lambda e: e.tensor_tensor(out=sqb[:], in0=obf[:], in1=obf[:], op=ALU.mult), [obf.b], [sqb.b])
        ps2 = psum()
        ps2s[n] = ps2
        Tt(lambda e: e.matmul(ps2[:, 0:BW], lhsT=blk64[:], rhs=sqb[:], start=True, stop=True), [blk64.b, sqb.b], [ps2.b])

    def gluB(n):
        ct2, blk = its[n]
        cs_ = slice(blk * BW, (blk + 1) * BW)
        obf, rs_ = obf2[n % 2], rs2[n % 2]
        ps2 = ps2s[n]
        Aa(lambda e: e.activation(out=rs_[:], in_=ps2[:, 0:BW], func=AF.Ln, bias=eps_t[:], scale=1.0 / 64.0), [ps2.b, eps_t.b], [rs_.b])
        Aa(lambda e: e.activation(out=rs_[:], in_=rs_[:], func=AF.Exp, scale=-0.5), [rs_.b], [rs_.b])
        Vv(lambda e: e.scalar_tensor_tensor(out=obT[:, ct2, cs_], in0=obf[:], scalar=g_ob[:, ct2:ct2 + 1], in1=rs_[:], op0=ALU.mult, op1=ALU.mult),
           [obf.b, g_ob.b, rs_.b], [obT.b])
        if dbg_d:
            Vv(lambda e: e.tensor_copy(out=dbt[:], in_=obT[:, ct2, cs_]), [obT.b], [dbt.b])
            DMA("sync", dbg_d["ob"][:, ct2, cs_], dbt[:], [dbt.b], [dbg_d["_b"]], store=True)

    gluA(0)
    for n in range(len(its)):
        if n + 1 < len(its):
            gluA(n + 1)
        gluB(n)


_dummy_tiles = {}


def cbZ_dummy(sba, name):
    if name not in _dummy_tiles:
        _dummy_tiles[name] = sba.tile(name, [128, 4, 2, 64], F32)
    return _dummy_tiles[name]


def phase_c(nc, P, sba, dr, l, seg, cfg, obT, oac_d, oac_bufs, x_in, x_out, xin_bufs, banks, eps_t, ident_f, ident_b,
            dbg_d, SEG, NT, NB, NBT, t0):
    Vv, Aa, Gg, Tt, DMA = _mk(P)
    bi = [0]

    def psum():
        t = banks[4 + bi[0] % 4]
        bi[0] += 1
        return t
    bf_ = [0]

    def psum_f():
        t = banks[bf_[0] % 8]
        bf_[0] += 1
        return t
    bo = [0]

    def psum_o():
        t = (banks[(bo[0] % 2) * 2], banks[(bo[0] % 2) * 2 + 1])
        bo[0] += 1
        return t
    L_last = (x_out is not None)
    w_out = sba.tile("w_out", [128, 8, 1024], BF16)
    wov = dr["w_out"][l].rearrange("(kc p) c -> p kc c", p=128)
    for kc in range(0, 8, 2):
        DMA("gpsimd", w_out[:, kc:kc + 2, :], wov[:, kc:kc + 2, :], [], [w_out.sb(kc)])
    w_out_b = [w_out.sb(kc) for kc in range(0, 8, 2)]
    gTf = sba.tile("gTf", [128, 1024], F32)
    DMA("sync", gTf[:], dr["gT_ffn"][l].rearrange("p a b -> p (a b)"), [], [gTf.b])
    w_r = sba.tile("w_r", [128, 8, 20], F32)
    DMA("sync", w_r[:], dr["w_r"][l].rearrange("(dc p) c -> p dc c", p=128), [], [w_r.b])
    b_r = sba.tile("b_r", [128, 20], F32)
    DMA("sync", b_r[:], dr["b_r"][l], [], [b_r.b])
    wgu = [sba.tile("wgu%d" % i, [128, 4, 8, 512], BF16) for i in range(2)]
    wdn = [sba.tile("wdn%d" % i, [128, 4, 2, 1024], BF16) for i in range(2)]
    acc = sba.tile("acc", [128, NBT, 1024], F32)
    accb = [Buf("acc%d" % i) for i in range(NBT)]
    h2T = sba.tile("h2T", [128, 8, NB], BF16)
    h2Tb = [Buf("h2T%d" % i) for i in range(NBT)]
    lg = sba.tile("lg", [128, NBT, 20], F32)
    lgb = [Buf("lg%d" % i) for i in range(NBT)]
    gate = sba.tile("gate", [128, NBT, 16], F32)
    xts = [sba.tile("cxt%d" % i, [128, 1024], F32) for i in range(2)]
    oacs = [sba.tile("oact%d" % i, [128, 768], BF16) for i in range(1)]
    oT = sba.tile("oT", [128, 6, 128], BF16)
    st = [sba.tile("cst%d" % i, [128, 8], F32) for i in range(2)]
    h2Tfs = [sba.tile("h2Tf%d" % i, [128, 8, 128], F32) for i in range(2)]
    sl = [sba.tile("sl%d" % i, [128, 256], BF16) for i in range(2)]
    hid = [sba.tile("hid%d" % i, [128, 256], BF16) for i in range(2)]
    hidT = [sba.tile("hidT%d" % i, [128, 2, 128], BF16) for i in range(2)]
    rt = {k: sba.tile("rt_" + k, [128, NBT, n], F32) for k, n in
          (("m", 1), ("gs", 4), ("eg", 4), ("sg", 1), ("pg", 1), ("ohg", 4), ("tmp", 16), ("es", 4), ("m1", 1), ("d1", 4), ("oh1", 4),
           ("msk", 4), ("m2", 1), ("oh2", 4), ("e2", 1), ("w1", 1), ("w2", 1), ("gin", 4), ("gi2", 4))}

    wq = [0]

    def load_group(gidx):
        buf = gidx % 2
        g = gidx % 4
        for e_ in range(4):
            E = 4 * g + e_
            DMA("gpsimd", wgu[buf][:, e_, :, 0:256], dr["w_gate"][l, E].rearrange("(dc p) f -> p dc f", p=128), [], [wgu[buf].sb((e_, 0))])
            DMA("gpsimd", wgu[buf][:, e_, :, 256:512], dr["w_up"][l, E].rearrange("(dc p) f -> p dc f", p=128), [], [wgu[buf].sb((e_, 1))])
            DMA("gpsimd", wdn[buf][:, e_, :, :], dr["w_down"][l, E].rearrange("(fc p) c -> p fc c", p=128), [], [wdn[buf].sb(e_)])

    nblk = SEG // NB
    load_group(0)
    gctr = 0
    for blk in range(nblk):
        def tile_front(tb, blk=blk):
            tl = blk * NBT + tb
            T = t0 + tl
            xt = xts[tb % 2]
            oc_ = oacs[0]
            s_ = st[tb % 2]
            h2Tf = h2Tfs[tb % 2]
            DMA("sync", xt[:], x_in[T * 128:(T + 1) * 128, :], [xin_bufs[l][T]], [xt.b])
            DMA("sync", oc_[:], oac_d[tl * 128:(tl + 1) * 128, :], [oac_bufs[tl]], [oc_.b])
            pt = psum_f()
            ptv = pt[:].bitcast(BF16)

            def tr(e):
                for c in range(6):
                    ins = e.transpose(out=ptv[:, c * 128:(c + 1) * 128], in_=oc_[:, c * 128:(c + 1) * 128], identity=ident_b[:])
                return ins
            Tt(tr, [oc_.b, ident_b.b], [pt.b])
            yield
            Aa(lambda e: e.activation(out=oT[:].rearrange("p a b -> p (a b)"), in_=ptv[:, 0:768], func=AF.Copy), [pt.b], [oT.b])
            yield
            for half in range(2):
                ps = psum_f()

                def mmo(e, half=half, ps=ps):
                    for kc in range(8):
                        if kc < 2:
                            lh = oT[:, kc, :]
                        elif kc < 4:
                            lh = obT[:, kc - 2, tl * 128:(tl + 1) * 128]
                        else:
                            lh = oT[:, kc - 2, :]
                        ins = e.matmul(ps[:, 0:512], lhsT=lh, rhs=w_out[:, kc, half * 512:(half + 1) * 512], start=(kc == 0), stop=(kc == 7))
                    return ins
                Tt(mmo, [oT.b, obT.b] + w_out_b, [ps.b])
                yield
                Vv(lambda e, half=half, ps=ps: e.tensor_tensor(out=acc[:, tb, half * 512:(half + 1) * 512], in0=ps[:, 0:512], in1=xt[:, half * 512:(half + 1) * 512], op=ALU.add),
                   [ps.b, xt.b], [accb[tb]])
                yield
            if dbg_d:
                DMA("sync", dbg_d["xm"][T * 128:(T + 1) * 128, :], acc[:, tb, :], [accb[tb]], [dbg_d["_b"]], store=True)
            Aa(lambda e: e.activation(out=xt[:], in_=acc[:, tb, :], func=AF.Square, scale=1.0 / 32.0, accum_out=s_[:, 0:1]), [accb[tb]], [xt.b, s_.sb("a")])
            Aa(lambda e: e.activation(out=s_[:, 1:2], in_=s_[:, 0:1], func=AF.Ln, bias=eps_t[:], scale=1.0), [s_.sb("a"), eps_t.b], [s_.sb("b")])
            Aa(lambda e: e.activation(out=s_[:, 2:3], in_=s_[:, 1:2], func=AF.Exp, scale=-0.5), [s_.sb("b")], [s_.sb("c")])
            yield
            Vv(lambda e: e.tensor_scalar(out=xt[:], in0=acc[:, tb, :], scalar1=s_[:, 2:3], scalar2=None, op0=ALU.mult), [accb[tb], s_.sb("c")], [xt.b])
            yield
            pa, pb = psum_f(), psum_f()

            def trf(e):
                for dc in range(8):
                    pp = pa if dc < 4 else pb
                    ins = e.transpose(out=pp[:, (dc % 4) * 128:(dc % 4 + 1) * 128], in_=xt[:, dc * 128:(dc + 1) * 128], identity=ident_f[:])
                return ins
            Tt(trf, [xt.b, ident_f.b], [pa.b, pb.b])
            yield
            Vv(lambda e: e.tensor_tensor(out=h2Tf[:, 0:4, :].rearrange("p a b -> p (a b)"), in0=pa[:, 0:512], in1=gTf[:, 0:512], op=ALU.mult), [pa.b, gTf.b], [h2Tf.sb(0)])
            Vv(lambda e: e.tensor_tensor(out=h2Tf[:, 4:8, :].rearrange("p a b -> p (a b)"), in0=pb[:, 0:512], in1=gTf[:, 512:1024], op=ALU.mult), [pb.b, gTf.b], [h2Tf.sb(1)])
            yield
            Gg(lambda e: e.tensor_copy(out=h2T[:, :, tb * 128:(tb + 1) * 128], in_=h2Tf[:]), [h2Tf.sb(0), h2Tf.sb(1)], [h2Tb[tb]])
            pr_ = psum_f()

            def mmr(e):
                for dc in range(8):
                    ins = e.matmul(pr_[:, 0:20], lhsT=h2Tf[:, dc, :], rhs=w_r[:, dc, :], start=(dc == 0), stop=(dc == 7))
                return ins
            Tt(mmr, [h2Tf.sb(0), h2Tf.sb(1), w_r.b], [pr_.b])
            yield
            Vv(lambda e: e.tensor_tensor(out=lg[:, tb, :], in0=pr_[:, 0:20], in1=b_r[:], op=ALU.add), [pr_.b, b_r.b], [lgb[tb]])

        act_f = []
        nxt_tb = 0
        steps = {}
        while nxt_tb < NBT or act_f:
            if nxt_tb < NBT and (not act_f or (len(act_f) == 1 and steps[act_f[0][0]] >= 5)):
                act_f.append((nxt_tb, tile_front(nxt_tb)))
                steps[nxt_tb] = 0
                nxt_tb += 1
            for (tb_, g_) in list(act_f):
                try:
                    next(g_)
                    steps[tb_] += 1
                except StopIteration:
                    act_f.remove((tb_, g_))

        def R(fn, r, w, eng="vector"):
            P.op(eng, fn, [rt[k].b if isinstance(k, str) else k for k in r], [rt[k].b if isinstance(k, str) else k for k in w])
        g4 = lg[:, :, 0:4]
        el = lg[:, :, 4:20].rearrange("p n (g e) -> p n g e", g=4)
        bc = lambda k, n: rt[k][:].to_broadcast([128, NBT, n])
        R(lambda e: e.tensor_reduce(out=rt["m"][:], in_=g4, axis=AX.X, op=ALU.max), lgb, ["m"])
        R(lambda e: e.tensor_tensor(out=rt["gs"][:], in0=g4, in1=bc("m", 4), op=ALU.subtract), lgb + ["m"], ["gs"])
        R(lambda e: e.activation(out=rt["eg"][:], in_=rt["gs"][:], func=AF.Exp), ["gs"], ["eg"], "scalar")
        R(lambda e: e.tensor_reduce(out=rt["sg"][:], in_=rt["eg"][:], axis=AX.X, op=ALU.add), ["eg"], ["sg"])
        R(lambda e: e.reciprocal(out=rt["pg"][:], in_=rt["sg"][:]), ["sg"], ["pg"])
        R(lambda e: e.tensor_single_scalar(out=rt["ohg"][:], in_=rt["gs"][:], scalar=0.0, op=ALU.is_equal), ["gs"], ["ohg"])
        R(lambda e: e.tensor_tensor(out=rt["tmp"][:].rearrange("p n (g e) -> p n g e", g=4), in0=el,
                                    in1=rt["ohg"][:].unsqueeze(3).to_broadcast([128, NBT, 4, 4]), op=ALU.mult), lgb + ["ohg"], ["tmp"])
        R(lambda e: e.tensor_reduce(out=rt["es"][:], in_=rt["tmp"][:].rearrange("p n (g e) -> p n e g", g=4), axis=AX.X, op=ALU.add), ["tmp"], ["es"])
        R(lambda e: e.tensor_reduce(out=rt["m1"][:], in_=rt["es"][:], axis=AX.X, op=ALU.max), ["es"], ["m1"])
        R(lambda e: e.tensor_tensor(out=rt["d1"][:], in0=rt["es"][:], in1=bc("m1", 4), op=ALU.subtract), ["es", "m1"], ["d1"])
        R(lambda e: e.tensor_single_scalar(out=rt["oh1"][:], in_=rt["d1"][:], scalar=0.0, op=ALU.is_equal), ["d1"], ["oh1"])
        R(lambda e: e.scalar_tensor_tensor(out=rt["msk"][:], in0=rt["oh1"][:], scalar=-1e30, in1=rt["d1"][:], op0=ALU.mult, op1=ALU.add), ["oh1", "d1"], ["msk"])
        R(lambda e: e.tensor_reduce(out=rt["m2"][:], in_=rt["msk"][:], axis=AX.X, op=ALU.max), ["msk"], ["m2"])
        R(lambda e: e.tensor_tensor(out=rt["oh2"][:], in0=rt["msk"][:], in1=bc("m2", 4), op=ALU.is_equal), ["msk", "m2"], ["oh2"])
        R(lambda e: e.activation(out=rt["e2"][:], in_=rt["m2"][:], func=AF.Exp), ["m2"], ["e2"], "scalar")
        R(lambda e: e.tensor_scalar(out=rt["w1"][:], in0=rt["e2"][:], scalar1=1.0, scalar2=None, op0=ALU.add), ["e2"], ["w1"])
        R(lambda e: e.reciprocal(out=rt["w1"][:], in_=rt["w1"][:]), ["w1"], ["w1"])
        R(lambda e: e.tensor_tensor(out=rt["w2"][:], in0=rt["e2"][:], in1=rt["w1"][:], op=ALU.mult), ["e2", "w1"], ["w2"])
        R(lambda e: e.tensor_tensor(out=rt["gin"][:], in0=rt["oh1"][:], in1=bc("w1", 4), op=ALU.mult), ["oh1", "w1"], ["gin"])
        R(lambda e: e.tensor_tensor(out=rt["gi2"][:], in0=rt["oh2"][:], in1=bc("w2", 4), op=ALU.mult), ["oh2", "w2"], ["gi2"])
        R(lambda e: e.tensor_tensor(out=rt["gin"][:], in0=rt["gin"][:], in1=rt["gi2"][:], op=ALU.add), ["gin", "gi2"], ["gin"])
        R(lambda e: e.tensor_tensor(out=rt["gin"][:], in0=rt["gin"][:], in1=bc("pg", 4), op=ALU.mult), ["gin", "pg"], ["gin"])
        R(lambda e: e.tensor_tensor(out=gate[:].rearrange("p n (g e) -> p n g e", g=4), in0=rt["ohg"][:].unsqueeze(3).to_broadcast([128, NBT, 4, 4]),
                                    in1=rt["gin"][:].unsqueeze(2).to_broadcast([128, NBT, 4, 4]), op=ALU.mult), ["ohg", "gin"], [gate.b])

        for g in range(4):
            buf = gctr % 2
            last_load = (blk == nblk - 1 and g == 3)
            if not last_load:
                load_group(gctr + 1)
            wbs = [[wgu[buf].sb((e_, 0)), wgu[buf].sb((e_, 1)), wdn[buf].sb(e_)] for e_ in range(4)]
            units = [(tb, e_) for tb in range(NBT) for e_ in range(4)]
            ust = {}

            def stA(i, g=g, buf=buf, wbs=wbs, ust=ust):
                tb, e_ = units[i]
                E = 4 * g + e_
                k = i % 2
                if e_ == 0:
                    ust[("po", tb)] = psum_o()
                pg_ = psum()

                def mm1(e):
                    for dc in range(8):
                        ins = e.matmul(pg_[:, 0:512], lhsT=h2T[:, dc, tb * 128:(tb + 1) * 128], rhs=wgu[buf][:, e_, dc, :], start=(dc == 0), stop=(dc == 7))
                    return ins
                Tt(mm1, [h2Tb[tb]] + wbs[e_], [pg_.b])
                Aa(lambda e: e.activation(out=sl[k][:], in_=pg_[:, 0:256], func=AF.Silu), [pg_.b], [sl[k].b])
                Vv(lambda e: e.scalar_tensor_tensor(out=hid[k][:], in0=pg_[:, 256:512], scalar=gate[:, tb, E:E + 1], in1=sl[k][:],
                                                    op0=ALU.mult, op1=ALU.mult), [pg_.b, gate.b, sl[k].b], [hid[k].b])

            def stB1(i):
                k = i % 2
                pt = psum()
                ptv = pt[:].bitcast(BF16)

                def tr2(e):
                    for fc in range(2):
                        ins = e.transpose(out=ptv[:, fc * 128:(fc + 1) * 128], in_=hid[k][:, fc * 128:(fc + 1) * 128], identity=ident_b[:])
                    return ins
                Tt(tr2, [hid[k].b, ident_b.b], [pt.b])
                Aa(lambda e: e.activation(out=hidT[k][:].rearrange("p a b -> p (a b)"), in_=ptv[:, 0:256], func=AF.Copy), [pt.b], [hidT[k].b])

            def stB2(i, g=g, buf=buf, wbs=wbs, ust=ust, blk=blk):
                tb, e_ = units[i]
                k = i % 2
                po = ust[("po", tb)]

                def mm2(e):
                    for half in range(2):
                        for fc in range(2):
                            ins = e.matmul(po[half][:, 0:512], lhsT=hidT[k][:, fc, :], rhs=wdn[buf][:, e_, fc, half * 512:(half + 1) * 512],
                                           start=(e_ == 0 and fc == 0), stop=(e_ == 3 and fc == 1))
                    return ins
                Tt(mm2, [hidT[k].b] + wbs[e_], [po[0].b, po[1].b])
                if e_ == 3:
                    for half in range(2):
                        Vv(lambda e, half=half: e.tensor_tensor(out=acc[:, tb, half * 512:(half + 1) * 512], in0=acc[:, tb, half * 512:(half + 1) * 512],
                                                                in1=po[half][:, 0:512], op=ALU.add), [po[half].b, accb[tb]], [accb[tb]])
                    if g == 3:
                        T = t0 + blk * NBT + tb
                        DMA("sync", x_out[T * 128:(T + 1) * 128, :], acc[:, tb, :], [accb[tb]], [xin_bufs[l + 1][T]], store=True)

            stA(0)
            for i in range(len(units)):
                if i + 1 < len(units):
                    stA(i + 1)
                stB1(i)
                if i >= 1:
                    stB2(i - 1)
            stB2(len(units) - 1)
            gctr += 1


_CACHE = {}


def run(inputs, S, L, n_cores, debug=False, SEG=None):
    lay = host_layouts(inputs, L)
    shapes = {k: v.shape for k, v in lay.items()}
    shapes["x"] = (S, D)
    key = (S, L, n_cores, debug, SEG)
    if key not in _CACHE:
        _CACHE[key] = build(dict(S=S, L=L, debug=debug, SEG=SEG), shapes)
    nc, peak, nops = _CACHE[key]
    x = np.ascontiguousarray(inputs["x"], dtype=np.float32)
    in_maps = []
    for c in range(n_cores):
        m = dict(lay)
        m["x"] = np.ascontiguousarray(x[c])
        in_maps.append(m)
    res = run_bass_kernel_spmd(nc, in_maps, core_ids=list(range(n_cores)))
    return res.results


def kernel(**inputs):
    x = inputs["x"]
    B, S, _ = x.shape
    L = inputs["w_in"].shape[0]
    results = run(inputs, S, L, B)
    return np.stack([np.asarray(r["out"], dtype=np.float32) for r in results], axis=0)
```
